# Optimizing a Trainium2 kernel written in Bass

```python
import jax, jax.numpy as jnp
from jax import lax
import numpy as np

D_MODEL = 1024
BATCH = 8
SEQ = 4096
DEPTH = 4

GRID_W = 64
N_MEM = 256
M_HEADS = 4
M_DQK = 64
M_DV = 128
M_CHUNK = 128
M_CONV = 5
NA_HEADS = 4
NA_DH = 64
NA_KH_MAX = 8
NA_KW = 16
MEM_HEADS = 4
MEM_DH = 64
N_EXPERTS = 16
EC_CAPACITY = 2
D_FF_EXPERT = 2048

N_GATES = 4 * M_HEADS
MIX_WIDTH = M_HEADS * M_DV + NA_HEADS * NA_DH + MEM_HEADS * MEM_DH
COL_SIZES = (M_HEADS * M_DQK, M_HEADS * M_DQK, M_HEADS * M_DV, M_HEADS * M_DV, N_GATES,
             NA_HEADS * NA_DH, NA_HEADS * NA_DH, NA_HEADS * NA_DH, MEM_HEADS * MEM_DH)
D_IN_PROJ = sum(COL_SIZES)
EPS = 1e-6

kernel_name = 'hybrid_mlstm_natten_ec_moe_encoder'


def rms_norm(x, g):
    xf = x.astype(jnp.float32)
    y = xf * lax.rsqrt(jnp.mean(xf * xf, axis=-1, keepdims=True) + EPS)
    return (y * g.astype(jnp.float32)).astype(x.dtype)


def to_heads(t, n_heads):
    bsz, s, _ = t.shape
    return t.reshape(bsz, s, n_heads, -1).transpose(0, 2, 1, 3)


def from_heads(t):
    bsz, nh, s, d = t.shape
    return t.transpose(0, 2, 1, 3).reshape(bsz, s, nh * d)


def split_columns(proj):
    parts, start = [], 0
    for size in COL_SIZES:
        parts.append(proj[..., start:start + size])
        start += size
    return parts


def centred_dwconv(u, w):
    kw = w.shape[0]
    pad = kw // 2
    s = u.shape[1]
    up = jnp.pad(u, ((0, 0), (pad, pad), (0, 0)))
    return sum(up[:, j:j + s] * w[j] for j in range(kw))


def mlstm_chunkwise(q, k, v, log_i, log_f):
    bsz, nh, s, dk = q.shape
    dv = v.shape[-1]
    L = M_CHUNK
    nc = s // L
    q = q.reshape(bsz, nh, nc, L, dk) * (dk ** -0.5)
    k = k.reshape(bsz, nh, nc, L, dk)
    v = v.reshape(bsz, nh, nc, L, dv)
    li = log_i.reshape(bsz, nh, nc, L)
    b = jnp.cumsum(log_f.reshape(bsz, nh, nc, L), axis=-1)
    g = b[..., -1]
    earlier_or_same = jnp.tril(jnp.ones((L, L), dtype=bool))
    d_log = jnp.where(earlier_or_same, b[..., :, None] - b[..., None, :] + li[..., None, :], -jnp.inf)
    w_log = g[..., None] - b + li
    a = jnp.max(w_log, axis=-1)
    w = jnp.exp(w_log - a[..., None])
    kv_chunk = jnp.einsum('bhcsk,bhcsv->bhckv', k * w[..., None], v)
    n_chunk = jnp.einsum('bhcs,bhcsk->bhck', w, k)

    def step(carry, inp):
        c_st, n_st, m_st = carry
        kv_c, n_c, a_c, g_c = inp
        m_new = jnp.maximum(g_c + m_st, a_c)
        s_prev = jnp.exp(g_c + m_st - m_new)
        s_cur = jnp.exp(a_c - m_new)
        c_new = s_prev[..., None, None] * c_st + s_cur[..., None, None] * kv_c
        n_new = s_prev[..., None] * n_st + s_cur[..., None] * n_c
        return (c_new, n_new, m_new), (c_st, n_st, m_st)

    init = (jnp.zeros((bsz, nh, dk, dv), jnp.float32),
            jnp.zeros((bsz, nh, dk), jnp.float32),
            jnp.zeros((bsz, nh), jnp.float32))
    xs = (jnp.moveaxis(kv_chunk, 2, 0), jnp.moveaxis(n_chunk, 2, 0), jnp.moveaxis(a, 2, 0), jnp.moveaxis(g, 2, 0))
    _, (c_prev, n_prev, m_prev) = lax.scan(step, init, xs)
    c_prev = jnp.moveaxis(c_prev, 0, 2)
    n_prev = jnp.moveaxis(n_prev, 0, 2)
    m_prev = jnp.moveaxis(m_prev, 0, 2)
    inter_log = b + m_prev[..., None]
    m_t = jnp.maximum(inter_log, jnp.max(d_log, axis=-1))
    s_inter = jnp.exp(inter_log - m_t)
    scores = jnp.einsum('bhctk,bhcsk->bhcts', q, k) * jnp.exp(d_log - m_t[..., None])
    num = s_inter[..., None] * jnp.einsum('bhctk,bhckv->bhctv', q, c_prev) + jnp.einsum('bhcts,bhcsv->bhctv', scores, v)
    den = s_inter * jnp.einsum('bhctk,bhck->bhct', q, n_prev) + jnp.sum(scores, axis=-1)
    h = num / jnp.maximum(jnp.abs(den), jnp.exp(-m_t))[..., None]
    return h.reshape(bsz, nh, s, dv)


def mlstm_mixer(q, k, v, o, gate_pre, b_gate, conv_w, g_head):
    dtype = v.dtype
    bsz, s, _ = v.shape
    qk = jax.nn.silu(centred_dwconv(jnp.concatenate([q, k], axis=-1), conv_w))
    q, k = jnp.split(qk, 2, axis=-1)
    q = to_heads(q, M_HEADS).astype(jnp.float32)
    k = to_heads(k, M_HEADS).astype(jnp.float32)
    v = to_heads(v, M_HEADS).astype(jnp.float32)
    gates = (gate_pre + b_gate).astype(jnp.float32).reshape(bsz, s, 4, M_HEADS).transpose(2, 0, 3, 1)
    i_fw, f_fw, i_bw, f_bw = gates[0], gates[1], gates[2], gates[3]
    flip = lambda t: jnp.flip(t, axis=2)
    h_fw = mlstm_chunkwise(q, k, v, i_fw, jax.nn.log_sigmoid(f_fw))
    h_bw = flip(mlstm_chunkwise(flip(q), flip(k), flip(v), flip(i_bw), jax.nn.log_sigmoid(flip(f_bw))))
    h = rms_norm(h_fw + h_bw, g_head.reshape(M_HEADS, 1, M_DV))
    return (jax.nn.sigmoid(o.astype(jnp.float32)) * from_heads(h)).astype(dtype)


def neighbourhood_attention(q, k, v, gq, gk, rpb):
    bsz, s, _ = q.shape
    rows = s // GRID_W
    kh = min(NA_KH_MAX, rows)
    q = rms_norm(to_heads(q, NA_HEADS), gq)
    k = rms_norm(to_heads(k, NA_HEADS), gk)
    v = to_heads(v, NA_HEADS)
    qg = q.reshape(bsz, NA_HEADS, rows, GRID_W, NA_DH)
    kg = k.reshape(bsz, NA_HEADS, rows, GRID_W, NA_DH)
    vg = v.reshape(bsz, NA_HEADS, rows, GRID_W, NA_DH)
    r_idx = jnp.arange(rows)
    row_start = jnp.clip(r_idx - kh // 2, 0, rows - kh)
    c_idx = jnp.arange(GRID_W)
    col_start = jnp.clip(c_idx - NA_KW // 2, 0, GRID_W - NA_KW)
    col_win = col_start[:, None] + jnp.arange(NA_KW)[None, :]
    dc = col_win - c_idx[:, None]
    scale = NA_DH ** -0.5

    def row_block(args):
        q_r, r, rs = args
        k_r = lax.dynamic_slice_in_dim(kg, rs, kh, axis=2)
        v_r = lax.dynamic_slice_in_dim(vg, rs, kh, axis=2)
        k_w = k_r[:, :, :, col_win]
        v_w = v_r[:, :, :, col_win]
        dr = rs + jnp.arange(kh) - r
        bias = rpb[:, dr[None, :, None] + NA_KH_MAX - 1, dc[:, None, :] + NA_KW - 1]
        sc = jnp.einsum('bhwd,bhiwjd->bhwij', q_r, k_w).astype(jnp.float32) * scale + bias.astype(jnp.float32)
        p = jax.nn.softmax(sc.reshape(sc.shape[:3] + (kh * NA_KW,)), axis=-1).reshape(sc.shape).astype(v_w.dtype)
        return jnp.einsum('bhwij,bhiwjd->bhwd', p, v_w)

    out = lax.map(row_block, (jnp.moveaxis(qg, 2, 0), r_idx, row_start))
    out = jnp.moveaxis(out, 0, 2).reshape(bsz, NA_HEADS, s, NA_DH)
    return from_heads(out)


def memory_cross_attention(q, mem_h, w_kv, gq, gk):
    q = rms_norm(to_heads(q, MEM_HEADS), gq)
    kv = jnp.einsum('bmd,de->bme', mem_h, w_kv)
    k_m, v_m = jnp.split(kv, 2, axis=-1)
    k_m = rms_norm(to_heads(k_m, MEM_HEADS), gk)
    v_m = to_heads(v_m, MEM_HEADS)
    sc = jnp.einsum('bhqd,bhmd->bhqm', q, k_m).astype(jnp.float32) * (MEM_DH ** -0.5)
    p = jax.nn.softmax(sc, axis=-1).astype(v_m.dtype)
    return from_heads(jnp.einsum('bhqm,bhmd->bhqd', p, v_m))


def expert_choice_ffn(h, w_router, w1, w3, w2):
    bsz, s, d = h.shape
    cap = EC_CAPACITY * s // N_EXPERTS
    aff = jax.nn.softmax(jnp.einsum('bsd,de->bse', h, w_router).astype(jnp.float32), axis=-1)
    gate, idx = lax.top_k(jnp.swapaxes(aff, 1, 2), cap)
    xe = jax.vmap(lambda hb, ib: hb[ib])(h, idx)
    hid = jax.nn.silu(jnp.einsum('becd,edf->becf', xe, w1)) * jnp.einsum('becd,edf->becf', xe, w3)
    ye = jnp.einsum('becf,efd->becd', hid, w2) * gate[..., None].astype(h.dtype)
    return jax.vmap(lambda yb, ib: jnp.zeros((s, d), h.dtype).at[ib.reshape(-1)].add(yb.reshape(-1, d)))(ye, idx)


def setup_inputs(seed: int = 0) -> dict:
    key = jax.random.key(seed)
    ks = jax.random.split(key, 24)
    nrm = lambda k, shape, sc: jax.random.normal(k, shape, jnp.float32) * sc
    gain = lambda k, shape: 1.0 + 0.02 * jax.random.normal(k, shape, jnp.float32)
    f_bias = jnp.linspace(3.0, 6.0, M_HEADS)
    zeros_h = jnp.zeros((M_HEADS,), jnp.float32)
    gate_base = jnp.concatenate([zeros_h, f_bias, zeros_h, f_bias])
    return {
        'x': nrm(ks[0], (BATCH, SEQ, D_MODEL), 1.0),
        'mem': nrm(ks[1], (BATCH, N_MEM, D_MODEL), 1.0),
        'g_mix': gain(ks[2], (DEPTH, D_MODEL)),
        'w_in': nrm(ks[3], (DEPTH, D_MODEL, D_IN_PROJ), D_MODEL ** -0.5),
        'b_gates': gate_base[None, :] + nrm(ks[4], (DEPTH, N_GATES), 0.1),
        'conv_qk': nrm(ks[5], (DEPTH, M_CONV, 2 * M_HEADS * M_DQK), M_CONV ** -0.5),
        'g_mlstm_head': gain(ks[6], (DEPTH, M_HEADS * M_DV)),
        'na_gq': gain(ks[7], (DEPTH, NA_DH)),
        'na_gk': gain(ks[8], (DEPTH, NA_DH)),
        'na_rpb': nrm(ks[9], (DEPTH, NA_HEADS, 2 * NA_KH_MAX - 1, 2 * NA_KW - 1), 0.1),
        'g_mem': gain(ks[10], (DEPTH, D_MODEL)),
        'w_mem_kv': nrm(ks[11], (DEPTH, D_MODEL, 2 * MEM_HEADS * MEM_DH), D_MODEL ** -0.5),
        'mem_gq': gain(ks[12], (DEPTH, MEM_DH)),
        'mem_gk': gain(ks[13], (DEPTH, MEM_DH)),
        'w_out': nrm(ks[14], (DEPTH, MIX_WIDTH, D_MODEL), MIX_WIDTH ** -0.5),
        'g_ffn': gain(ks[15], (DEPTH, D_MODEL)),
        'w_router': nrm(ks[16], (DEPTH, D_MODEL, N_EXPERTS), D_MODEL ** -0.5),
        'w1': nrm(ks[17], (DEPTH, N_EXPERTS, D_MODEL, D_FF_EXPERT), D_MODEL ** -0.5),
        'w3': nrm(ks[18], (DEPTH, N_EXPERTS, D_MODEL, D_FF_EXPERT), D_MODEL ** -0.5),
        'w2': nrm(ks[19], (DEPTH, N_EXPERTS, D_FF_EXPERT, D_MODEL), D_FF_EXPERT ** -0.5),
    }


def reference(x, mem, g_mix, w_in, b_gates, conv_qk, g_mlstm_head, na_gq, na_gk, na_rpb,
              g_mem, w_mem_kv, mem_gq, mem_gk, w_out, g_ffn, w_router, w1, w3, w2):
    for l in range(DEPTH):
        h = rms_norm(x, g_mix[l])
        proj = jnp.einsum('bsd,de->bse', h, w_in[l])
        mq, mk, mv, mo, mg, nq, nk, nv, cq = split_columns(proj)
        y_m = mlstm_mixer(mq, mk, mv, mo, mg, b_gates[l], conv_qk[l], g_mlstm_head[l])
        y_n = neighbourhood_attention(nq, nk, nv, na_gq[l], na_gk[l], na_rpb[l])
        y_c = memory_cross_attention(cq, rms_norm(mem, g_mem[l]), w_mem_kv[l], mem_gq[l], mem_gk[l])
        x = x + jnp.einsum('bse,ed->bsd', jnp.concatenate([y_m, y_n, y_c], axis=-1), w_out[l])
        x = x + expert_choice_ffn(rms_norm(x, g_ffn[l]), w_router[l], w1[l], w3[l], w2[l])
    return x
```

```python
import contextlib
import numpy as np
import concourse.bass as bass
import concourse.mybir as mybir
from concourse.bass_utils import run_bass_kernel_spmd

F32 = mybir.dt.float32
BF16 = mybir.dt.bfloat16
I32 = mybir.dt.int32
ALU = mybir.AluOpType
AF = mybir.ActivationFunctionType
AX = mybir.AxisListType

ENGS = ("pe", "act", "dve", "pool", "sp")

S = 4096
D = 1024
NT = 32
DIN = 2576
NE = 16
CAP = 512
DFF = 2048
EPS = 1e-6
LN8 = float(np.log(8.0))


class Obj:
    __slots__ = ("name", "ap", "last_write", "readers")

    def __init__(self, name, ap):
        self.name = name
        self.ap = ap
        self.last_write = None
        self.readers = []

    def __getitem__(self, k):
        return self.ap[k]


class Op:
    __slots__ = ("eng", "fn", "deps", "is_dma", "sem", "sobj", "value", "signal", "waits", "epoch", "release")

    def __init__(self, eng, fn, deps, is_dma):
        self.eng = eng
        self.fn = fn
        self.deps = deps
        self.is_dma = is_dma
        self.sem = None
        self.sobj = None
        self.value = None
        self.signal = False
        self.waits = []
        self.epoch = 0
        self.release = False


class StopBuild(Exception):
    pass


_dram_objs = []
_dram_inputs = set()


class FW:
    def __init__(self, nc):
        self.nc = nc
        self.stack = contextlib.ExitStack()
        self.scopes = [self.stack]
        self.ops = []
        self.eng_ops = {e: [] for e in ENGS}
        self.last_eng_op = {e: None for e in ENGS}
        self.dma_since_barrier = []
        self.uid = 0

    @contextlib.contextmanager
    def scope(self):
        st = contextlib.ExitStack()
        self.scopes.append(st)
        try:
            yield
        finally:
            self.scopes.pop()
            st.close()

    def _nm(self, name):
        self.uid += 1
        return "%s_%d" % (name, self.uid)

    def sbuf(self, name, shape, dtype):
        t = self.scopes[-1].enter_context(self.nc.sbuf_tensor(self._nm(name), list(shape), dtype))
        return Obj(name, t)

    def psum(self, name, shape, dtype=F32):
        t = self.scopes[-1].enter_context(self.nc.psum_tensor(self._nm(name), list(shape), dtype))
        return Obj(name, t)

    def dram(self, name, shape, dtype, kind="Internal"):
        t = self.nc.dram_tensor(name, list(shape), dtype, kind=kind)
        o = Obj(name, t.ap())
        if kind != "Internal":
            _dram_objs.append(o)
            if kind == "ExternalInput":
                _dram_inputs.add(name)
        return o

    def op(self, eng, fn, reads=(), writes=(), dma=False, sem_obj=None, extra_deps=()):
        deps = list(extra_deps)
        for o in reads:
            if o.last_write is not None:
                deps.append(o.last_write)
        for o in writes:
            if o.last_write is not None:
                deps.append(o.last_write)
            deps.extend(o.readers)
        op = Op(eng, fn, deps, dma)
        op.epoch = getattr(self, "epoch", 0)
        if dma:
            assert sem_obj is not None
            op.sobj = sem_obj
            self.dma_since_barrier.append(op)
        for o in writes:
            o.last_write = op
            o.readers = []
        for o in reads:
            o.readers.append(op)
        self.ops.append(op)
        self.eng_ops[eng].append(op)
        self.last_eng_op[eng] = op
        lim = getattr(self, "op_limit", 0)
        if lim and len(self.ops) == lim:
            self.op_limit = 0
            raise StopBuild()
        return op

    def dma(self, eng, out_ap, in_ap, reads, writes, sem_obj, **kw):
        return self.op(eng, lambda e: e.dma_start(out=out_ap, in_=in_ap, **kw), reads=reads, writes=writes,
                       dma=True, sem_obj=sem_obj)

    def barrier(self):
        self.nbar = getattr(self, "nbar", 0) + 1
        self._barrier()
        if self.nbar == getattr(self, "stop_at", -1):
            raise StopBuild()

    def _barrier(self):
        tails = [o for o in self.last_eng_op.values() if o is not None]
        seen = {}
        for o in self.dma_since_barrier:
            seen[id(o.sobj)] = o
        deps = tails + list(seen.values())
        self.dma_since_barrier = []
        last = None
        for e in ENGS:
            last = self.op(e, lambda en: en.nop(nofuse=True), extra_deps=deps)
        last.release = True
        self.epoch = getattr(self, "epoch", 0) + 1
        self.last_eng_op = {e: None for e in ENGS}

    def emit(self):
        nc = self.nc
        for op in self.ops:
            for d in op.deps:
                if d.eng == "pe" and op.eng == "pe" and not d.is_dma:
                    continue
                d.signal = True
        eng_sem = {}
        for e in ENGS:
            eng_sem[e] = self.stack.enter_context(nc.semaphore("sem_" + e))
        phys = []
        free = []
        cur_map = {}
        cur_epoch = 0
        eng_cnt = {e: 0 for e in ENGS}
        known = {e: {} for e in ENGS}
        for op in self.ops:
            w = {}
            for d in op.deps:
                if d.eng == "pe" and op.eng == "pe" and not d.is_dma:
                    continue
                if d.is_dma:
                    if d.epoch < cur_epoch:
                        continue
                    rec = phys[cur_map[id(d.sobj)]]
                    s, v = rec[0], rec[1]
                else:
                    s, v = eng_sem[d.eng], d.value
                key = id(s)
                if known[op.eng].get(key, 0) >= v:
                    continue
                if key not in w or w[key][1] < v:
                    w[key] = (s, v)
            for key, (s, v) in w.items():
                known[op.eng][key] = v
            op.waits = list(w.values())
            if op.is_dma:
                k = id(op.sobj)
                if k not in cur_map:
                    if free:
                        cur_map[k] = free.pop()
                    else:
                        phys.append([self.stack.enter_context(nc.semaphore("dsem_%d" % len(phys))), 0])
                        cur_map[k] = len(phys) - 1
                rec = phys[cur_map[k]]
                rec[1] += 16
                op.value = rec[1]
                op.sem = rec[0]
                op.signal = True
            elif op.signal:
                eng_cnt[op.eng] += 1
                op.value = eng_cnt[op.eng]
                op.sem = eng_sem[op.eng]
            if op.release:
                free.extend(cur_map.values())
                cur_map.clear()
                cur_epoch += 1
        final_dma = [(rec[0], rec[1]) for rec in phys]
        self.n_sems = len(phys) + len(ENGS)
        self.n_ops = len(self.ops)

        def run(eng_name, e):
            for op in self.eng_ops[eng_name]:
                for (s, v) in op.waits:
                    e.wait_ge(s, v)
                ins = op.fn(e)
                if op.signal:
                    ins.then_inc(op.sem, 16 if op.is_dma else 1)
            if eng_name == "sp":
                for (s, v) in final_dma:
                    if v > 0:
                        e.wait_ge(s, v)

        with nc.Block() as block:
            @block.tensor
            def _(e):
                run("pe", e)

            @block.scalar
            def _(e):
                run("act", e)

            @block.vector
            def _(e):
                run("dve", e)

            @block.gpsimd
            def _(e):
                run("pool", e)

            @block.sync
            def _(e):
                run("sp", e)
        self.stack.close()


def build(nl, debug=False, stop_at=-1, op_limit=0, ne_decl=NE):
    nc = bass.Bass("TRN2", target_bir_lowering=False)
    del _dram_objs[:]
    _dram_inputs.clear()
    fw = FW(nc)
    fw.stop_at = stop_at
    fw.op_limit = op_limit
    op = fw.op

    def MM(out, lhsT, rhs, R, W, start=True, stop=True):
        op("pe", lambda e: e.matmul(out, lhsT=lhsT, rhs=rhs, start=start, stop=stop), R, W)

    def TR(out, in_, ident, R, W):
        op("pe", lambda e: e.transpose(out=out, in_=in_, identity=ident), R, W)

    def ACT(out, in_, func, R, W, **kw):
        op("act", lambda e: e.activation(out=out, in_=in_, func=func, **kw), R, W)

    def TT(out, in0, in1, alu, R, W, eng="dve"):
        op(eng, lambda e: e.tensor_tensor(out=out, in0=in0, in1=in1, op=alu), R, W)

    def TS(out, in0, s1, s2, op0, op1, R, W, eng="dve", **kw):
        if op1 is None:
            op(eng, lambda e: e.tensor_scalar(out=out, in0=in0, scalar1=s1, scalar2=s2, op0=op0, **kw), R, W)
        else:
            op(eng, lambda e: e.tensor_scalar(out=out, in0=in0, scalar1=s1, scalar2=s2, op0=op0, op1=op1, **kw), R, W)

    def STT(out, in0, scalar, in1, op0, op1, R, W, eng="dve"):
        op(eng, lambda e: e.scalar_tensor_tensor(out=out, in0=in0, scalar=scalar, in1=in1, op0=op0, op1=op1), R, W)

    def CP(out, in_, R, W, eng="dve"):
        op(eng, lambda e: e.tensor_copy(out=out, in_=in_), R, W)

    def MS(ap, val, W, eng="dve"):
        op(eng, lambda e: e.memset(ap, val), (), W)

    def RED(out, in_, alu, R, W):
        op("dve", lambda e: e.tensor_reduce(out=out, in_=in_, axis=AX.X, op=alu), R, W)

    def RCP(out, in_, R, W):
        op("dve", lambda e: e.reciprocal(out=out, in_=in_), R, W)

    def rstd_from_ss(ss_obj, ss_ap, tmp_ap, out_ap, inv_n):
        TS(tmp_ap, ss_ap, inv_n, EPS, ALU.mult, ALU.add, [ss_obj], [ss_obj])
        ACT(tmp_ap, tmp_ap, AF.Sqrt, [ss_obj], [ss_obj])
        RCP(out_ap, tmp_ap, [ss_obj], [ss_obj])

    def din(name, shape, dt=F32):
        return fw.dram(name, shape, dt, kind="ExternalInput")

    x_in = din("x", [S, D])
    mem_in = din("mem", [256, D])
    g_mix = din("g_mix", [nl, D])
    w_in = din("w_in", [nl, D, DIN])
    b_gates = din("b_gates", [nl, 16])
    convw_d = din("convw", [nl, 128, 4, 5])
    g_head = din("g_head", [nl, 512])
    gcols_d = din("gcols", [nl, 128, 4])
    natab = din("natab", [nl, 5, 128, 2560])
    g_mem = din("g_mem", [nl, D])
    w_mkv = din("w_mem_kv", [nl, D, 512])
    w_out = din("w_out", [nl, D, D])
    g_ffn = din("g_ffn", [nl, D])
    w_rt = din("w_router", [nl, D, NE])
    w1 = din("w1", [nl, ne_decl, D, DFF])
    w3 = din("w3", [nl, ne_decl, D, DFF])
    w2 = din("w2", [nl, ne_decl, DFF, D])
    out_d = fw.dram("out", [S, D], F32, kind="ExternalOutput")
    dk = "ExternalOutput"
    qkraw_d = fw.dram("qkraw_d", [512, S], F32, kind=dk)
    so_d = fw.dram("so_d", [S, 512], BF16, kind=dk)
    v_d = fw.dram("v_d", [S, 512], BF16, kind=dk)
    hf_d = fw.dram("hf_d", [S, D], BF16, kind=dk)
    aff_d = fw.dram("aff_d", [S, NE], F32, kind=dk)
    if debug:
        dbg_xmix = fw.dram("dbg_xmix", [S, D], F32, kind=dk)
        dbg_yTm = fw.dram("dbg_yTm", [128, 4, S], BF16, kind=dk)
        dbg_yTnc = fw.dram("dbg_yTnc", [128, 4, S], BF16, kind=dk)
        dbg_qkT = fw.dram("dbg_qkT", [128, 4, S], BF16, kind=dk)
        dbg_idx = fw.dram("dbg_idx", [128, NE * 4], I32, kind=dk)
        dbg_nqT = fw.dram("dbg_nqT", [128, 2, S], BF16, kind=dk)
        dbg_gates = fw.dram("dbg_gates", [128, NT, 16], F32, kind=dk)
    out_tiles = [Obj("out_t%d" % t, None) for t in range(NT)]

    ident_f = fw.sbuf("ident_f", [128, 128], F32)
    ident_b = fw.sbuf("ident_b", [128, 128], BF16)
    io = fw.sbuf("io", [128, 128], F32)
    triU = fw.sbuf("triU", [128, 128], F32)
    triL = fw.sbuf("triL", [128, 128], F32)
    ones_f = fw.sbuf("ones_f", [128, 128], F32)
    ones_b = fw.sbuf("ones_b", [128, 128], BF16)
    iota512 = fw.sbuf("iota512", [128, 512], F32)
    regs = {}

    def _mkreg(e):
        regs["bc"] = e.alloc_register("bcreg")
        return e.reg_mov(regs["bc"], S - 1)
    op("pool", _mkreg, (), ())
    op("pool", lambda e: e.iota(io[:], pattern=[[1, 128]], base=0, channel_multiplier=-1,
                                allow_small_or_imprecise_dtypes=True), (), [io])
    op("pool", lambda e: e.iota(iota512[:], pattern=[[1, 512]], base=0, channel_multiplier=0,
                                allow_small_or_imprecise_dtypes=True), (), [iota512])
    op("dve", lambda e: e.tensor_single_scalar(out=ident_f[:], in_=io[:], scalar=0.0, op=ALU.is_equal), [io], [ident_f])
    CP(ident_b[:], ident_f[:], [ident_f], [ident_b])
    op("dve", lambda e: e.tensor_single_scalar(out=triU[:], in_=io[:], scalar=0.0, op=ALU.is_ge), [io], [triU])
    op("dve", lambda e: e.tensor_single_scalar(out=triL[:], in_=io[:], scalar=0.0, op=ALU.is_le), [io], [triL])
    MS(ones_f[:], 1.0, [ones_f])
    MS(ones_b[:], 1.0, [ones_b])
    try:
        fw.barrier()
        _layers(nl, debug, fw, locals())
    except StopBuild:
        while len(fw.scopes) > 1:
            fw.scopes.pop().close()
        scr = fw.sbuf("scr", [1, 64], F32)
        scrb = fw.sbuf("scrb", [1, 64], BF16)
        scri = fw.sbuf("scri", [1, 64], I32)
        for nm, o in list(locals().items()):
            if isinstance(o, Obj) and o.ap is not None and hasattr(o.ap, "shape") and "dram" in str(type(o.ap.tensor if hasattr(o.ap, "tensor") else "")).lower():
                pass
        for o in _dram_objs:
            ap = o.ap
            ix = tuple([0] * (len(ap.shape) - 2)) + (slice(0, 1), slice(0, 1))
            t = {F32: scr, BF16: scrb, I32: scri}[ap.dtype]
            if o.name in _dram_inputs:
                fw.dma("sp", t[0:1, 0:1], ap[ix], [o], [t], t)
            else:
                fw.dma("sp", ap[ix], t[0:1, 0:1], [t], [o], t)
    fw.emit()
    return nc, fw


def _layers(nl, debug, fw, env):
    globals().update({k: v for k, v in env.items() if k not in ("nl", "debug", "fw", "env")})
    op = fw.op
    for l in range(nl):
        x_src = x_in if l == 0 else out_d
        with fw.scope():
            kmT = fw.sbuf("kmT", [128, 2, 256], BF16)
            vm = fw.sbuf("vm", [128, 2, 4, 65], BF16)
            gates = fw.sbuf("gates", [128, NT, 16], F32)
            aff_all = fw.sbuf("aff_all", [128, NT, NE], F32)
            cnt_all = fw.sbuf("cnt_all", [128, NT, NE], F32)
            gcols = fw.sbuf("gcols", [128, 4], F32)
            fw.dma("sp", gcols[:], gcols_d[l], [gcols_d], [gcols], gcols)

            with fw.scope():
                yT_nc = fw.sbuf("yT_nc", [128, 4, S], BF16)
                with fw.scope():
                    nqT = fw.sbuf("nqT", [128, 2, S], BF16)
                    nkT = fw.sbuf("nkT", [128, 2, S], BF16)
                    cqT = fw.sbuf("cqT", [128, 2, S], BF16)
                    nv = fw.sbuf("nv", [128, NT, 4, 65], BF16)
                    MS(nv[:, :, :, 64:65], 1.0, [nv])
                    MS(vm[:, :, :, 64:65], 1.0, [vm])

                    with fw.scope():
                        gmem_b = fw.sbuf("gmem_b", [128, D], F32)
                        wkv = fw.sbuf("wkv", [128, 8, 512], BF16)
                        mt = [fw.sbuf("mt%d" % i, [128, D], F32) for i in range(2)]
                        mjunk = fw.sbuf("mjunk", [128, D], F32)
                        mn = fw.sbuf("mn", [128, D], BF16)
                        memT = fw.sbuf("memT", [128, 8, 256], BF16)
                        mst = fw.sbuf("mst", [128, 8], F32)
                        msq = fw.sbuf("msq", [128, 256], F32)
                        mss = fw.sbuf("mss", [128, 8], F32)
                        kmn = fw.sbuf("kmn", [128, 4, 64], BF16)
                        pT0 = fw.psum("pT0", [128, 8, 128], BF16)
                        pkv = fw.psum("pkv", [128, 512], F32)
                        fw.dma("sp", gmem_b[:], g_mem[l:l + 1, :].partition_broadcast(128), [g_mem], [gmem_b], gmem_b)
                        fw.dma("pool", wkv[:], w_mkv[l].rearrange("(k p) n -> p k n", p=128), [w_mkv], [wkv], wkv)
                        for i in range(2):
                            fw.dma("sp", mt[i][:], mem_in[i * 128:(i + 1) * 128, :], [mem_in], [mt[i]], mt[i])
                            MS(mst[:, 0:1], 0.0, [mst])
                            ACT(mjunk[:], mt[i][:], AF.Square, [mt[i]], [mjunk, mst], accum_out=mst[:, 0:1])
                            rstd_from_ss(mst, mst[:, 0:1], mst[:, 1:2], mst[:, 2:3], 1.0 / D)
                            STT(mn[:], mt[i][:], mst[:, 2:3], gmem_b[:], ALU.mult, ALU.mult, [mt[i], mst, gmem_b], [mn])
                            for k in range(8):
                                TR(pT0[:, k, :], mn[:, k * 128:(k + 1) * 128], ident_b[:], [mn, ident_b], [pT0])
                            ACT(memT[:, :, i * 128:(i + 1) * 128], pT0[:], AF.Copy, [pT0], [memT])
                        for i in range(2):
                            for k in range(8):
                                MM(pkv[:], memT[:, k, i * 128:(i + 1) * 128], wkv[:, k, :], [memT, wkv], [pkv],
                                   start=(k == 0), stop=(k == 7))
                            CP(vm[:, i, :, 0:64], pkv[:, 256:512].rearrange("p (h d) -> p h d", d=64), [pkv], [vm])
                            MS(mss[:, 0:4], 0.0, [mss])
                            for hh in range(4):
                                ACT(msq[:, hh * 64:(hh + 1) * 64], pkv[:, hh * 64:(hh + 1) * 64], AF.Square, [pkv], [msq, mss],
                                    accum_out=mss[:, hh:hh + 1])
                            rstd_from_ss(mss, mss[:, 0:4], mss[:, 4:8], mss[:, 4:8], 1.0 / 64)
                            TT(kmn[:], pkv[:, 0:256].rearrange("p (h d) -> p h d", d=64),
                               mss[:, 4:8].unsqueeze(2).to_broadcast([128, 4, 64]), ALU.mult, [pkv, mss], [kmn])
                            for hp in range(2):
                                TR(pT0[:, hp, :], kmn[:, 2 * hp:2 * hp + 2, :].rearrange("p h d -> p (h d)"), ident_b[:],
                                   [kmn, ident_b], [pT0])
                            ACT(kmT[:, :, i * 128:(i + 1) * 128], pT0[:, 0:2, :], AF.Copy, [pT0, gcols], [kmT],
                                scale=gcols[:, 3:4])
                    fw.barrier()

                    with fw.scope():
                        wi = fw.sbuf("wi", [128, 8, DIN], BF16)
                        gmix_b = fw.sbuf("gmix_b", [128, D], F32)
                        bg_b = fw.sbuf("bg_b", [128, 16], F32)
                        xt = [fw.sbuf("xt%d" % i, [128, D], F32) for i in range(2)]
                        xjunk = fw.sbuf("xjunk", [128, D], F32)
                        xn = [fw.sbuf("xn%d" % i, [128, D], BF16) for i in range(2)]
                        hT = [fw.sbuf("hT%d" % i, [128, 8, 512], BF16) for i in range(2)]
                        xst = [fw.sbuf("xst%d" % i, [128, 4], F32) for i in range(2)]
                        qkst = [fw.sbuf("qkst%d" % i, [128, 512], F32) for i in range(2)]
                        sot = [fw.sbuf("sot%d" % i, [128, 512], BF16) for i in range(2)]
                        vt = [fw.sbuf("vt%d" % i, [128, 512], BF16) for i in range(2)]
                        nsq = [fw.sbuf("nsq%d" % i, [128, 512], F32) for i in range(2)]
                        nss = [fw.sbuf("nss%d" % i, [128, 24], F32) for i in range(2)]
                        nqn = [fw.sbuf("nqn%d" % i, [128, 512], BF16) for i in range(2)]
                        cqn = [fw.sbuf("cqn%d" % i, [128, 256], BF16) for i in range(2)]
                        pT1 = [fw.psum("pT1_%d" % i, [128, 8, 128], BF16) for i in range(2)]
                        pmm = [fw.psum("pmm%d" % i, [128, 512], F32) for i in range(4)]
                        pqk = [fw.psum("pqk%d" % i, [128, 512], F32) for i in range(2)]
                        fw.dma("pool", wi[:, 0:4, :], w_in[l, 0:512, :].rearrange("(k p) n -> p k n", p=128), [w_in], [wi], wi)
                        fw.dma("pool", wi[:, 4:8, :], w_in[l, 512:1024, :].rearrange("(k p) n -> p k n", p=128), [w_in], [wi], wi)
                        fw.dma("sp", gmix_b[:], g_mix[l:l + 1, :].partition_broadcast(128), [g_mix], [gmix_b], gmix_b)
                        fw.dma("sp", bg_b[:], b_gates[l:l + 1, :].partition_broadcast(128), [b_gates], [bg_b], bg_b)
                        mmi = [0]

                        def p1_prep(st):
                            hTs = hT[st % 2]
                            for tt in range(4):
                                t = st * 4 + tt
                                b = t % 2
                                fw.dma("sp", xt[b][:], x_src[t * 128:(t + 1) * 128, :], [out_tiles[t]], [xt[b]], xt[b])
                                MS(xst[b][:, 0:1], 0.0, [xst[b]])
                                ACT(xjunk[:], xt[b][:], AF.Square, [xt[b]], [xjunk, xst[b]], accum_out=xst[b][:, 0:1])
                                rstd_from_ss(xst[b], xst[b][:, 0:1], xst[b][:, 1:2], xst[b][:, 2:3], 1.0 / D)
                                STT(xn[b][:], xt[b][:], xst[b][:, 2:3], gmix_b[:], ALU.mult, ALU.mult,
                                    [xt[b], xst[b], gmix_b], [xn[b]])
                                for k in range(8):
                                    TR(pT1[b][:, k, :], xn[b][:, k * 128:(k + 1) * 128], ident_b[:], [xn[b], ident_b], [pT1[b]])
                                ACT(hTs[:, :, tt * 128:(tt + 1) * 128], pT1[b][:], AF.Copy, [pT1[b]], [hTs])
                        def p1_body(st):
                            hTs = hT[st % 2]
                            for c in range(4):
                                pq = pqk[c % 2]
                                for k in range(8):
                                    MM(pq[:], wi[:, k, c * 128:(c + 1) * 128], hTs[:, k, :], [wi, hTs], [pq],
                                       start=(k == 0), stop=(k == 7))
                                qs = qkst[c % 2]
                                CP(qs[:], pq[:], [pq], [qs])
                                fw.dma("sp", qkraw_d[c * 128:(c + 1) * 128, st * 512:(st + 1) * 512], qs[:], [qs], [qkraw_d], qs)
                            for tt in range(4):
                                t = st * 4 + tt
                                b = t % 2
                                lh = lambda k: hTs[:, k, tt * 128:(tt + 1) * 128]
                                pm = pmm[mmi[0] % 4]; mmi[0] += 1
                                for k in range(8):
                                    MM(pm[:], lh(k), wi[:, k, 512:1024], [hTs, wi], [pm], start=(k == 0), stop=(k == 7))
                                ACT(vt[b][:], pm[:], AF.Copy, [pm], [vt[b]])
                                fw.dma("sp", v_d[t * 128:(t + 1) * 128, :], vt[b][:], [vt[b]], [v_d], vt[b])
                                pm = pmm[mmi[0] % 4]; mmi[0] += 1
                                for k in range(8):
                                    MM(pm[:], lh(k), wi[:, k, 1024:1536], [hTs, wi], [pm], start=(k == 0), stop=(k == 7))
                                ACT(sot[b][:], pm[:], AF.Sigmoid, [pm], [sot[b]])
                                fw.dma("sp", so_d[t * 128:(t + 1) * 128, :], sot[b][:], [sot[b]], [so_d], sot[b])
                                pm = pmm[mmi[0] % 4]; mmi[0] += 1
                                for k in range(8):
                                    MM(pm[:], lh(k), wi[:, k, 1552:2064], [hTs, wi], [pm], start=(k == 0), stop=(k == 7))
                                MS(nss[b][:, 0:8], 0.0, [nss[b]])
                                for hh in range(8):
                                    ACT(nsq[b][:, hh * 64:(hh + 1) * 64], pm[:, hh * 64:(hh + 1) * 64], AF.Square, [pm], [nsq[b], nss[b]],
                                        accum_out=nss[b][:, hh:hh + 1])
                                rstd_from_ss(nss[b], nss[b][:, 0:8], nss[b][:, 8:16], nss[b][:, 8:16], 1.0 / 64)
                                TT(nqn[b][:].rearrange("p (h d) -> p h d", d=64), pm[:].rearrange("p (h d) -> p h d", d=64),
                                   nss[b][:, 8:16].unsqueeze(2).to_broadcast([128, 8, 64]), ALU.mult, [pm, nss[b]], [nqn[b]])
                                for j in range(4):
                                    TR(pT1[b][:, j, :], nqn[b][:, j * 128:(j + 1) * 128], ident_b[:], [nqn[b], ident_b], [pT1[b]])
                                ACT(nqT[:, :, t * 128:(t + 1) * 128], pT1[b][:, 0:2, :], AF.Copy, [pT1[b], gcols], [nqT],
                                    scale=gcols[:, 0:1])
                                ACT(nkT[:, :, t * 128:(t + 1) * 128], pT1[b][:, 2:4, :], AF.Copy, [pT1[b], gcols], [nkT],
                                    scale=gcols[:, 1:2])
                                pm = pmm[mmi[0] % 4]; mmi[0] += 1
                                for k in range(8):
                                    MM(pm[:], lh(k), wi[:, k, 2064:2576], [hTs, wi], [pm], start=(k == 0), stop=(k == 7))
                                CP(nv[:, t, :, 0:64], pm[:, 0:256].rearrange("p (h d) -> p h d", d=64), [pm], [nv])
                                MS(nss[b][:, 16:20], 0.0, [nss[b]])
                                for hh in range(4):
                                    ACT(nsq[b][:, hh * 64:(hh + 1) * 64], pm[:, 256 + hh * 64:256 + (hh + 1) * 64], AF.Square, [pm],
                                        [nsq[b], nss[b]], accum_out=nss[b][:, 16 + hh:17 + hh])
                                rstd_from_ss(nss[b], nss[b][:, 16:20], nss[b][:, 20:24], nss[b][:, 20:24], 1.0 / 64)
                                TT(cqn[b][:].rearrange("p (h d) -> p h d", d=64), pm[:, 256:512].rearrange("p (h d) -> p h d", d=64),
                                   nss[b][:, 20:24].unsqueeze(2).to_broadcast([128, 4, 64]), ALU.mult, [pm, nss[b]], [cqn[b]])
                                for j in range(2):
                                    TR(pT1[b][:, 4 + j, :], cqn[b][:, j * 128:(j + 1) * 128], ident_b[:], [cqn[b], ident_b], [pT1[b]])
                                ACT(cqT[:, :, t * 128:(t + 1) * 128], pT1[b][:, 4:6, :], AF.Copy, [pT1[b], gcols], [cqT],
                                    scale=gcols[:, 2:3])
                                pm = pmm[mmi[0] % 4]; mmi[0] += 1
                                for k in range(8):
                                    MM(pm[:, 0:16], lh(k), wi[:, k, 1536:1552], [hTs, wi], [pm], start=(k == 0), stop=(k == 7))
                                TT(gates[:, t, :], pm[:, 0:16], bg_b[:], ALU.add, [pm, bg_b], [gates])

                        p1_prep(0)
                        for st in range(8):
                            if st + 1 < 8:
                                p1_prep(st + 1)
                            p1_body(st)
                    fw.barrier()

                    if debug and l == 0:
                        fw.dma("sp", dbg_nqT.ap, nqT[:], [nqT], [dbg_nqT], nqT)
                        fw.barrier()
                    with fw.scope():
                        EB = [fw.sbuf("EB%d" % i, [128, 2560], BF16) for i in range(5)]
                        tabst = fw.sbuf("tabst", [128, 2560], F32)
                        Ena = [fw.sbuf("Ena%d" % i, [128, 640], BF16) for i in range(2)]
                        PTn = [fw.sbuf("PTn%d" % i, [128, 640], BF16) for i in range(2)]
                        Ec = [fw.sbuf("Ec%d" % i, [128, 256], BF16) for i in range(2)]
                        ycat = [fw.sbuf("ycat%d" % i, [128, 512], BF16) for i in range(2)]
                        rec = [fw.sbuf("rec%d" % i, [128, 8], F32) for i in range(2)]
                        ps_na = [fw.psum("ps_na%d" % i, [128, 1024], F32) for i in range(2)]
                        ps_c = fw.psum("ps_c", [128, 2, 256], F32)
                        po_na = fw.psum("po_na", [128, 4, 65], F32)
                        po_c = fw.psum("po_c", [128, 4, 65], F32)
                        pT3 = fw.psum("pT3", [128, 4, 128], BF16)
                        for p in range(5):
                            fw.dma("sp", tabst[:], natab[l, p], [natab], [tabst], tabst)
                            ACT(EB[p][:], tabst[:], AF.Exp, [tabst], [EB[p]])
                        ui = 0
                        for j in range(NT):
                            kb = min(max(j - 2, 0), 27)
                            pat = {0: 0, 1: 1, 30: 3, 31: 4}.get(j, 2)
                            yb = ycat[j % 2]
                            rb = rec[j % 2]
                            qs = slice(j * 128, (j + 1) * 128)
                            for h in range(4):
                                hp, lo = h // 2, (h % 2) * 64
                                pn = ps_na[ui % 2]
                                En = Ena[ui % 2]
                                Pn = PTn[ui % 2]
                                Ecb = Ec[ui % 2]
                                ui += 1
                                for dl in range(5):
                                    ks = slice((kb + dl) * 128, (kb + dl + 1) * 128)
                                    MM(pn[:, dl * 128:(dl + 1) * 128], nkT[lo:lo + 64, hp, ks], nqT[lo:lo + 64, hp, qs],
                                       [nkT, nqT], [pn])
                                ACT(En[:], pn[:, 0:640], AF.Exp, [pn], [En], scale=0.125)
                                TT(Pn[:], En[:], EB[pat][:, h * 640:(h + 1) * 640], ALU.mult, [En, EB[pat]], [Pn])
                                for dl in range(5):
                                    MM(po_na[:, h, :], Pn[:, dl * 128:(dl + 1) * 128], nv[:, kb + dl, h, :], [Pn, nv], [po_na],
                                       start=(dl == 0), stop=(dl == 4))
                                for i in range(2):
                                    MM(ps_c[:, h % 2, i * 128:(i + 1) * 128], kmT[lo:lo + 64, hp, i * 128:(i + 1) * 128],
                                       cqT[lo:lo + 64, hp, qs], [kmT, cqT], [ps_c])
                                ACT(Ecb[:], ps_c[:, h % 2, :], AF.Exp, [ps_c], [Ecb], scale=0.125)
                                for i in range(2):
                                    MM(po_c[:, h, :], Ecb[:, i * 128:(i + 1) * 128], vm[:, i, h, :], [Ecb, vm], [po_c],
                                       start=(i == 0), stop=(i == 1))
                            RCP(rb[:, 0:4], po_na[:, :, 64], [po_na], [rb])
                            TT(yb[:, 0:256].rearrange("p (h d) -> p h d", d=64), po_na[:, :, 0:64],
                               rb[:, 0:4].unsqueeze(2).to_broadcast([128, 4, 64]), ALU.mult, [po_na, rb], [yb])
                            RCP(rb[:, 4:8], po_c[:, :, 64], [po_c], [rb])
                            TT(yb[:, 256:512].rearrange("p (h d) -> p h d", d=64), po_c[:, :, 0:64],
                               rb[:, 4:8].unsqueeze(2).to_broadcast([128, 4, 64]), ALU.mult, [po_c, rb], [yb])
                            for k in range(4):
                                TR(pT3[:, k, :], yb[:, k * 128:(k + 1) * 128], ident_b[:], [yb, ident_b], [pT3])
                            ACT(yT_nc[:, :, qs], pT3[:], AF.Copy, [pT3], [yT_nc])
                    fw.barrier()

                with fw.scope():
                    yT_m = fw.sbuf("yT_m", [128, 4, S], BF16)
                    with fw.scope():
                        qkT = fw.sbuf("qkT", [128, 4, S], BF16)
                        with fw.scope():
                            convw = fw.sbuf("convw", [128, 4, 5], F32)
                            raw = [fw.sbuf("raw%d" % i, [128, 516], F32) for i in range(2)]
                            cacc = [fw.sbuf("cacc%d" % i, [128, 512], F32) for i in range(2)]
                            fw.dma("sp", convw[:], convw_d[l], [convw_d], [convw], convw)
                            ui = 0
                            for st in range(8):
                                for c in range(4):
                                    r = raw[ui % 2]
                                    a = cacc[ui % 2]
                                    ui += 1
                                    lo_t = st * 512 - 2
                                    hi_t = st * 512 + 514
                                    d0, d1 = 0, 516
                                    if st == 0:
                                        MS(r[:, 0:2], 0.0, [r])
                                        lo_t, d0 = 0, 2
                                    if st == 7:
                                        MS(r[:, 514:516], 0.0, [r])
                                        hi_t, d1 = S, 514
                                    fw.dma("sp", r[:, d0:d1], qkraw_d[c * 128:(c + 1) * 128, lo_t:hi_t], [qkraw_d], [r], r)
                                    TS(a[:], r[:, 0:512], convw[:, c, 0:1], None, ALU.mult, None, [r, convw], [a])
                                    for jj in range(1, 5):
                                        STT(a[:], r[:, jj:jj + 512], convw[:, c, jj:jj + 1], a[:], ALU.mult, ALU.add,
                                            [r, convw, a], [a])
                                    ACT(qkT[:, c, st * 512:(st + 1) * 512], a[:], AF.Silu, [a], [qkT])
                        fw.barrier()

                        if debug and l == 0:
                            fw.dma("sp", dbg_qkT.ap, qkT[:], [qkT], [dbg_qkT], qkT)
                            fw.dma("sp", dbg_gates.ap, gates[:], [gates], [dbg_gates], gates)
                            fw.barrier()
                        with fw.scope():
                            ghead_b = fw.sbuf("ghead_b", [128, 512], F32)
                            fw.dma("sp", ghead_b[:], g_head[l:l + 1, :].partition_broadcast(128), [g_head], [ghead_b], ghead_b)
                            LI = [fw.sbuf("LI%d" % d, [128, 128], F32) for d in range(2)]
                            LF = [fw.sbuf("LF%d" % d, [128, 128], F32) for d in range(2)]
                            Bc = [fw.sbuf("Bc%d" % d, [128, 128], F32) for d in range(2)]
                            Gc = [fw.sbuf("Gc%d" % d, [128, 128], F32) for d in range(2)]
                            Es = [fw.sbuf("Es%d" % d, [128, 128], F32) for d in range(2)]
                            Ws = [fw.sbuf("Ws%d" % d, [128, 128], F32) for d in range(2)]
                            EM = [fw.sbuf("EM%d" % d, [128, 128], F32) for d in range(2)]
                            EG = [fw.sbuf("EG%d" % d, [128, 128], F32) for d in range(2)]
                            gtmp = fw.sbuf("gtmp", [128, 128], F32)
                            pg = fw.psum("pg", [128, 2, 128], F32)
                            v3 = lambda o: o[:].rearrange("p (c h) -> p c h", h=4)
                            for d in range(2):
                                CP(v3(LI[d]), gates[:, :, 8 * d:8 * d + 4], [gates], [LI[d]])
                                ACT(v3(gtmp), gates[:, :, 8 * d + 4:8 * d + 8], AF.Exp, [gates], [gtmp], scale=-1.0)
                                ACT(gtmp[:], gtmp[:], AF.Ln, [gtmp], [gtmp], bias=1.0)
                                TS(LF[d][:], gtmp[:], -1.0, None, ALU.mult, None, [gtmp], [LF[d]])
                                MM(pg[:, 0, :], (triU if d == 0 else triL)[:], LF[d][:], [triU, triL, LF[d]], [pg])
                                MM(pg[:, 1, :], ones_f[:], LF[d][:], [ones_f, LF[d]], [pg])
                                CP(Bc[d][:], pg[:, 0, :], [pg], [Bc[d]])
                                CP(Gc[d][:], pg[:, 1, :], [pg], [Gc[d]])
                                TT(gtmp[:], LI[d][:], Bc[d][:], ALU.subtract, [LI[d], Bc[d]], [gtmp])
                                ACT(Es[d][:], gtmp[:], AF.Exp, [gtmp], [Es[d]])
                                TT(gtmp[:], gtmp[:], Gc[d][:], ALU.add, [gtmp, Gc[d]], [gtmp])
                                ACT(Ws[d][:], gtmp[:], AF.Exp, [gtmp], [Ws[d]])
                                ACT(EM[d][:], Bc[d][:], AF.Exp, [Bc[d]], [EM[d]], scale=-1.0, bias=LN8)
                                ACT(EG[d][:], Gc[d][:], AF.Exp, [Gc[d]], [EG[d]])
                            maskd = [triU, triL]

                            Cprev = [fw.sbuf("Cprev%d" % d, [128, NT, 129], BF16) for d in range(2)]
                            Cst = [fw.sbuf("Cst%d" % d, [128, 129], F32) for d in range(2)]
                            EGs = [fw.sbuf("EGs%d" % d, [128, NT], F32) for d in range(2)]
                            kwA = [fw.sbuf("kwA%d" % i, [128, 128], BF16) for i in range(4)]
                            kwB = [fw.sbuf("kwB%d" % i, [128, 128], BF16) for i in range(4)]
                            v1 = [fw.sbuf("v1_%d" % i, [128, 2, 129], BF16) for i in range(4)]
                            sob = [fw.sbuf("sob%d" % i, [128, 256], BF16) for i in range(2)]
                            PTm = [fw.sbuf("PTm%d" % i, [128, 2, 2, 128], BF16) for i in range(2)]
                            dn = [fw.sbuf("dn%d" % i, [128, 16], F32) for i in range(2)]
                            hs = [fw.sbuf("hs%d" % i, [128, 2, 128], F32) for i in range(2)]
                            hj = fw.sbuf("hj", [128, 128], F32)
                            ymb = [fw.sbuf("ymb%d" % i, [128, 256], BF16) for i in range(2)]
                            pTk_t = fw.psum("pTk", [128, 2, 128], BF16)
                            pTk = [Obj("pTk%d" % i, pTk_t[:, i, :]) for i in range(2)]
                            pkvm_t = fw.psum("pkvm", [128, 2, 129], F32)
                            pkvm = [Obj("pkvm%d" % i, pkvm_t[:, i, :]) for i in range(2)]
                            psm = fw.psum("psm", [128, 2, 512], F32)
                            pnd = fw.psum("pnd", [128, 2, 2, 256], F32)
                            pTy = fw.psum("pTy", [128, 2, 128], BF16)
                            for i in range(4):
                                MS(kwA[i][:, 64:128], 0.0, [kwA[i]])
                                MS(kwB[i][:, 0:64], 0.0, [kwB[i]])
                                MS(v1[i][:, :, 128:129], 1.0, [v1[i]])
                            for hp in range(2):
                                h0 = 2 * hp
                                for d in range(2):
                                    MS(Cst[d][:], 0.0, [Cst[d]])
                                    CP(EGs[d][0:64, :], v3(EG[d])[0:64, :, h0], [EG[d]], [EGs[d]])
                                    CP(EGs[d][64:128, :], v3(EG[d])[64:128, :, h0 + 1], [EG[d]], [EGs[d]])
                                ui = 0
                                for ci in range(NT):
                                    for d in range(2):
                                        c = ci if d == 0 else NT - 1 - ci
                                        cs = slice(c * 128, (c + 1) * 128)
                                        ka, kb_, vv, pk, pt = kwA[ui % 4], kwB[ui % 4], v1[ui % 4], pkvm[ui % 2], pTk[ui % 2]
                                        ui += 1
                                        ACT(Cprev[d][:, c, :], Cst[d][:], AF.Copy, [Cst[d]], [Cprev[d]])
                                        fw.dma("sp", vv[:, :, 0:128], v_d[cs, h0 * 128:(h0 + 2) * 128].rearrange("p (h d) -> p h d", d=128),
                                               [v_d], [vv], vv)
                                        TR(pt[:], qkT[:, 2 + hp, cs], ident_b[:], [qkT, ident_b], [pt])
                                        TS(ka[:, 0:64], pt[:, 0:64], Ws[d][:, c * 4 + h0:c * 4 + h0 + 1], None, ALU.mult, None,
                                           [pt, Ws[d]], [ka])
                                        TS(kb_[:, 64:128], pt[:, 64:128], Ws[d][:, c * 4 + h0 + 1:c * 4 + h0 + 2], None, ALU.mult, None,
                                           [pt, Ws[d]], [kb_])
                                        MM(pk[:], ka[:], vv[:, 0, :], [ka, vv], [pk], start=True, stop=False)
                                        MM(pk[:], kb_[:], vv[:, 1, :], [kb_, vv], [pk], start=False, stop=True)
                                        STT(Cst[d][:], Cst[d][:], EGs[d][:, c:c + 1], pk[:], ALU.mult, ALU.add,
                                            [Cst[d], EGs[d], pk], [Cst[d]])
                                for c in range(NT):
                                    cs = slice(c * 128, (c + 1) * 128)
                                    vv = v1[c % 4]
                                    sb = sob[c % 2]
                                    Pm = PTm[c % 2]
                                    dnb = dn[c % 2]
                                    hsb = hs[c % 2]
                                    yb = ymb[c % 2]
                                    fw.dma("sp", vv[:, :, 0:128], v_d[cs, h0 * 128:(h0 + 2) * 128].rearrange("p (h d) -> p h d", d=128),
                                           [v_d], [vv], vv)
                                    fw.dma("sp", sb[:], so_d[cs, h0 * 128:(h0 + 2) * 128], [so_d], [sb], sb)
                                    for i in range(2):
                                        lo = i * 64
                                        MM(psm[:, i, 0:128], qkT[lo:lo + 64, 2 + hp, cs], qkT[lo:lo + 64, hp, cs], [qkT], [psm])
                                    for d in range(2):
                                        for i in range(2):
                                            col = c * 4 + h0 + i
                                            STT(Pm[:, d, i, :], psm[:, i, 0:128], Es[d][:, col:col + 1], maskd[d][:], ALU.mult, ALU.mult,
                                                [psm, Es[d], maskd[d]], [Pm])
                                    for d in range(2):
                                        for i in range(2):
                                            lo = i * 64
                                            MM(pnd[:, d, i, 0:129], Pm[:, d, i, :], vv[:, i, :], [Pm, vv], [pnd], start=True, stop=False)
                                            MM(pnd[:, d, i, 0:129], qkT[lo:lo + 64, hp, cs], Cprev[d][lo:lo + 64, c, :],
                                               [qkT, Cprev[d]], [pnd], start=False, stop=True)
                                    ACT(dnb[:, 0:4].rearrange("p (d i) -> p d i", i=2), pnd[:, :, :, 128], AF.Abs, [pnd], [dnb])
                                    for d in range(2):
                                        col = c * 4 + h0
                                        TT(dnb[:, 4 + 2 * d:6 + 2 * d], dnb[:, 2 * d:2 * d + 2], EM[d][:, col:col + 2], ALU.max,
                                           [dnb, EM[d]], [dnb])
                                    RCP(dnb[:, 8:12], dnb[:, 4:8], [dnb], [dnb])
                                    for i in range(2):
                                        TS(hsb[:, i, :], pnd[:, 0, i, 0:128], dnb[:, 8 + i:9 + i], None, ALU.mult, None, [pnd, dnb], [hsb])
                                        STT(hsb[:, i, :], pnd[:, 1, i, 0:128], dnb[:, 10 + i:11 + i], hsb[:, i, :], ALU.mult, ALU.add,
                                            [pnd, dnb, hsb], [hsb])
                                        MS(dnb[:, 12 + i:13 + i], 0.0, [dnb])
                                        ACT(hj[:], hsb[:, i, :], AF.Square, [hsb], [hj, dnb], accum_out=dnb[:, 12 + i:13 + i])
                                    rstd_from_ss(dnb, dnb[:, 12:14], dnb[:, 14:16], dnb[:, 14:16], 1.0 / 128)
                                    for i in range(2):
                                        STT(hsb[:, i, :], hsb[:, i, :], dnb[:, 14 + i:15 + i], ghead_b[:, (h0 + i) * 128:(h0 + i + 1) * 128],
                                            ALU.mult, ALU.mult, [hsb, dnb, ghead_b], [hsb])
                                    TT(yb[:], hsb[:].rearrange("p i d -> p (i d)"), sb[:], ALU.mult, [hsb, sb], [yb])
                                    for i in range(2):
                                        TR(pTy[:, i, :], yb[:, i * 128:(i + 1) * 128], ident_b[:], [yb, ident_b], [pTy])
                                    ACT(yT_m[:, h0:h0 + 2, cs], pTy[:], AF.Copy, [pTy], [yT_m])
                        fw.barrier()

                    with fw.scope():
                        wo = fw.sbuf("wo", [128, 8, D], BF16)
                        wr = fw.sbuf("wr", [128, 8, NE], BF16)
                        gffn_b = fw.sbuf("gffn_b", [128, D], F32)
                        xt5 = [fw.sbuf("xt5_%d" % i, [128, D], F32) for i in range(2)]
                        xo = [fw.sbuf("xo%d" % i, [128, D], F32) for i in range(2)]
                        xj5 = fw.sbuf("xj5", [128, D], F32)
                        hf = [fw.sbuf("hf%d" % i, [128, D], BF16) for i in range(2)]
                        hfT = [fw.sbuf("hfT%d" % i, [128, 8, 128], BF16) for i in range(2)]
                        st5 = [fw.sbuf("st5_%d" % i, [128, 8], F32) for i in range(2)]
                        ex5 = [fw.sbuf("ex5_%d" % i, [128, NE], F32) for i in range(2)]
                        po5 = [fw.psum("po5_%d" % i, [128, D], F32) for i in range(2)]
                        pT5 = [fw.psum("pT5_%d" % i, [128, 8, 128], BF16) for i in range(2)]
                        plog = fw.psum("plog", [128, NE], F32)
                        fw.dma("pool", wo[:], w_out[l].rearrange("(k p) n -> p k n", p=128), [w_out], [wo], wo)
                        if debug and l == 0:
                            fw.dma("sp", dbg_yTm.ap, yT_m[:], [yT_m], [dbg_yTm], yT_m)
                            fw.dma("sp", dbg_yTnc.ap, yT_nc[:], [yT_nc], [dbg_yTnc], yT_nc)
                        fw.dma("pool", wr[:], w_rt[l].rearrange("(k p) n -> p k n", p=128), [w_rt], [wr], wr)
                        fw.dma("sp", gffn_b[:], g_ffn[l:l + 1, :].partition_broadcast(128), [g_ffn], [gffn_b], gffn_b)
                        for t in range(NT):
                            b = t % 2
                            ts_ = slice(t * 128, (t + 1) * 128)
                            fw.dma("sp", xt5[b][:], x_src[ts_, :], [out_tiles[t]], [xt5[b]], xt5[b])
                            for n in range(2):
                                for k in range(8):
                                    src = yT_m if k < 4 else yT_nc
                                    MM(po5[b][:, n * 512:(n + 1) * 512], src[:, k % 4, ts_], wo[:, k, n * 512:(n + 1) * 512],
                                       [src, wo], [po5[b]], start=(k == 0), stop=(k == 7))
                            TT(xo[b][:], po5[b][:], xt5[b][:], ALU.add, [po5[b], xt5[b]], [xo[b]])
                            fw.dma("sp", out_d[ts_, :], xo[b][:], [xo[b]], [out_tiles[t]], xo[b])
                            if debug and l == 0:
                                fw.dma("sp", dbg_xmix[ts_, :], xo[b][:], [xo[b]], [dbg_xmix], xo[b])
                            MS(st5[b][:, 0:1], 0.0, [st5[b]])
                            ACT(xj5[:], xo[b][:], AF.Square, [xo[b]], [xj5, st5[b]], accum_out=st5[b][:, 0:1])
                            rstd_from_ss(st5[b], st5[b][:, 0:1], st5[b][:, 1:2], st5[b][:, 2:3], 1.0 / D)
                            STT(hf[b][:], xo[b][:], st5[b][:, 2:3], gffn_b[:], ALU.mult, ALU.mult, [xo[b], st5[b], gffn_b], [hf[b]])
                            fw.dma("sp", hf_d[ts_, :], hf[b][:], [hf[b]], [hf_d], hf[b])
                            for k in range(8):
                                TR(pT5[b][:, k, :], hf[b][:, k * 128:(k + 1) * 128], ident_b[:], [hf[b], ident_b], [pT5[b]])
                            ACT(hfT[b][:], pT5[b][:], AF.Copy, [pT5[b]], [hfT[b]])
                            for k in range(8):
                                MM(plog[:], hfT[b][:, k, :], wr[:, k, :], [hfT[b], wr], [plog], start=(k == 0), stop=(k == 7))
                            MS(st5[b][:, 5:6], 0.0, [st5[b]])
                            ACT(ex5[b][:], plog[:], AF.Exp, [plog, st5[b]], [ex5[b], st5[b]], accum_out=st5[b][:, 5:6])
                            RCP(st5[b][:, 6:7], st5[b][:, 5:6], [st5[b]], [st5[b]])
                            TS(aff_all[:, t, :], ex5[b][:], st5[b][:, 6:7], None, ALU.mult, None, [ex5[b], st5[b]], [aff_all])
                        fw.dma("sp", aff_d.ap.rearrange("(i p) e -> p i e", p=128), aff_all[:], [aff_all], [aff_d], aff_all)
                    fw.barrier()

            with fw.scope():
                affT = fw.sbuf("affT", [16, S], F32)
                rjunk = fw.sbuf("rjunk", [16, S], F32)
                rmask = fw.sbuf("rmask", [16, S], F32)
                rones = fw.sbuf("rones", [16, S], F32)
                rs = fw.sbuf("rs", [16, 8], F32)
                pTa = [fw.psum("pTa%d" % i, [16, 512], F32) for i in range(2)]
                pcn = fw.psum("pcn", [128, NT, NE], F32)
                for g4 in range(8):
                    pa = pTa[g4 % 2]
                    for i in range(4):
                        t = g4 * 4 + i
                        TR(pa[:, i * 128:(i + 1) * 128], aff_all[:, t, :], ident_f[:], [aff_all, ident_f], [pa])
                    CP(affT[:, g4 * 512:(g4 + 1) * 512], pa[:], [pa], [affT])
                MS(rones[:], 1.0, [rones])
                MS(rs[:, 0:1], 0.0, [rs])
                MS(rs[:, 1:2], 1.0, [rs])
                for it in range(28):
                    TT(rs[:, 2:3], rs[:, 0:1], rs[:, 1:2], ALU.add, [rs], [rs])
                    TS(rs[:, 2:3], rs[:, 2:3], 0.5, None, ALU.mult, None, [rs], [rs])
                    TS(rjunk[:], affT[:], rs[:, 2:3], 0.0, ALU.is_ge, ALU.add, [affT, rs], [rjunk, rs], accum_out=rs[:, 3:4])
                    TS(rs[:, 4:5], rs[:, 3:4], float(CAP), None, ALU.is_ge, None, [rs], [rs])
                    TT(rs[:, 5:6], rs[:, 2:3], rs[:, 0:1], ALU.subtract, [rs], [rs])
                    STT(rs[:, 0:1], rs[:, 5:6], rs[:, 4:5], rs[:, 0:1], ALU.mult, ALU.add, [rs], [rs])
                    TT(rs[:, 5:6], rs[:, 1:2], rs[:, 2:3], ALU.subtract, [rs], [rs])
                    STT(rs[:, 1:2], rs[:, 5:6], rs[:, 4:5], rs[:, 2:3], ALU.mult, ALU.add, [rs], [rs])
                TS(rmask[:], affT[:], rs[:, 0:1], None, ALU.is_ge, None, [affT, rs], [rmask])
                op("dve", lambda e: e.tensor_tensor_scan(out=rjunk[:], data0=rones[:], data1=rmask[:], initial=0.0,
                                                         op0=ALU.mult, op1=ALU.add), [rones, rmask], [rjunk])
                for t in range(NT):
                    TR(pcn[:, t, :], rjunk[:, t * 128:(t + 1) * 128], ident_f[0:16, 0:16], [rjunk, ident_f], [pcn])
                CP(cnt_all[:], pcn[:], [pcn], [cnt_all])
            fw.barrier()

            with fw.scope():
                NB13 = 3
                w1c = [fw.sbuf("w1c%d" % i, [128, 8, 512], BF16) for i in range(NB13)]
                w3c = [fw.sbuf("w3c%d" % i, [128, 8, 512], BF16) for i in range(NB13)]
                w2c = [fw.sbuf("w2c%d" % i, [128, 4, D], BF16) for i in range(4)]
                xes = [fw.sbuf("xes%d" % i, [128, D], BF16) for i in range(4)]
                gts = [fw.sbuf("gts%d" % i, [128, NE], F32) for i in range(4)]
                xeT = [fw.sbuf("xeT%d" % i, [128, 8, 512], BF16) for i in range(2)]
                hidT = fw.sbuf("hidT", [128, 16, 512], BF16)
                stmp = [fw.sbuf("stmp%d" % i, [128, 512], F32) for i in range(2)]
                ye = [fw.sbuf("ye%d" % i, [128, D], F32) for i in range(2)]
                idxf = fw.sbuf("idxf", [128, NE * 4], I32)
                cacc7 = [fw.sbuf("cacc7_%d" % i, [128, 512], BF16) for i in range(2)]
                pT7 = fw.psum("pT7", [128, 8, 128], BF16)
                ph = [fw.psum("ph%d" % i, [128, 512], F32) for i in range(4)]
                py = [fw.psum("py%d" % i, [128, 512], F32) for i in range(2)]
                pidx = fw.psum("pidx", [128, NE * 4], F32)
                idx_objs = [Obj("idx%d" % e, None) for e in range(NE)]
                idxg = [fw.sbuf("idxg%d" % i, [128, 1], I32) for i in range(4)]
                idxs = [fw.sbuf("idxs%d" % i, [128, 1], I32) for i in range(4)]
                w13_i = [0]

                def load_w13(e, c):
                    bi = w13_i[0] % NB13
                    w13_i[0] += 1
                    fw.dma("pool", w1c[bi][:], w1[l, e, :, c * 512:(c + 1) * 512].rearrange("(k p) f -> p k f", p=128),
                           [w1], [w1c[bi]], w1c[bi])
                    fw.dma("pool", w3c[bi][:], w3[l, e, :, c * 512:(c + 1) * 512].rearrange("(k p) f -> p k f", p=128),
                           [w3], [w3c[bi]], w3c[bi])
                    return bi

                def load_w2(e):
                    for c in range(4):
                        fw.dma("pool", w2c[c][:], w2[l, e, c * 512:(c + 1) * 512, :].rearrange("(c p) d -> p c d", p=128),
                               [w2], [w2c[c]], w2c[c])

                def build_idx_gen(e):
                    a = cacc7[e % 2]
                    MS(a[:], 0.0, [a])
                    for t in range(NT):
                        STT(a[:], iota512[:], cnt_all[:, t, e:e + 1], a[:], ALU.is_ge, ALU.add, [iota512, cnt_all, a], [a])
                        if t % 2 == 1:
                            yield
                    for g in range(4):
                        MM(pidx[:, e * 4 + g:e * 4 + g + 1], a[:, g * 128:(g + 1) * 128], ones_b[:, 0:1], [a, ones_b], [pidx])
                    CP(idxf[:, e * 4:(e + 1) * 4], pidx[:, e * 4:(e + 1) * 4], [pidx], [idx_objs[e]])

                def gather(e):
                    for g in range(4):
                        col = e * 4 + g
                        CP(idxg[g][:, 0:1], idxf[:, col:col + 1], [idx_objs[e]], [idxg[g]])
                        op("pool", lambda en, g=g: en.indirect_dma_start(
                            out=xes[g][:], out_offset=None, in_=hf_d.ap,
                            in_offset=bass.IndirectOffsetOnAxis(ap=idxg[g][:, 0:1], axis=0),
                            bounds_check=regs["bc"], oob_is_err=False), [hf_d, idxg[g]], [xes[g]], dma=True, sem_obj=xes[g])
                        op("pool", lambda en, g=g: en.indirect_dma_start(
                            out=gts[g][:], out_offset=None, in_=aff_d.ap,
                            in_offset=bass.IndirectOffsetOnAxis(ap=idxg[g][:, 0:1], axis=0),
                            bounds_check=regs["bc"], oob_is_err=False), [aff_d, idxg[g]], [gts[g]], dma=True, sem_obj=gts[g])

                def build_idx(e):
                    for _ in build_idx_gen(e):
                        pass

                build_idx(0)
                pending = {}
                for c in range(3):
                    pending[(0, c)] = load_w13(0, c)
                for g in range(4):
                    MS(xes[g][:], 0.0, [xes[g]])
                    MS(gts[g][:], 0.0, [gts[g]])
                gather(0)
                load_w2(0)
                hi_ = 0
                for e in range(NE):
                    xT = xeT[e % 2]
                    gen = build_idx_gen(e + 1) if e + 1 < NE else iter(())
                    gate_cols = []
                    for g in range(4):
                        for k in range(8):
                            TR(pT7[:, k, :], xes[g][:, k * 128:(k + 1) * 128], ident_b[:], [xes[g], ident_b], [pT7])
                        ACT(xT[:, :, g * 128:(g + 1) * 128], pT7[:], AF.Copy, [pT7], [xT])
                    for c in range(4):
                        bi = pending.pop((e, c))
                        for fcl in range(4):
                            fc = c * 4 + fcl
                            p1 = ph[hi_ % 4]; hi_ += 1
                            p3 = ph[hi_ % 4]; hi_ += 1
                            for k in range(8):
                                MM(p1[:], w1c[bi][:, k, fcl * 128:(fcl + 1) * 128], xT[:, k, :], [w1c[bi], xT], [p1],
                                   start=(k == 0), stop=(k == 7))
                            for k in range(8):
                                MM(p3[:], w3c[bi][:, k, fcl * 128:(fcl + 1) * 128], xT[:, k, :], [w3c[bi], xT], [p3],
                                   start=(k == 0), stop=(k == 7))
                            sb = stmp[fc % 2]
                            ACT(sb[:], p1[:], AF.Silu, [p1], [sb])
                            TT(hidT[:, fc, :], sb[:], p3[:], ALU.mult, [sb, p3], [hidT])
                            next(gen, None)
                        if c == 0:
                            pending[(e, 3)] = load_w13(e, 3)
                        elif e + 1 < NE:
                            pending[(e + 1, c - 1)] = load_w13(e + 1, c - 1)
                    for _ in gen:
                        pass
                    for g in range(4):
                        yb = ye[g % 2]
                        for n in range(2):
                            pyb = py[(g * 2 + n) % 2]
                            for fc in range(16):
                                MM(pyb[:], hidT[:, fc, g * 128:(g + 1) * 128], w2c[fc // 4][:, fc % 4, n * 512:(n + 1) * 512],
                                   [hidT, w2c[fc // 4]], [pyb], start=(fc == 0), stop=(fc == 15))
                            ACT(yb[:, n * 512:(n + 1) * 512], pyb[:], AF.Copy, [pyb, gts[g]], [yb], scale=gts[g][:, e:e + 1])
                        col = e * 4 + g
                        CP(idxs[g][:, 0:1], idxf[:, col:col + 1], [idx_objs[e]], [idxs[g]])
                        op("pool", lambda en, yb=yb, g=g: en.indirect_dma_start(
                            out=out_d.ap, out_offset=bass.IndirectOffsetOnAxis(ap=idxs[g][:, 0:1], axis=0),
                            in_=yb[:], in_offset=None, bounds_check=regs["bc"], oob_is_err=False, compute_op=ALU.add),
                            [yb, idxs[g]], out_tiles, dma=True, sem_obj=yb)
                    if e + 1 < NE:
                        gather(e + 1)
                        load_w2(e + 1)
                if debug and l == 0:
                    fw.dma("sp", dbg_idx.ap, idxf[:], idx_objs, [dbg_idx], idxf)
            fw.barrier()


def _natab(rpb):
    L = rpb.shape[0]
    pad = np.concatenate([rpb.reshape(L, 4, 15 * 31), np.full((L, 4, 1), -30000.0, np.float32)], axis=2)
    idx = np.zeros((5, 128, 5, 128), np.int64)
    k = np.arange(128)[:, None, None]
    dl = np.arange(5)[None, :, None]
    q = np.arange(128)[None, None, :]
    for p, j in enumerate([0, 1, 2, 30, 31]):
        kb = min(max(j - 2, 0), 27)
        key = (kb + dl) * 128 + k
        qq = j * 128 + q
        rk, ck = key // 64, key % 64
        rq, cq = qq // 64, qq % 64
        rs = np.clip(rq - 4, 0, 56)
        cs = np.clip(cq - 8, 0, 48)
        ok = (rk >= rs) & (rk < rs + 8) & (ck >= cs) & (ck < cs + 16)
        ii = (rk - rq + 7) * 31 + (ck - cq + 15)
        idx[p] = np.where(ok, ii, 465)
    tab = pad[:, :, idx]
    tab = np.ascontiguousarray(tab.transpose(0, 2, 3, 1, 4, 5)).reshape(L, 5, 128, 2560)
    return tab.astype(np.float32)


def _layer_inputs(inp, ls):
    f = lambda a: np.ascontiguousarray(a, dtype=np.float32)
    L = len(ls)
    sel = lambda k: f(np.asarray(inp[k])[ls])
    convw = sel("conv_qk").transpose(0, 2, 1).reshape(L, 4, 128, 5).transpose(0, 2, 1, 3)
    gc = np.stack([np.tile(sel(k), (1, 2)) for k in ("na_gq", "na_gk", "mem_gq", "mem_gk")], axis=2)
    return {
        "g_mix": sel("g_mix"), "w_in": sel("w_in"), "b_gates": sel("b_gates"), "convw": f(convw),
        "g_head": sel("g_mlstm_head"), "gcols": f(gc), "natab": _natab(sel("na_rpb")),
        "g_mem": sel("g_mem"), "w_mem_kv": sel("w_mem_kv"), "w_out": sel("w_out"), "g_ffn": sel("g_ffn"),
        "w_router": sel("w_router"), "w1": sel("w1"), "w3": sel("w3"), "w2": sel("w2"),
    }


_CACHE = {}
N_FUSED_LAYERS = 4


def kernel(**inputs):
    x = np.ascontiguousarray(inputs["x"], dtype=np.float32)
    mem = np.ascontiguousarray(inputs["mem"], dtype=np.float32)
    depth = np.asarray(inputs["g_mix"]).shape[0]
    nl = N_FUSED_LAYERS
    if nl not in _CACHE:
        _CACHE[nl] = build(nl)[0]
    nc = _CACHE[nl]
    cur = x
    for l0 in range(0, depth, nl):
        shared = _layer_inputs(inputs, list(range(l0, l0 + nl)))
        in_maps = []
        for c in range(8):
            m = dict(shared)
            m["x"] = cur[c]
            m["mem"] = mem[c]
            in_maps.append(m)
        res = run_bass_kernel_spmd(nc, in_maps, core_ids=list(range(8)))
        cur = np.stack([np.asarray(r["out"]) for r in res.results], axis=0).astype(np.float32)
    return cur
```

```python
import contextlib
import numpy as np
import concourse.bass as bass
import concourse.mybir as mybir
from concourse.bass_utils import run_bass_kernel_spmd

F32 = mybir.dt.float32
BF16 = mybir.dt.bfloat16
I32 = mybir.dt.int32
ALU = mybir.AluOpType
AF = mybir.ActivationFunctionType
AX = mybir.AxisListType

ENGS = ("pe", "act", "dve", "pool", "sp")

S = 4096
D = 1024
NT = 32
DIN = 2576
NE = 16
CAP = 512
DFF = 2048
EPS = 1e-6
LN8 = float(np.log(8.0))


class Obj:
    __slots__ = ("name", "ap", "last_write", "readers")

    def __init__(self, name, ap):
        self.name = name
        self.ap = ap
        self.last_write = None
        self.readers = []

    def __getitem__(self, k):
        return self.ap[k]


class Op:
    __slots__ = ("eng", "fn", "deps", "is_dma", "sem", "sobj", "value", "signal", "waits", "epoch", "release")

    def __init__(self, eng, fn, deps, is_dma):
        self.eng = eng
        self.fn = fn
        self.deps = deps
        self.is_dma = is_dma
        self.sem = None
        self.sobj = None
        self.value = None
        self.signal = False
        self.waits = []
        self.epoch = 0
        self.release = False


class StopBuild(Exception):
    pass


_dram_objs = []
_dram_inputs = set()


class FW:
    def __init__(self, nc):
        self.nc = nc
        self.stack = contextlib.ExitStack()
        self.scopes = [self.stack]
        self.ops = []
        self.eng_ops = {e: [] for e in ENGS}
        self.last_eng_op = {e: None for e in ENGS}
        self.dma_since_barrier = []
        self.uid = 0

    @contextlib.contextmanager
    def scope(self):
        st = contextlib.ExitStack()
        self.scopes.append(st)
        try:
            yield
        finally:
            self.scopes.pop()
            st.close()

    def _nm(self, name):
        self.uid += 1
        return "%s_%d" % (name, self.uid)

    def sbuf(self, name, shape, dtype):
        t = self.scopes[-1].enter_context(self.nc.sbuf_tensor(self._nm(name), list(shape), dtype))
        return Obj(name, t)

    def psum(self, name, shape, dtype=F32):
        t = self.scopes[-1].enter_context(self.nc.psum_tensor(self._nm(name), list(shape), dtype))
        return Obj(name, t)

    def dram(self, name, shape, dtype, kind="Internal"):
        t = self.nc.dram_tensor(name, list(shape), dtype, kind=kind)
        o = Obj(name, t.ap())
        if kind != "Internal":
            _dram_objs.append(o)
            if kind == "ExternalInput":
                _dram_inputs.add(name)
        return o

    def op(self, eng, fn, reads=(), writes=(), dma=False, sem_obj=None, extra_deps=()):
        deps = list(extra_deps)
        for o in reads:
            if o.last_write is not None:
                deps.append(o.last_write)
        for o in writes:
            if o.last_write is not None:
                deps.append(o.last_write)
            deps.extend(o.readers)
        op = Op(eng, fn, deps, dma)
        op.epoch = getattr(self, "epoch", 0)
        if dma:
            assert sem_obj is not None
            op.sobj = sem_obj
            self.dma_since_barrier.append(op)
        for o in writes:
            o.last_write = op
            o.readers = []
        for o in reads:
            o.readers.append(op)
        self.ops.append(op)
        self.eng_ops[eng].append(op)
        self.last_eng_op[eng] = op
        lim = getattr(self, "op_limit", 0)
        if lim and len(self.ops) == lim:
            self.op_limit = 0
            raise StopBuild()
        return op

    def dma(self, eng, out_ap, in_ap, reads, writes, sem_obj, **kw):
        return self.op(eng, lambda e: e.dma_start(out=out_ap, in_=in_ap, **kw), reads=reads, writes=writes,
                       dma=True, sem_obj=sem_obj)

    def barrier(self):
        self.nbar = getattr(self, "nbar", 0) + 1
        self._barrier()
        if self.nbar == getattr(self, "stop_at", -1):
            raise StopBuild()

    def _barrier(self):
        tails = [o for o in self.last_eng_op.values() if o is not None]
        seen = {}
        for o in self.dma_since_barrier:
            seen[id(o.sobj)] = o
        deps = tails + list(seen.values())
        self.dma_since_barrier = []
        last = None
        for e in ENGS:
            last = self.op(e, lambda en: en.nop(nofuse=True), extra_deps=deps)
        last.release = True
        self.epoch = getattr(self, "epoch", 0) + 1
        self.last_eng_op = {e: None for e in ENGS}

    def emit(self):
        nc = self.nc
        for op in self.ops:
            for d in op.deps:
                if d.eng == "pe" and op.eng == "pe" and not d.is_dma:
                    continue
                d.signal = True
        eng_sem = {}
        for e in ENGS:
            eng_sem[e] = self.stack.enter_context(nc.semaphore("sem_" + e))
        phys = []
        free = []
        cur_map = {}
        cur_epoch = 0
        eng_cnt = {e: 0 for e in ENGS}
        known = {e: {} for e in ENGS}
        for op in self.ops:
            w = {}
            for d in op.deps:
                if d.eng == "pe" and op.eng == "pe" and not d.is_dma:
                    continue
                if d.is_dma:
                    if d.epoch < cur_epoch:
                        continue
                    rec = phys[cur_map[id(d.sobj)]]
                    s, v = rec[0], rec[1]
                else:
                    s, v = eng_sem[d.eng], d.value
                key = id(s)
                if known[op.eng].get(key, 0) >= v:
                    continue
                if key not in w or w[key][1] < v:
                    w[key] = (s, v)
            for key, (s, v) in w.items():
                known[op.eng][key] = v
            op.waits = list(w.values())
            if op.is_dma:
                k = id(op.sobj)
                if k not in cur_map:
                    if free:
                        cur_map[k] = free.pop()
                    else:
                        phys.append([self.stack.enter_context(nc.semaphore("dsem_%d" % len(phys))), 0])
                        cur_map[k] = len(phys) - 1
                rec = phys[cur_map[k]]
                rec[1] += 16
                op.value = rec[1]
                op.sem = rec[0]
                op.signal = True
            elif op.signal:
                eng_cnt[op.eng] += 1
                op.value = eng_cnt[op.eng]
                op.sem = eng_sem[op.eng]
            if op.release:
                free.extend(cur_map.values())
                cur_map.clear()
                cur_epoch += 1
        final_dma = [(rec[0], rec[1]) for rec in phys]
        self.n_sems = len(phys) + len(ENGS)
        self.n_ops = len(self.ops)

        def run(eng_name, e):
            for op in self.eng_ops[eng_name]:
                for (s, v) in op.waits:
                    e.wait_ge(s, v)
                ins = op.fn(e)
                if op.signal:
                    ins.then_inc(op.sem, 16 if op.is_dma else 1)
            if eng_name == "sp":
                for (s, v) in final_dma:
                    if v > 0:
                        e.wait_ge(s, v)

        with nc.Block() as block:
            @block.tensor
            def _(e):
                run("pe", e)

            @block.scalar
            def _(e):
                run("act", e)

            @block.vector
            def _(e):
                run("dve", e)

            @block.gpsimd
            def _(e):
                run("pool", e)

            @block.sync
            def _(e):
                run("sp", e)
        self.stack.close()


def build(nl, debug=False, stop_at=-1, op_limit=0, ne_decl=NE):
    nc = bass.Bass("TRN2", target_bir_lowering=False)
    del _dram_objs[:]
    _dram_inputs.clear()
    fw = FW(nc)
    fw.stop_at = stop_at
    fw.op_limit = op_limit
    op = fw.op

    def MM(out, lhsT, rhs, R, W, start=True, stop=True):
        op("pe", lambda e: e.matmul(out, lhsT=lhsT, rhs=rhs, start=start, stop=stop), R, W)

    def TR(out, in_, ident, R, W):
        op("pe", lambda e: e.transpose(out=out, in_=in_, identity=ident), R, W)

    def ACT(out, in_, func, R, W, **kw):
        op("act", lambda e: e.activation(out=out, in_=in_, func=func, **kw), R, W)

    def TT(out, in0, in1, alu, R, W, eng="dve"):
        op(eng, lambda e: e.tensor_tensor(out=out, in0=in0, in1=in1, op=alu), R, W)

    def TS(out, in0, s1, s2, op0, op1, R, W, eng="dve", **kw):
        if op1 is None:
            op(eng, lambda e: e.tensor_scalar(out=out, in0=in0, scalar1=s1, scalar2=s2, op0=op0, **kw), R, W)
        else:
            op(eng, lambda e: e.tensor_scalar(out=out, in0=in0, scalar1=s1, scalar2=s2, op0=op0, op1=op1, **kw), R, W)

    def STT(out, in0, scalar, in1, op0, op1, R, W, eng="dve"):
        op(eng, lambda e: e.scalar_tensor_tensor(out=out, in0=in0, scalar=scalar, in1=in1, op0=op0, op1=op1), R, W)

    def CP(out, in_, R, W, eng="dve"):
        op(eng, lambda e: e.tensor_copy(out=out, in_=in_), R, W)

    def MS(ap, val, W, eng="dve"):
        op(eng, lambda e: e.memset(ap, val), (), W)

    def RED(out, in_, alu, R, W):
        op("dve", lambda e: e.tensor_reduce(out=out, in_=in_, axis=AX.X, op=alu), R, W)

    def RCP(out, in_, R, W):
        op("dve", lambda e: e.reciprocal(out=out, in_=in_), R, W)

    def rstd_from_ss(ss_obj, ss_ap, tmp_ap, out_ap, inv_n):
        ACT(tmp_ap, ss_ap, AF.Sqrt, [ss_obj], [ss_obj], scale=float(inv_n), bias=float(EPS))
        RCP(out_ap, tmp_ap, [ss_obj], [ss_obj])

    def din(name, shape, dt=F32):
        return fw.dram(name, shape, dt, kind="ExternalInput")

    x_in = din("x", [S, D])
    mem_in = din("mem", [256, D])
    g_mix = din("g_mix", [nl, D])
    w_in = din("w_in", [nl, D, DIN])
    b_gates = din("b_gates", [nl, 16])
    convw_d = din("convw", [nl, 128, 4, 5])
    g_head = din("g_head", [nl, 512])
    gcols_d = din("gcols", [nl, 128, 4])
    natab = din("natab", [nl, 5, 128, 2560])
    g_mem = din("g_mem", [nl, D])
    w_mkv = din("w_mem_kv", [nl, D, 512])
    w_out = din("w_out", [nl, D, D])
    g_ffn = din("g_ffn", [nl, D])
    w_rt = din("w_router", [nl, D, NE])
    w1 = din("w1", [nl, ne_decl, D, DFF])
    w3 = din("w3", [nl, ne_decl, D, DFF])
    w2 = din("w2", [nl, ne_decl, DFF, D])
    out_d = fw.dram("out", [S, D], F32, kind="ExternalOutput")
    dk = "ExternalOutput"
    qkraw_d = fw.dram("qkraw_d", [512, S], F32, kind=dk)
    so_d = fw.dram("so_d", [S, 512], BF16, kind=dk)
    v_d = fw.dram("v_d", [S, 512], BF16, kind=dk)
    hf_d = fw.dram("hf_d", [S, D], BF16, kind=dk)
    aff_d = fw.dram("aff_d", [S, NE], F32, kind=dk)
    if debug:
        dbg_xmix = fw.dram("dbg_xmix", [S, D], F32, kind=dk)
        dbg_yTm = fw.dram("dbg_yTm", [128, 4, S], BF16, kind=dk)
        dbg_yTnc = fw.dram("dbg_yTnc", [128, 4, S], BF16, kind=dk)
        dbg_qkT = fw.dram("dbg_qkT", [128, 4, S], BF16, kind=dk)
        dbg_idx = fw.dram("dbg_idx", [128, NE * 4], I32, kind=dk)
        dbg_nqT = fw.dram("dbg_nqT", [128, 2, S], BF16, kind=dk)
        dbg_gates = fw.dram("dbg_gates", [128, NT, 16], F32, kind=dk)
    out_tiles = [Obj("out_t%d" % t, None) for t in range(NT)]

    ident_f = fw.sbuf("ident_f", [128, 128], F32)
    ident_b = fw.sbuf("ident_b", [128, 128], BF16)
    io = fw.sbuf("io", [128, 128], F32)
    triU = fw.sbuf("triU", [128, 128], F32)
    triL = fw.sbuf("triL", [128, 128], F32)
    ones_f = fw.sbuf("ones_f", [128, 128], F32)
    ones_b = fw.sbuf("ones_b", [128, 128], BF16)
    iota512 = fw.sbuf("iota512", [128, 512], F32)
    regs = {}

    def _mkreg(e):
        regs["bc"] = e.alloc_register("bcreg")
        return e.reg_mov(regs["bc"], S - 1)
    op("pool", _mkreg, (), ())
    op("pool", lambda e: e.iota(io[:], pattern=[[1, 128]], base=0, channel_multiplier=-1,
                                allow_small_or_imprecise_dtypes=True), (), [io])
    op("pool", lambda e: e.iota(iota512[:], pattern=[[1, 512]], base=0, channel_multiplier=0,
                                allow_small_or_imprecise_dtypes=True), (), [iota512])
    op("dve", lambda e: e.tensor_single_scalar(out=ident_f[:], in_=io[:], scalar=0.0, op=ALU.is_equal), [io], [ident_f])
    CP(ident_b[:], ident_f[:], [ident_f], [ident_b])
    op("dve", lambda e: e.tensor_single_scalar(out=triU[:], in_=io[:], scalar=0.0, op=ALU.is_ge), [io], [triU])
    op("dve", lambda e: e.tensor_single_scalar(out=triL[:], in_=io[:], scalar=0.0, op=ALU.is_le), [io], [triL])
    MS(ones_f[:], 1.0, [ones_f])
    MS(ones_b[:], 1.0, [ones_b])
    try:
        fw.barrier()
        _layers(nl, debug, fw, locals())
    except StopBuild:
        while len(fw.scopes) > 1:
            fw.scopes.pop().close()
        scr = fw.sbuf("scr", [1, 64], F32)
        scrb = fw.sbuf("scrb", [1, 64], BF16)
        scri = fw.sbuf("scri", [1, 64], I32)
        for nm, o in list(locals().items()):
            if isinstance(o, Obj) and o.ap is not None and hasattr(o.ap, "shape") and "dram" in str(type(o.ap.tensor if hasattr(o.ap, "tensor") else "")).lower():
                pass
        for o in _dram_objs:
            ap = o.ap
            ix = tuple([0] * (len(ap.shape) - 2)) + (slice(0, 1), slice(0, 1))
            t = {F32: scr, BF16: scrb, I32: scri}[ap.dtype]
            if o.name in _dram_inputs:
                fw.dma("sp", t[0:1, 0:1], ap[ix], [o], [t], t)
            else:
                fw.dma("sp", ap[ix], t[0:1, 0:1], [t], [o], t)
    fw.emit()
    return nc, fw


def _layers(nl, debug, fw, env):
    globals().update({k: v for k, v in env.items() if k not in ("nl", "debug", "fw", "env")})
    op = fw.op
    for l in range(nl):
        x_src = x_in if l == 0 else out_d
        with fw.scope():
            kmT = fw.sbuf("kmT", [128, 2, 256], BF16)
            vm = fw.sbuf("vm", [128, 2, 4, 65], BF16)
            gates = fw.sbuf("gates", [128, NT, 16], F32)
            aff_all = fw.sbuf("aff_all", [128, NT, NE], F32)
            cnt_all = fw.sbuf("cnt_all", [128, NT, NE], F32)
            gcols = fw.sbuf("gcols", [128, 4], F32)
            fw.dma("sp", gcols[:], gcols_d[l], [gcols_d], [gcols], gcols)

            with fw.scope():
                yT_nc = fw.sbuf("yT_nc", [128, 4, S], BF16)
                with fw.scope():
                    nqT = fw.sbuf("nqT", [128, 2, S], BF16)
                    nkT = fw.sbuf("nkT", [128, 2, S], BF16)
                    cqT = fw.sbuf("cqT", [128, 2, S], BF16)
                    nv = fw.sbuf("nv", [128, NT, 4, 65], BF16)
                    MS(nv[:, :, :, 64:65], 1.0, [nv])
                    MS(vm[:, :, :, 64:65], 1.0, [vm])

                    with fw.scope():
                        gmem_b = fw.sbuf("gmem_b", [128, D], F32)
                        wkv = fw.sbuf("wkv", [128, 8, 512], BF16)
                        mt = [fw.sbuf("mt%d" % i, [128, D], F32) for i in range(2)]
                        mjunk = fw.sbuf("mjunk", [128, D], F32)
                        mn = fw.sbuf("mn", [128, D], BF16)
                        memT = fw.sbuf("memT", [128, 8, 256], BF16)
                        mst = fw.sbuf("mst", [128, 8], F32)
                        msq = fw.sbuf("msq", [128, 256], F32)
                        mss = fw.sbuf("mss", [128, 8], F32)
                        kmn = fw.sbuf("kmn", [128, 4, 64], BF16)
                        pT0 = fw.psum("pT0", [128, 8, 128], BF16)
                        pkv = fw.psum("pkv", [128, 512], F32)
                        fw.dma("sp", gmem_b[:], g_mem[l:l + 1, :].partition_broadcast(128), [g_mem], [gmem_b], gmem_b)
                        fw.dma("pool", wkv[:], w_mkv[l].rearrange("(k p) n -> p k n", p=128), [w_mkv], [wkv], wkv)
                        for i in range(2):
                            fw.dma("sp", mt[i][:], mem_in[i * 128:(i + 1) * 128, :], [mem_in], [mt[i]], mt[i])
                            MS(mst[:, 0:1], 0.0, [mst])
                            ACT(mjunk[:], mt[i][:], AF.Square, [mt[i]], [mjunk, mst], accum_out=mst[:, 0:1])
                            rstd_from_ss(mst, mst[:, 0:1], mst[:, 1:2], mst[:, 2:3], 1.0 / D)
                            STT(mn[:], mt[i][:], mst[:, 2:3], gmem_b[:], ALU.mult, ALU.mult, [mt[i], mst, gmem_b], [mn])
                            for k in range(8):
                                TR(pT0[:, k, :], mn[:, k * 128:(k + 1) * 128], ident_b[:], [mn, ident_b], [pT0])
                            ACT(memT[:, :, i * 128:(i + 1) * 128], pT0[:], AF.Copy, [pT0], [memT])
                        for i in range(2):
                            for k in range(8):
                                MM(pkv[:], memT[:, k, i * 128:(i + 1) * 128], wkv[:, k, :], [memT, wkv], [pkv],
                                   start=(k == 0), stop=(k == 7))
                            CP(vm[:, i, :, 0:64], pkv[:, 256:512].rearrange("p (h d) -> p h d", d=64), [pkv], [vm])
                            MS(mss[:, 0:4], 0.0, [mss])
                            for hh in range(4):
                                ACT(msq[:, hh * 64:(hh + 1) * 64], pkv[:, hh * 64:(hh + 1) * 64], AF.Square, [pkv], [msq, mss],
                                    accum_out=mss[:, hh:hh + 1])
                            rstd_from_ss(mss, mss[:, 0:4], mss[:, 4:8], mss[:, 4:8], 1.0 / 64)
                            TT(kmn[:], pkv[:, 0:256].rearrange("p (h d) -> p h d", d=64),
                               mss[:, 4:8].unsqueeze(2).to_broadcast([128, 4, 64]), ALU.mult, [pkv, mss], [kmn])
                            for hp in range(2):
                                TR(pT0[:, hp, :], kmn[:, 2 * hp:2 * hp + 2, :].rearrange("p h d -> p (h d)"), ident_b[:],
                                   [kmn, ident_b], [pT0])
                            ACT(kmT[:, :, i * 128:(i + 1) * 128], pT0[:, 0:2, :], AF.Copy, [pT0, gcols], [kmT],
                                scale=gcols[:, 3:4])
                    fw.barrier()

                    with fw.scope():
                        wi = fw.sbuf("wi", [128, 8, DIN], BF16)
                        gmix_b = fw.sbuf("gmix_b", [128, D], F32)
                        bg_b = fw.sbuf("bg_b", [128, 16], F32)
                        xt = [fw.sbuf("xt%d" % i, [128, D], F32) for i in range(2)]
                        xjunk = fw.sbuf("xjunk", [128, D], F32)
                        xn = [fw.sbuf("xn%d" % i, [128, D], BF16) for i in range(2)]
                        hT = [fw.sbuf("hT%d" % i, [128, 8, 512], BF16) for i in range(2)]
                        xst = [fw.sbuf("xst%d" % i, [128, 4], F32) for i in range(2)]
                        qkst = [fw.sbuf("qkst%d" % i, [128, 512], F32) for i in range(2)]
                        sot = [fw.sbuf("sot%d" % i, [128, 512], BF16) for i in range(2)]
                        vt = [fw.sbuf("vt%d" % i, [128, 512], BF16) for i in range(2)]
                        nsq = [fw.sbuf("nsq%d" % i, [128, 512], F32) for i in range(2)]
                        nss = [fw.sbuf("nss%d" % i, [128, 24], F32) for i in range(2)]
                        nqn = [fw.sbuf("nqn%d" % i, [128, 512], BF16) for i in range(2)]
                        cqn = [fw.sbuf("cqn%d" % i, [128, 256], BF16) for i in range(2)]
                        pT1 = [fw.psum("pT1_%d" % i, [128, 8, 128], BF16) for i in range(2)]
                        pmm = [fw.psum("pmm%d" % i, [128, 512], F32) for i in range(4)]
                        pqk = [fw.psum("pqk%d" % i, [128, 512], F32) for i in range(2)]
                        fw.dma("pool", wi[:, 0:4, :], w_in[l, 0:512, :].rearrange("(k p) n -> p k n", p=128), [w_in], [wi], wi)
                        fw.dma("pool", wi[:, 4:8, :], w_in[l, 512:1024, :].rearrange("(k p) n -> p k n", p=128), [w_in], [wi], wi)
                        fw.dma("sp", gmix_b[:], g_mix[l:l + 1, :].partition_broadcast(128), [g_mix], [gmix_b], gmix_b)
                        fw.dma("sp", bg_b[:], b_gates[l:l + 1, :].partition_broadcast(128), [b_gates], [bg_b], bg_b)
                        mmi = [0]

                        def p1_prep(st):
                            hTs = hT[st % 2]
                            for tt in range(4):
                                t = st * 4 + tt
                                b = t % 2
                                fw.dma("sp", xt[b][:], x_src[t * 128:(t + 1) * 128, :], [out_tiles[t]], [xt[b]], xt[b])
                                MS(xst[b][:, 0:1], 0.0, [xst[b]])
                                ACT(xjunk[:], xt[b][:], AF.Square, [xt[b]], [xjunk, xst[b]], accum_out=xst[b][:, 0:1])
                                rstd_from_ss(xst[b], xst[b][:, 0:1], xst[b][:, 1:2], xst[b][:, 2:3], 1.0 / D)
                                STT(xn[b][:], xt[b][:], xst[b][:, 2:3], gmix_b[:], ALU.mult, ALU.mult,
                                    [xt[b], xst[b], gmix_b], [xn[b]])
                                for k in range(8):
                                    TR(pT1[b][:, k, :], xn[b][:, k * 128:(k + 1) * 128], ident_b[:], [xn[b], ident_b], [pT1[b]])
                                ACT(hTs[:, :, tt * 128:(tt + 1) * 128], pT1[b][:], AF.Copy, [pT1[b]], [hTs])
                        def p1_body(st):
                            hTs = hT[st % 2]
                            for c in range(4):
                                pq = pqk[c % 2]
                                for k in range(8):
                                    MM(pq[:], wi[:, k, c * 128:(c + 1) * 128], hTs[:, k, :], [wi, hTs], [pq],
                                       start=(k == 0), stop=(k == 7))
                                qs = qkst[c % 2]
                                CP(qs[:], pq[:], [pq], [qs])
                                fw.dma("sp", qkraw_d[c * 128:(c + 1) * 128, st * 512:(st + 1) * 512], qs[:], [qs], [qkraw_d], qs)
                            for tt in range(4):
                                t = st * 4 + tt
                                b = t % 2
                                lh = lambda k: hTs[:, k, tt * 128:(tt + 1) * 128]
                                pm = pmm[mmi[0] % 4]; mmi[0] += 1
                                for k in range(8):
                                    MM(pm[:], lh(k), wi[:, k, 512:1024], [hTs, wi], [pm], start=(k == 0), stop=(k == 7))
                                ACT(vt[b][:], pm[:], AF.Copy, [pm], [vt[b]])
                                fw.dma("sp", v_d[t * 128:(t + 1) * 128, :], vt[b][:], [vt[b]], [v_d], vt[b])
                                pm = pmm[mmi[0] % 4]; mmi[0] += 1
                                for k in range(8):
                                    MM(pm[:], lh(k), wi[:, k, 1024:1536], [hTs, wi], [pm], start=(k == 0), stop=(k == 7))
                                ACT(sot[b][:], pm[:], AF.Sigmoid, [pm], [sot[b]])
                                fw.dma("sp", so_d[t * 128:(t + 1) * 128, :], sot[b][:], [sot[b]], [so_d], sot[b])
                                pm = pmm[mmi[0] % 4]; mmi[0] += 1
                                for k in range(8):
                                    MM(pm[:], lh(k), wi[:, k, 1552:2064], [hTs, wi], [pm], start=(k == 0), stop=(k == 7))
                                MS(nss[b][:, 0:8], 0.0, [nss[b]])
                                for hh in range(8):
                                    ACT(nsq[b][:, hh * 64:(hh + 1) * 64], pm[:, hh * 64:(hh + 1) * 64], AF.Square, [pm], [nsq[b], nss[b]],
                                        accum_out=nss[b][:, hh:hh + 1])
                                rstd_from_ss(nss[b], nss[b][:, 0:8], nss[b][:, 8:16], nss[b][:, 8:16], 1.0 / 64)
                                TT(nqn[b][:].rearrange("p (h d) -> p h d", d=64), pm[:].rearrange("p (h d) -> p h d", d=64),
                                   nss[b][:, 8:16].unsqueeze(2).to_broadcast([128, 8, 64]), ALU.mult, [pm, nss[b]], [nqn[b]])
                                for j in range(4):
                                    TR(pT1[b][:, j, :], nqn[b][:, j * 128:(j + 1) * 128], ident_b[:], [nqn[b], ident_b], [pT1[b]])
                                ACT(nqT[:, :, t * 128:(t + 1) * 128], pT1[b][:, 0:2, :], AF.Copy, [pT1[b], gcols], [nqT],
                                    scale=gcols[:, 0:1])
                                ACT(nkT[:, :, t * 128:(t + 1) * 128], pT1[b][:, 2:4, :], AF.Copy, [pT1[b], gcols], [nkT],
                                    scale=gcols[:, 1:2])
                                pm = pmm[mmi[0] % 4]; mmi[0] += 1
                                for k in range(8):
                                    MM(pm[:], lh(k), wi[:, k, 2064:2576], [hTs, wi], [pm], start=(k == 0), stop=(k == 7))
                                CP(nv[:, t, :, 0:64], pm[:, 0:256].rearrange("p (h d) -> p h d", d=64), [pm], [nv])
                                MS(nss[b][:, 16:20], 0.0, [nss[b]])
                                for hh in range(4):
                                    ACT(nsq[b][:, hh * 64:(hh + 1) * 64], pm[:, 256 + hh * 64:256 + (hh + 1) * 64], AF.Square, [pm],
                                        [nsq[b], nss[b]], accum_out=nss[b][:, 16 + hh:17 + hh])
                                rstd_from_ss(nss[b], nss[b][:, 16:20], nss[b][:, 20:24], nss[b][:, 20:24], 1.0 / 64)
                                TT(cqn[b][:].rearrange("p (h d) -> p h d", d=64), pm[:, 256:512].rearrange("p (h d) -> p h d", d=64),
                                   nss[b][:, 20:24].unsqueeze(2).to_broadcast([128, 4, 64]), ALU.mult, [pm, nss[b]], [cqn[b]])
                                for j in range(2):
                                    TR(pT1[b][:, 4 + j, :], cqn[b][:, j * 128:(j + 1) * 128], ident_b[:], [cqn[b], ident_b], [pT1[b]])
                                ACT(cqT[:, :, t * 128:(t + 1) * 128], pT1[b][:, 4:6, :], AF.Copy, [pT1[b], gcols], [cqT],
                                    scale=gcols[:, 2:3])
                                pm = pmm[mmi[0] % 4]; mmi[0] += 1
                                for k in range(8):
                                    MM(pm[:, 0:16], lh(k), wi[:, k, 1536:1552], [hTs, wi], [pm], start=(k == 0), stop=(k == 7))
                                TT(gates[:, t, :], pm[:, 0:16], bg_b[:], ALU.add, [pm, bg_b], [gates])

                        p1_prep(0)
                        for st in range(8):
                            if st + 1 < 8:
                                p1_prep(st + 1)
                            p1_body(st)
                    fw.barrier()

                    if debug and l == 0:
                        fw.dma("sp", dbg_nqT.ap, nqT[:], [nqT], [dbg_nqT], nqT)
                        fw.barrier()
                    with fw.scope():
                        EB = [fw.sbuf("EB%d" % i, [128, 2560], BF16) for i in range(5)]
                        tabst = fw.sbuf("tabst", [128, 2560], F32)
                        Ena = [fw.sbuf("Ena%d" % i, [128, 640], BF16) for i in range(2)]
                        PTn = [fw.sbuf("PTn%d" % i, [128, 640], BF16) for i in range(2)]
                        Ec = [fw.sbuf("Ec%d" % i, [128, 256], BF16) for i in range(2)]
                        ycat = [fw.sbuf("ycat%d" % i, [128, 512], BF16) for i in range(2)]
                        rec = [fw.sbuf("rec%d" % i, [128, 8], F32) for i in range(2)]
                        ps_na = [fw.psum("ps_na%d" % i, [128, 1024], F32) for i in range(2)]
                        ps_c = fw.psum("ps_c", [128, 2, 256], F32)
                        po_na = fw.psum("po_na", [128, 4, 65], F32)
                        po_c = fw.psum("po_c", [128, 4, 65], F32)
                        pT3 = fw.psum("pT3", [128, 4, 128], BF16)
                        for p in range(5):
                            fw.dma("sp", tabst[:], natab[l, p], [natab], [tabst], tabst)
                            ACT(EB[p][:], tabst[:], AF.Exp, [tabst], [EB[p]])
                        ui = 0
                        for j in range(NT):
                            kb = min(max(j - 2, 0), 27)
                            pat = {0: 0, 1: 1, 30: 3, 31: 4}.get(j, 2)
                            yb = ycat[j % 2]
                            rb = rec[j % 2]
                            qs = slice(j * 128, (j + 1) * 128)
                            for h in range(4):
                                hp, lo = h // 2, (h % 2) * 64
                                pn = ps_na[ui % 2]
                                En = Ena[ui % 2]
                                Pn = PTn[ui % 2]
                                Ecb = Ec[ui % 2]
                                ui += 1
                                for dl in range(5):
                                    ks = slice((kb + dl) * 128, (kb + dl + 1) * 128)
                                    MM(pn[:, dl * 128:(dl + 1) * 128], nkT[lo:lo + 64, hp, ks], nqT[lo:lo + 64, hp, qs],
                                       [nkT, nqT], [pn])
                                ACT(En[:], pn[:, 0:640], AF.Exp, [pn], [En], scale=0.125)
                                TT(Pn[:], En[:], EB[pat][:, h * 640:(h + 1) * 640], ALU.mult, [En, EB[pat]], [Pn])
                                for dl in range(5):
                                    MM(po_na[:, h, :], Pn[:, dl * 128:(dl + 1) * 128], nv[:, kb + dl, h, :], [Pn, nv], [po_na],
                                       start=(dl == 0), stop=(dl == 4))
                                for i in range(2):
                                    MM(ps_c[:, h % 2, i * 128:(i + 1) * 128], kmT[lo:lo + 64, hp, i * 128:(i + 1) * 128],
                                       cqT[lo:lo + 64, hp, qs], [kmT, cqT], [ps_c])
                                ACT(Ecb[:], ps_c[:, h % 2, :], AF.Exp, [ps_c], [Ecb], scale=0.125)
                                for i in range(2):
                                    MM(po_c[:, h, :], Ecb[:, i * 128:(i + 1) * 128], vm[:, i, h, :], [Ecb, vm], [po_c],
                                       start=(i == 0), stop=(i == 1))
                            RCP(rb[:, 0:4], po_na[:, :, 64], [po_na], [rb])
                            TT(yb[:, 0:256].rearrange("p (h d) -> p h d", d=64), po_na[:, :, 0:64],
                               rb[:, 0:4].unsqueeze(2).to_broadcast([128, 4, 64]), ALU.mult, [po_na, rb], [yb])
                            RCP(rb[:, 4:8], po_c[:, :, 64], [po_c], [rb])
                            TT(yb[:, 256:512].rearrange("p (h d) -> p h d", d=64), po_c[:, :, 0:64],
                               rb[:, 4:8].unsqueeze(2).to_broadcast([128, 4, 64]), ALU.mult, [po_c, rb], [yb])
                            for k in range(4):
                                TR(pT3[:, k, :], yb[:, k * 128:(k + 1) * 128], ident_b[:], [yb, ident_b], [pT3])
                            ACT(yT_nc[:, :, qs], pT3[:], AF.Copy, [pT3], [yT_nc])
                    fw.barrier()

                with fw.scope():
                    yT_m = fw.sbuf("yT_m", [128, 4, S], BF16)
                    with fw.scope():
                        qkT = fw.sbuf("qkT", [128, 4, S], BF16)
                        with fw.scope():
                            convw = fw.sbuf("convw", [128, 4, 5], F32)
                            raw = [fw.sbuf("raw%d" % i, [128, 516], F32) for i in range(2)]
                            cacc = [fw.sbuf("cacc%d" % i, [128, 512], F32) for i in range(2)]
                            fw.dma("sp", convw[:], convw_d[l], [convw_d], [convw], convw)
                            ui = 0
                            for st in range(8):
                                for c in range(4):
                                    r = raw[ui % 2]
                                    a = cacc[ui % 2]
                                    ui += 1
                                    lo_t = st * 512 - 2
                                    hi_t = st * 512 + 514
                                    d0, d1 = 0, 516
                                    if st == 0:
                                        MS(r[:, 0:2], 0.0, [r])
                                        lo_t, d0 = 0, 2
                                    if st == 7:
                                        MS(r[:, 514:516], 0.0, [r])
                                        hi_t, d1 = S, 514
                                    fw.dma("sp", r[:, d0:d1], qkraw_d[c * 128:(c + 1) * 128, lo_t:hi_t], [qkraw_d], [r], r)
                                    TS(a[:], r[:, 0:512], convw[:, c, 0:1], None, ALU.mult, None, [r, convw], [a])
                                    for jj in range(1, 5):
                                        STT(a[:], r[:, jj:jj + 512], convw[:, c, jj:jj + 1], a[:], ALU.mult, ALU.add,
                                            [r, convw, a], [a])
                                    ACT(qkT[:, c, st * 512:(st + 1) * 512], a[:], AF.Silu, [a], [qkT])
                        fw.barrier()

                        if debug and l == 0:
                            fw.dma("sp", dbg_qkT.ap, qkT[:], [qkT], [dbg_qkT], qkT)
                            fw.dma("sp", dbg_gates.ap, gates[:], [gates], [dbg_gates], gates)
                            fw.barrier()
                        with fw.scope():
                            ghead_b = fw.sbuf("ghead_b", [128, 512], F32)
                            fw.dma("sp", ghead_b[:], g_head[l:l + 1, :].partition_broadcast(128), [g_head], [ghead_b], ghead_b)
                            LI = [fw.sbuf("LI%d" % d, [128, 128], F32) for d in range(2)]
                            LF = [fw.sbuf("LF%d" % d, [128, 128], F32) for d in range(2)]
                            Bc = [fw.sbuf("Bc%d" % d, [128, 128], F32) for d in range(2)]
                            Gc = [fw.sbuf("Gc%d" % d, [128, 128], F32) for d in range(2)]
                            Es = [fw.sbuf("Es%d" % d, [128, 128], F32) for d in range(2)]
                            Ws = [fw.sbuf("Ws%d" % d, [128, 128], F32) for d in range(2)]
                            EM = [fw.sbuf("EM%d" % d, [128, 128], F32) for d in range(2)]
                            EG = [fw.sbuf("EG%d" % d, [128, 128], F32) for d in range(2)]
                            gtmp = fw.sbuf("gtmp", [128, 128], F32)
                            pg = fw.psum("pg", [128, 2, 128], F32)
                            v3 = lambda o: o[:].rearrange("p (c h) -> p c h", h=4)
                            for d in range(2):
                                CP(v3(LI[d]), gates[:, :, 8 * d:8 * d + 4], [gates], [LI[d]])
                                ACT(v3(gtmp), gates[:, :, 8 * d + 4:8 * d + 8], AF.Exp, [gates], [gtmp], scale=-1.0)
                                ACT(gtmp[:], gtmp[:], AF.Ln, [gtmp], [gtmp], bias=1.0)
                                TS(LF[d][:], gtmp[:], -1.0, None, ALU.mult, None, [gtmp], [LF[d]])
                                MM(pg[:, 0, :], (triU if d == 0 else triL)[:], LF[d][:], [triU, triL, LF[d]], [pg])
                                MM(pg[:, 1, :], ones_f[:], LF[d][:], [ones_f, LF[d]], [pg])
                                CP(Bc[d][:], pg[:, 0, :], [pg], [Bc[d]])
                                CP(Gc[d][:], pg[:, 1, :], [pg], [Gc[d]])
                                TT(gtmp[:], LI[d][:], Bc[d][:], ALU.subtract, [LI[d], Bc[d]], [gtmp])
                                ACT(Es[d][:], gtmp[:], AF.Exp, [gtmp], [Es[d]])
                                TT(gtmp[:], gtmp[:], Gc[d][:], ALU.add, [gtmp, Gc[d]], [gtmp])
                                ACT(Ws[d][:], gtmp[:], AF.Exp, [gtmp], [Ws[d]])
                                ACT(EM[d][:], Bc[d][:], AF.Exp, [Bc[d]], [EM[d]], scale=-1.0, bias=LN8)
                                ACT(EG[d][:], Gc[d][:], AF.Exp, [Gc[d]], [EG[d]])
                            maskd = [triU, triL]

                            Cprev = [fw.sbuf("Cprev%d" % d, [128, NT, 129], BF16) for d in range(2)]
                            Cst = [fw.sbuf("Cst%d" % d, [128, 129], F32) for d in range(2)]
                            EGs = [fw.sbuf("EGs%d" % d, [128, NT], F32) for d in range(2)]
                            kwA = [fw.sbuf("kwA%d" % i, [128, 128], BF16) for i in range(4)]
                            kwB = [fw.sbuf("kwB%d" % i, [128, 128], BF16) for i in range(4)]
                            v1 = [fw.sbuf("v1_%d" % i, [128, 2, 129], BF16) for i in range(4)]
                            sob = [fw.sbuf("sob%d" % i, [128, 256], BF16) for i in range(2)]
                            PTm = [fw.sbuf("PTm%d" % i, [128, 2, 2, 128], BF16) for i in range(2)]
                            dn = [fw.sbuf("dn%d" % i, [128, 16], F32) for i in range(2)]
                            hs = [fw.sbuf("hs%d" % i, [128, 2, 128], F32) for i in range(2)]
                            hj = fw.sbuf("hj", [128, 128], F32)
                            ymb = [fw.sbuf("ymb%d" % i, [128, 256], BF16) for i in range(2)]
                            pTk_t = fw.psum("pTk", [128, 2, 128], BF16)
                            pTk = [Obj("pTk%d" % i, pTk_t[:, i, :]) for i in range(2)]
                            pkvm_t = fw.psum("pkvm", [128, 2, 129], F32)
                            pkvm = [Obj("pkvm%d" % i, pkvm_t[:, i, :]) for i in range(2)]
                            psm = fw.psum("psm", [128, 2, 512], F32)
                            pnd = fw.psum("pnd", [128, 2, 2, 256], F32)
                            pTy = fw.psum("pTy", [128, 2, 128], BF16)
                            for i in range(4):
                                MS(kwA[i][:, 64:128], 0.0, [kwA[i]])
                                MS(kwB[i][:, 0:64], 0.0, [kwB[i]])
                                MS(v1[i][:, :, 128:129], 1.0, [v1[i]])
                            for hp in range(2):
                                h0 = 2 * hp
                                for d in range(2):
                                    MS(Cst[d][:], 0.0, [Cst[d]])
                                    CP(EGs[d][0:64, :], v3(EG[d])[0:64, :, h0], [EG[d]], [EGs[d]])
                                    CP(EGs[d][64:128, :], v3(EG[d])[64:128, :, h0 + 1], [EG[d]], [EGs[d]])
                                ui = 0
                                for ci in range(NT):
                                    for d in range(2):
                                        c = ci if d == 0 else NT - 1 - ci
                                        cs = slice(c * 128, (c + 1) * 128)
                                        ka, kb_, vv, pk, pt = kwA[ui % 4], kwB[ui % 4], v1[ui % 4], pkvm[ui % 2], pTk[ui % 2]
                                        ui += 1
                                        ACT(Cprev[d][:, c, :], Cst[d][:], AF.Copy, [Cst[d]], [Cprev[d]])
                                        fw.dma("sp", vv[:, :, 0:128], v_d[cs, h0 * 128:(h0 + 2) * 128].rearrange("p (h d) -> p h d", d=128),
                                               [v_d], [vv], vv)
                                        TR(pt[:], qkT[:, 2 + hp, cs], ident_b[:], [qkT, ident_b], [pt])
                                        TS(ka[:, 0:64], pt[:, 0:64], Ws[d][:, c * 4 + h0:c * 4 + h0 + 1], None, ALU.mult, None,
                                           [pt, Ws[d]], [ka])
                                        TS(kb_[:, 64:128], pt[:, 64:128], Ws[d][:, c * 4 + h0 + 1:c * 4 + h0 + 2], None, ALU.mult, None,
                                           [pt, Ws[d]], [kb_])
                                        MM(pk[:], ka[:], vv[:, 0, :], [ka, vv], [pk], start=True, stop=False)
                                        MM(pk[:], kb_[:], vv[:, 1, :], [kb_, vv], [pk], start=False, stop=True)
                                        STT(Cst[d][:], Cst[d][:], EGs[d][:, c:c + 1], pk[:], ALU.mult, ALU.add,
                                            [Cst[d], EGs[d], pk], [Cst[d]])
                                for c in range(NT):
                                    cs = slice(c * 128, (c + 1) * 128)
                                    vv = v1[c % 4]
                                    sb = sob[c % 2]
                                    Pm = PTm[c % 2]
                                    dnb = dn[c % 2]
                                    hsb = hs[c % 2]
                                    yb = ymb[c % 2]
                                    fw.dma("sp", vv[:, :, 0:128], v_d[cs, h0 * 128:(h0 + 2) * 128].rearrange("p (h d) -> p h d", d=128),
                                           [v_d], [vv], vv)
                                    fw.dma("sp", sb[:], so_d[cs, h0 * 128:(h0 + 2) * 128], [so_d], [sb], sb)
                                    for i in range(2):
                                        lo = i * 64
                                        MM(psm[:, i, 0:128], qkT[lo:lo + 64, 2 + hp, cs], qkT[lo:lo + 64, hp, cs], [qkT], [psm])
                                    for d in range(2):
                                        for i in range(2):
                                            col = c * 4 + h0 + i
                                            STT(Pm[:, d, i, :], psm[:, i, 0:128], Es[d][:, col:col + 1], maskd[d][:], ALU.mult, ALU.mult,
                                                [psm, Es[d], maskd[d]], [Pm])
                                    for d in range(2):
                                        for i in range(2):
                                            lo = i * 64
                                            MM(pnd[:, d, i, 0:129], Pm[:, d, i, :], vv[:, i, :], [Pm, vv], [pnd], start=True, stop=False)
                                            MM(pnd[:, d, i, 0:129], qkT[lo:lo + 64, hp, cs], Cprev[d][lo:lo + 64, c, :],
                                               [qkT, Cprev[d]], [pnd], start=False, stop=True)
                                    for d in range(2):
                                        col = c * 4 + h0
                                        TT(dnb[:, 2 * d:2 * d + 2], pnd[:, d, :, 128], EM[d][:, col:col + 2], ALU.max,
                                           [pnd, EM[d]], [dnb])
                                        STT(dnb[:, 4 + 2 * d:6 + 2 * d], pnd[:, d, :, 128], -1.0, dnb[:, 2 * d:2 * d + 2],
                                            ALU.mult, ALU.max, [pnd, dnb], [dnb])
                                    RCP(dnb[:, 8:12], dnb[:, 4:8], [dnb], [dnb])
                                    for i in range(2):
                                        TS(hsb[:, i, :], pnd[:, 0, i, 0:128], dnb[:, 8 + i:9 + i], None, ALU.mult, None, [pnd, dnb], [hsb])
                                        STT(hsb[:, i, :], pnd[:, 1, i, 0:128], dnb[:, 10 + i:11 + i], hsb[:, i, :], ALU.mult, ALU.add,
                                            [pnd, dnb, hsb], [hsb])
                                        MS(dnb[:, 12 + i:13 + i], 0.0, [dnb])
                                        ACT(hj[:], hsb[:, i, :], AF.Square, [hsb], [hj, dnb], accum_out=dnb[:, 12 + i:13 + i])
                                    rstd_from_ss(dnb, dnb[:, 12:14], dnb[:, 14:16], dnb[:, 14:16], 1.0 / 128)
                                    for i in range(2):
                                        STT(hsb[:, i, :], hsb[:, i, :], dnb[:, 14 + i:15 + i], ghead_b[:, (h0 + i) * 128:(h0 + i + 1) * 128],
                                            ALU.mult, ALU.mult, [hsb, dnb, ghead_b], [hsb])
                                    TT(yb[:], hsb[:].rearrange("p i d -> p (i d)"), sb[:], ALU.mult, [hsb, sb], [yb])
                                    for i in range(2):
                                        TR(pTy[:, i, :], yb[:, i * 128:(i + 1) * 128], ident_b[:], [yb, ident_b], [pTy])
                                    ACT(yT_m[:, h0:h0 + 2, cs], pTy[:], AF.Copy, [pTy], [yT_m])
                        fw.barrier()

                    with fw.scope():
                        wo = fw.sbuf("wo", [128, 8, D], BF16)
                        wr = fw.sbuf("wr", [128, 8, NE], BF16)
                        gffn_b = fw.sbuf("gffn_b", [128, D], F32)
                        xt5 = [fw.sbuf("xt5_%d" % i, [128, D], F32) for i in range(2)]
                        xo = [fw.sbuf("xo%d" % i, [128, D], F32) for i in range(2)]
                        xj5 = fw.sbuf("xj5", [128, D], F32)
                        hf = [fw.sbuf("hf%d" % i, [128, D], BF16) for i in range(2)]
                        hfT = [fw.sbuf("hfT%d" % i, [128, 8, 128], BF16) for i in range(2)]
                        st5 = [fw.sbuf("st5_%d" % i, [128, 8], F32) for i in range(2)]
                        ex5 = [fw.sbuf("ex5_%d" % i, [128, NE], F32) for i in range(2)]
                        po5 = [fw.psum("po5_%d" % i, [128, D], F32) for i in range(2)]
                        pT5 = [fw.psum("pT5_%d" % i, [128, 8, 128], BF16) for i in range(2)]
                        plog = fw.psum("plog", [128, NE], F32)
                        fw.dma("pool", wo[:], w_out[l].rearrange("(k p) n -> p k n", p=128), [w_out], [wo], wo)
                        if debug and l == 0:
                            fw.dma("sp", dbg_yTm.ap, yT_m[:], [yT_m], [dbg_yTm], yT_m)
                            fw.dma("sp", dbg_yTnc.ap, yT_nc[:], [yT_nc], [dbg_yTnc], yT_nc)
                        fw.dma("pool", wr[:], w_rt[l].rearrange("(k p) n -> p k n", p=128), [w_rt], [wr], wr)
                        fw.dma("sp", gffn_b[:], g_ffn[l:l + 1, :].partition_broadcast(128), [g_ffn], [gffn_b], gffn_b)
                        for t in range(NT):
                            b = t % 2
                            ts_ = slice(t * 128, (t + 1) * 128)
                            fw.dma("sp", xt5[b][:], x_src[ts_, :], [out_tiles[t]], [xt5[b]], xt5[b])
                            for n in range(2):
                                for k in range(8):
                                    src = yT_m if k < 4 else yT_nc
                                    MM(po5[b][:, n * 512:(n + 1) * 512], src[:, k % 4, ts_], wo[:, k, n * 512:(n + 1) * 512],
                                       [src, wo], [po5[b]], start=(k == 0), stop=(k == 7))
                            TT(xo[b][:], po5[b][:], xt5[b][:], ALU.add, [po5[b], xt5[b]], [xo[b]])
                            fw.dma("sp", out_d[ts_, :], xo[b][:], [xo[b]], [out_tiles[t]], xo[b])
                            if debug and l == 0:
                                fw.dma("sp", dbg_xmix[ts_, :], xo[b][:], [xo[b]], [dbg_xmix], xo[b])
                            MS(st5[b][:, 0:1], 0.0, [st5[b]])
                            ACT(xj5[:], xo[b][:], AF.Square, [xo[b]], [xj5, st5[b]], accum_out=st5[b][:, 0:1])
                            rstd_from_ss(st5[b], st5[b][:, 0:1], st5[b][:, 1:2], st5[b][:, 2:3], 1.0 / D)
                            STT(hf[b][:], xo[b][:], st5[b][:, 2:3], gffn_b[:], ALU.mult, ALU.mult, [xo[b], st5[b], gffn_b], [hf[b]])
                            fw.dma("sp", hf_d[ts_, :], hf[b][:], [hf[b]], [hf_d], hf[b])
                            for k in range(8):
                                TR(pT5[b][:, k, :], hf[b][:, k * 128:(k + 1) * 128], ident_b[:], [hf[b], ident_b], [pT5[b]])
                            ACT(hfT[b][:], pT5[b][:], AF.Copy, [pT5[b]], [hfT[b]])
                            for k in range(8):
                                MM(plog[:], hfT[b][:, k, :], wr[:, k, :], [hfT[b], wr], [plog], start=(k == 0), stop=(k == 7))
                            MS(st5[b][:, 5:6], 0.0, [st5[b]])
                            ACT(ex5[b][:], plog[:], AF.Exp, [plog, st5[b]], [ex5[b], st5[b]], accum_out=st5[b][:, 5:6])
                            RCP(st5[b][:, 6:7], st5[b][:, 5:6], [st5[b]], [st5[b]])
                            TS(aff_all[:, t, :], ex5[b][:], st5[b][:, 6:7], None, ALU.mult, None, [ex5[b], st5[b]], [aff_all])
                        fw.dma("sp", aff_d.ap.rearrange("(i p) e -> p i e", p=128), aff_all[:], [aff_all], [aff_d], aff_all)
                    fw.barrier()

            with fw.scope():
                affT = fw.sbuf("affT", [16, S], F32)
                rjunk = fw.sbuf("rjunk", [16, S], F32)
                rmask = fw.sbuf("rmask", [16, S], F32)
                rones = fw.sbuf("rones", [16, S], F32)
                rs = fw.sbuf("rs", [16, 8], F32)
                pTa = [fw.psum("pTa%d" % i, [16, 512], F32) for i in range(2)]
                pcn = fw.psum("pcn", [128, NT, NE], F32)
                for g4 in range(8):
                    pa = pTa[g4 % 2]
                    for i in range(4):
                        t = g4 * 4 + i
                        TR(pa[:, i * 128:(i + 1) * 128], aff_all[:, t, :], ident_f[:], [aff_all, ident_f], [pa])
                    CP(affT[:, g4 * 512:(g4 + 1) * 512], pa[:], [pa], [affT])
                MS(rones[:], 1.0, [rones])
                MS(rs[:, 0:1], 0.0, [rs])
                MS(rs[:, 1:2], 1.0, [rs])
                for it in range(28):
                    TT(rs[:, 2:3], rs[:, 0:1], rs[:, 1:2], ALU.add, [rs], [rs])
                    TS(rs[:, 2:3], rs[:, 2:3], 0.5, None, ALU.mult, None, [rs], [rs])
                    TS(rjunk[:], affT[:], rs[:, 2:3], 0.0, ALU.is_ge, ALU.add, [affT, rs], [rjunk, rs], accum_out=rs[:, 3:4])
                    TS(rs[:, 4:5], rs[:, 3:4], float(CAP), None, ALU.is_ge, None, [rs], [rs])
                    TT(rs[:, 5:6], rs[:, 2:3], rs[:, 0:1], ALU.subtract, [rs], [rs])
                    STT(rs[:, 0:1], rs[:, 5:6], rs[:, 4:5], rs[:, 0:1], ALU.mult, ALU.add, [rs], [rs])
                    TT(rs[:, 5:6], rs[:, 1:2], rs[:, 2:3], ALU.subtract, [rs], [rs])
                    STT(rs[:, 1:2], rs[:, 5:6], rs[:, 4:5], rs[:, 2:3], ALU.mult, ALU.add, [rs], [rs])
                TS(rmask[:], affT[:], rs[:, 0:1], None, ALU.is_ge, None, [affT, rs], [rmask])
                op("dve", lambda e: e.tensor_tensor_scan(out=rjunk[:], data0=rones[:], data1=rmask[:], initial=0.0,
                                                         op0=ALU.mult, op1=ALU.add), [rones, rmask], [rjunk])
                for t in range(NT):
                    TR(pcn[:, t, :], rjunk[:, t * 128:(t + 1) * 128], ident_f[0:16, 0:16], [rjunk, ident_f], [pcn])
                CP(cnt_all[:], pcn[:], [pcn], [cnt_all])
            fw.barrier()

            with fw.scope():
                NB13 = 3
                w1c = [fw.sbuf("w1c%d" % i, [128, 8, 512], BF16) for i in range(NB13)]
                w3c = [fw.sbuf("w3c%d" % i, [128, 8, 512], BF16) for i in range(NB13)]
                w2c = [fw.sbuf("w2c%d" % i, [128, 4, D], BF16) for i in range(4)]
                xes = [fw.sbuf("xes%d" % i, [128, D], BF16) for i in range(4)]
                gts = [fw.sbuf("gts%d" % i, [128, NE], F32) for i in range(4)]
                xeT = [fw.sbuf("xeT%d" % i, [128, 8, 512], BF16) for i in range(2)]
                hidT = fw.sbuf("hidT", [128, 16, 512], BF16)
                stmp = [fw.sbuf("stmp%d" % i, [128, 512], F32) for i in range(2)]
                ye = [fw.sbuf("ye%d" % i, [128, D], F32) for i in range(2)]
                idxf = fw.sbuf("idxf", [128, NE * 4], I32)
                cacc7 = [fw.sbuf("cacc7_%d" % i, [128, 512], BF16) for i in range(2)]
                pT7 = fw.psum("pT7", [128, 8, 128], BF16)
                ph = [fw.psum("ph%d" % i, [128, 512], F32) for i in range(4)]
                py = [fw.psum("py%d" % i, [128, 512], F32) for i in range(2)]
                pidx = fw.psum("pidx", [128, NE * 4], F32)
                idx_objs = [Obj("idx%d" % e, None) for e in range(NE)]
                idxg = [fw.sbuf("idxg%d" % i, [128, 1], I32) for i in range(4)]
                idxs = [fw.sbuf("idxs%d" % i, [128, 1], I32) for i in range(4)]
                w13_i = [0]

                def load_w13(e, c):
                    bi = w13_i[0] % NB13
                    w13_i[0] += 1
                    fw.dma("pool", w1c[bi][:], w1[l, e, :, c * 512:(c + 1) * 512].rearrange("(k p) f -> p k f", p=128),
                           [w1], [w1c[bi]], w1c[bi])
                    fw.dma("pool", w3c[bi][:], w3[l, e, :, c * 512:(c + 1) * 512].rearrange("(k p) f -> p k f", p=128),
                           [w3], [w3c[bi]], w3c[bi])
                    return bi

                def load_w2(e):
                    for c in range(4):
                        fw.dma("pool", w2c[c][:], w2[l, e, c * 512:(c + 1) * 512, :].rearrange("(c p) d -> p c d", p=128),
                               [w2], [w2c[c]], w2c[c])

                def build_idx_gen(e):
                    a = cacc7[e % 2]
                    MS(a[:], 0.0, [a])
                    for t in range(NT):
                        STT(a[:], iota512[:], cnt_all[:, t, e:e + 1], a[:], ALU.is_ge, ALU.add, [iota512, cnt_all, a], [a])
                        if t % 2 == 1:
                            yield
                    for g in range(4):
                        MM(pidx[:, e * 4 + g:e * 4 + g + 1], a[:, g * 128:(g + 1) * 128], ones_b[:, 0:1], [a, ones_b], [pidx])
                    CP(idxf[:, e * 4:(e + 1) * 4], pidx[:, e * 4:(e + 1) * 4], [pidx], [idx_objs[e]])

                def gather(e):
                    for g in range(4):
                        col = e * 4 + g
                        CP(idxg[g][:, 0:1], idxf[:, col:col + 1], [idx_objs[e]], [idxg[g]])
                        op("pool", lambda en, g=g: en.indirect_dma_start(
                            out=xes[g][:], out_offset=None, in_=hf_d.ap,
                            in_offset=bass.IndirectOffsetOnAxis(ap=idxg[g][:, 0:1], axis=0),
                            bounds_check=regs["bc"], oob_is_err=False), [hf_d, idxg[g]], [xes[g]], dma=True, sem_obj=xes[g])
                        op("pool", lambda en, g=g: en.indirect_dma_start(
                            out=gts[g][:], out_offset=None, in_=aff_d.ap,
                            in_offset=bass.IndirectOffsetOnAxis(ap=idxg[g][:, 0:1], axis=0),
                            bounds_check=regs["bc"], oob_is_err=False), [aff_d, idxg[g]], [gts[g]], dma=True, sem_obj=gts[g])

                def build_idx(e):
                    for _ in build_idx_gen(e):
                        pass

                build_idx(0)
                pending = {}
                for c in range(3):
                    pending[(0, c)] = load_w13(0, c)
                for g in range(4):
                    MS(xes[g][:], 0.0, [xes[g]])
                    MS(gts[g][:], 0.0, [gts[g]])
                gather(0)
                load_w2(0)
                hi_ = 0
                for e in range(NE):
                    xT = xeT[e % 2]
                    gen = build_idx_gen(e + 1) if e + 1 < NE else iter(())
                    gate_cols = []
                    for g in range(4):
                        for k in range(8):
                            TR(pT7[:, k, :], xes[g][:, k * 128:(k + 1) * 128], ident_b[:], [xes[g], ident_b], [pT7])
                        ACT(xT[:, :, g * 128:(g + 1) * 128], pT7[:], AF.Copy, [pT7], [xT])
                    for c in range(4):
                        bi = pending.pop((e, c))
                        for fcl in range(4):
                            fc = c * 4 + fcl
                            p1 = ph[hi_ % 4]; hi_ += 1
                            p3 = ph[hi_ % 4]; hi_ += 1
                            for k in range(8):
                                MM(p1[:], w1c[bi][:, k, fcl * 128:(fcl + 1) * 128], xT[:, k, :], [w1c[bi], xT], [p1],
                                   start=(k == 0), stop=(k == 7))
                            for k in range(8):
                                MM(p3[:], w3c[bi][:, k, fcl * 128:(fcl + 1) * 128], xT[:, k, :], [w3c[bi], xT], [p3],
                                   start=(k == 0), stop=(k == 7))
                            sb = stmp[fc % 2]
                            ACT(sb[:], p1[:], AF.Silu, [p1], [sb])
                            TT(hidT[:, fc, :], sb[:], p3[:], ALU.mult, [sb, p3], [hidT])
                            next(gen, None)
                        if c == 0:
                            pending[(e, 3)] = load_w13(e, 3)
                        elif e + 1 < NE:
                            pending[(e + 1, c - 1)] = load_w13(e + 1, c - 1)
                    for _ in gen:
                        pass
                    for g in range(4):
                        yb = ye[g % 2]
                        for n in range(2):
                            pyb = py[(g * 2 + n) % 2]
                            for fc in range(16):
                                MM(pyb[:], hidT[:, fc, g * 128:(g + 1) * 128], w2c[fc // 4][:, fc % 4, n * 512:(n + 1) * 512],
                                   [hidT, w2c[fc // 4]], [pyb], start=(fc == 0), stop=(fc == 15))
                            ACT(yb[:, n * 512:(n + 1) * 512], pyb[:], AF.Copy, [pyb, gts[g]], [yb], scale=gts[g][:, e:e + 1])
                        col = e * 4 + g
                        CP(idxs[g][:, 0:1], idxf[:, col:col + 1], [idx_objs[e]], [idxs[g]])
                        op("pool", lambda en, yb=yb, g=g: en.indirect_dma_start(
                            out=out_d.ap, out_offset=bass.IndirectOffsetOnAxis(ap=idxs[g][:, 0:1], axis=0),
                            in_=yb[:], in_offset=None, bounds_check=regs["bc"], oob_is_err=False, compute_op=ALU.add),
                            [yb, idxs[g]], out_tiles, dma=True, sem_obj=yb)
                    if e + 1 < NE:
                        gather(e + 1)
                        load_w2(e + 1)
                if debug and l == 0:
                    fw.dma("sp", dbg_idx.ap, idxf[:], idx_objs, [dbg_idx], idxf)
            fw.barrier()


def _natab(rpb):
    L = rpb.shape[0]
    pad = np.concatenate([rpb.reshape(L, 4, 15 * 31), np.full((L, 4, 1), -30000.0, np.float32)], axis=2)
    idx = np.zeros((5, 128, 5, 128), np.int64)
    k = np.arange(128)[:, None, None]
    dl = np.arange(5)[None, :, None]
    q = np.arange(128)[None, None, :]
    for p, j in enumerate([0, 1, 2, 30, 31]):
        kb = min(max(j - 2, 0), 27)
        key = (kb + dl) * 128 + k
        qq = j * 128 + q
        rk, ck = key // 64, key % 64
        rq, cq = qq // 64, qq % 64
        rs = np.clip(rq - 4, 0, 56)
        cs = np.clip(cq - 8, 0, 48)
        ok = (rk >= rs) & (rk < rs + 8) & (ck >= cs) & (ck < cs + 16)
        ii = (rk - rq + 7) * 31 + (ck - cq + 15)
        idx[p] = np.where(ok, ii, 465)
    tab = pad[:, :, idx]
    tab = np.ascontiguousarray(tab.transpose(0, 2, 3, 1, 4, 5)).reshape(L, 5, 128, 2560)
    return tab.astype(np.float32)


def _layer_inputs(inp, ls):
    f = lambda a: np.ascontiguousarray(a, dtype=np.float32)
    L = len(ls)
    sel = lambda k: f(np.asarray(inp[k])[ls])
    convw = sel("conv_qk").transpose(0, 2, 1).reshape(L, 4, 128, 5).transpose(0, 2, 1, 3)
    gc = np.stack([np.tile(sel(k), (1, 2)) for k in ("na_gq", "na_gk", "mem_gq", "mem_gk")], axis=2)
    return {
        "g_mix": sel("g_mix"), "w_in": sel("w_in"), "b_gates": sel("b_gates"), "convw": f(convw),
        "g_head": sel("g_mlstm_head"), "gcols": f(gc), "natab": _natab(sel("na_rpb")),
        "g_mem": sel("g_mem"), "w_mem_kv": sel("w_mem_kv"), "w_out": sel("w_out"), "g_ffn": sel("g_ffn"),
        "w_router": sel("w_router"), "w1": sel("w1"), "w3": sel("w3"), "w2": sel("w2"),
    }


_CACHE = {}
N_FUSED_LAYERS = 4


def kernel(**inputs):
    x = np.ascontiguousarray(inputs["x"], dtype=np.float32)
    mem = np.ascontiguousarray(inputs["mem"], dtype=np.float32)
    depth = np.asarray(inputs["g_mix"]).shape[0]
    nl = N_FUSED_LAYERS
    if nl not in _CACHE:
        _CACHE[nl] = build(nl)[0]
    nc = _CACHE[nl]
    cur = x
    for l0 in range(0, depth, nl):
        shared = _layer_inputs(inputs, list(range(l0, l0 + nl)))
        in_maps = []
        for c in range(8):
            m = dict(shared)
            m["x"] = cur[c]
            m["mem"] = mem[c]
            in_maps.append(m)
        res = run_bass_kernel_spmd(nc, in_maps, core_ids=list(range(8)))
        cur = np.stack([np.asarray(r["out"]) for r in res.results], axis=0).astype(np.float32)
    return cur
```

```python
import contextlib
import numpy as np
import concourse.bass as bass
import concourse.mybir as mybir
from concourse.bass_utils import run_bass_kernel_spmd

F32 = mybir.dt.float32
BF16 = mybir.dt.bfloat16
I32 = mybir.dt.int32
ALU = mybir.AluOpType
AF = mybir.ActivationFunctionType
AX = mybir.AxisListType

ENGS = ("pe", "act", "dve", "pool", "sp")

S = 4096
D = 1024
NT = 32
DIN = 2576
NE = 16
CAP = 512
DFF = 2048
EPS = 1e-6
LN8 = float(np.log(8.0))


class Obj:
    __slots__ = ("name", "ap", "last_write", "readers")

    def __init__(self, name, ap):
        self.name = name
        self.ap = ap
        self.last_write = None
        self.readers = []

    def __getitem__(self, k):
        return self.ap[k]


class Op:
    __slots__ = ("eng", "fn", "deps", "is_dma", "sem", "sobj", "value", "signal", "waits", "epoch", "release")

    def __init__(self, eng, fn, deps, is_dma):
        self.eng = eng
        self.fn = fn
        self.deps = deps
        self.is_dma = is_dma
        self.sem = None
        self.sobj = None
        self.value = None
        self.signal = False
        self.waits = []
        self.epoch = 0
        self.release = False


class StopBuild(Exception):
    pass


_dram_objs = []
_dram_inputs = set()


class FW:
    def __init__(self, nc):
        self.nc = nc
        self.stack = contextlib.ExitStack()
        self.scopes = [self.stack]
        self.ops = []
        self.eng_ops = {e: [] for e in ENGS}
        self.last_eng_op = {e: None for e in ENGS}
        self.dma_since_barrier = []
        self.uid = 0

    @contextlib.contextmanager
    def scope(self):
        st = contextlib.ExitStack()
        self.scopes.append(st)
        try:
            yield
        finally:
            self.scopes.pop()
            st.close()

    def _nm(self, name):
        self.uid += 1
        return "%s_%d" % (name, self.uid)

    def sbuf(self, name, shape, dtype):
        t = self.scopes[-1].enter_context(self.nc.sbuf_tensor(self._nm(name), list(shape), dtype))
        return Obj(name, t)

    def psum(self, name, shape, dtype=F32):
        t = self.scopes[-1].enter_context(self.nc.psum_tensor(self._nm(name), list(shape), dtype))
        return Obj(name, t)

    def dram(self, name, shape, dtype, kind="Internal"):
        t = self.nc.dram_tensor(name, list(shape), dtype, kind=kind)
        o = Obj(name, t.ap())
        if kind != "Internal":
            _dram_objs.append(o)
            if kind == "ExternalInput":
                _dram_inputs.add(name)
        return o

    def op(self, eng, fn, reads=(), writes=(), dma=False, sem_obj=None, extra_deps=()):
        deps = list(extra_deps)
        for o in reads:
            if o.last_write is not None:
                deps.append(o.last_write)
        for o in writes:
            if o.last_write is not None:
                deps.append(o.last_write)
            deps.extend(o.readers)
        op = Op(eng, fn, deps, dma)
        op.epoch = getattr(self, "epoch", 0)
        if dma:
            assert sem_obj is not None
            op.sobj = sem_obj
            self.dma_since_barrier.append(op)
        for o in writes:
            o.last_write = op
            o.readers = []
        for o in reads:
            o.readers.append(op)
        self.ops.append(op)
        self.eng_ops[eng].append(op)
        self.last_eng_op[eng] = op
        lim = getattr(self, "op_limit", 0)
        if lim and len(self.ops) == lim:
            self.op_limit = 0
            raise StopBuild()
        return op

    def dma(self, eng, out_ap, in_ap, reads, writes, sem_obj, **kw):
        return self.op(eng, lambda e: e.dma_start(out=out_ap, in_=in_ap, **kw), reads=reads, writes=writes,
                       dma=True, sem_obj=sem_obj)

    def barrier(self):
        self.nbar = getattr(self, "nbar", 0) + 1
        self._barrier()
        if self.nbar == getattr(self, "stop_at", -1):
            raise StopBuild()

    def _barrier(self):
        tails = [o for o in self.last_eng_op.values() if o is not None]
        seen = {}
        for o in self.dma_since_barrier:
            seen[id(o.sobj)] = o
        deps = tails + list(seen.values())
        self.dma_since_barrier = []
        last = None
        for e in ENGS:
            last = self.op(e, lambda en: en.nop(nofuse=True), extra_deps=deps)
        last.release = True
        self.epoch = getattr(self, "epoch", 0) + 1
        self.last_eng_op = {e: None for e in ENGS}

    def emit(self):
        nc = self.nc
        for op in self.ops:
            for d in op.deps:
                if d.eng == "pe" and op.eng == "pe" and not d.is_dma:
                    continue
                d.signal = True
        eng_sem = {}
        for e in ENGS:
            eng_sem[e] = self.stack.enter_context(nc.semaphore("sem_" + e))
        phys = []
        free = []
        cur_map = {}
        cur_epoch = 0
        eng_cnt = {e: 0 for e in ENGS}
        known = {e: {} for e in ENGS}
        for op in self.ops:
            w = {}
            for d in op.deps:
                if d.eng == "pe" and op.eng == "pe" and not d.is_dma:
                    continue
                if d.is_dma:
                    if d.epoch < cur_epoch:
                        continue
                    rec = phys[cur_map[id(d.sobj)]]
                    s, v = rec[0], rec[1]
                else:
                    s, v = eng_sem[d.eng], d.value
                key = id(s)
                if known[op.eng].get(key, 0) >= v:
                    continue
                if key not in w or w[key][1] < v:
                    w[key] = (s, v)
            for key, (s, v) in w.items():
                known[op.eng][key] = v
            op.waits = list(w.values())
            if op.is_dma:
                k = id(op.sobj)
                if k not in cur_map:
                    if free:
                        cur_map[k] = free.pop()
                    else:
                        phys.append([self.stack.enter_context(nc.semaphore("dsem_%d" % len(phys))), 0])
                        cur_map[k] = len(phys) - 1
                rec = phys[cur_map[k]]
                rec[1] += 16
                op.value = rec[1]
                op.sem = rec[0]
                op.signal = True
            elif op.signal:
                eng_cnt[op.eng] += 1
                op.value = eng_cnt[op.eng]
                op.sem = eng_sem[op.eng]
            if op.release:
                free.extend(cur_map.values())
                cur_map.clear()
                cur_epoch += 1
        final_dma = [(rec[0], rec[1]) for rec in phys]
        self.n_sems = len(phys) + len(ENGS)
        self.n_ops = len(self.ops)

        def run(eng_name, e):
            for op in self.eng_ops[eng_name]:
                for (s, v) in op.waits:
                    e.wait_ge(s, v)
                ins = op.fn(e)
                if op.signal:
                    ins.then_inc(op.sem, 16 if op.is_dma else 1)
            if eng_name == "sp":
                for (s, v) in final_dma:
                    if v > 0:
                        e.wait_ge(s, v)

        with nc.Block() as block:
            @block.tensor
            def _(e):
                run("pe", e)

            @block.scalar
            def _(e):
                run("act", e)

            @block.vector
            def _(e):
                run("dve", e)

            @block.gpsimd
            def _(e):
                run("pool", e)

            @block.sync
            def _(e):
                run("sp", e)
        self.stack.close()


def build(nl, debug=False, stop_at=-1, op_limit=0, ne_decl=NE):
    nc = bass.Bass("TRN2", target_bir_lowering=False)
    del _dram_objs[:]
    _dram_inputs.clear()
    fw = FW(nc)
    fw.stop_at = stop_at
    fw.op_limit = op_limit
    op = fw.op

    def MM(out, lhsT, rhs, R, W, start=True, stop=True):
        op("pe", lambda e: e.matmul(out, lhsT=lhsT, rhs=rhs, start=start, stop=stop), R, W)

    def TR(out, in_, ident, R, W):
        op("pe", lambda e: e.transpose(out=out, in_=in_, identity=ident), R, W)

    def ACT(out, in_, func, R, W, **kw):
        op("act", lambda e: e.activation(out=out, in_=in_, func=func, **kw), R, W)

    def TT(out, in0, in1, alu, R, W, eng="dve"):
        op(eng, lambda e: e.tensor_tensor(out=out, in0=in0, in1=in1, op=alu), R, W)

    def TS(out, in0, s1, s2, op0, op1, R, W, eng="dve", **kw):
        if op1 is None:
            op(eng, lambda e: e.tensor_scalar(out=out, in0=in0, scalar1=s1, scalar2=s2, op0=op0, **kw), R, W)
        else:
            op(eng, lambda e: e.tensor_scalar(out=out, in0=in0, scalar1=s1, scalar2=s2, op0=op0, op1=op1, **kw), R, W)

    def STT(out, in0, scalar, in1, op0, op1, R, W, eng="dve"):
        op(eng, lambda e: e.scalar_tensor_tensor(out=out, in0=in0, scalar=scalar, in1=in1, op0=op0, op1=op1), R, W)

    def CP(out, in_, R, W, eng="dve"):
        op(eng, lambda e: e.tensor_copy(out=out, in_=in_), R, W)

    def MS(ap, val, W, eng="dve"):
        op(eng, lambda e: e.memset(ap, val), (), W)

    def RED(out, in_, alu, R, W):
        op("dve", lambda e: e.tensor_reduce(out=out, in_=in_, axis=AX.X, op=alu), R, W)

    def RCP(out, in_, R, W):
        op("dve", lambda e: e.reciprocal(out=out, in_=in_), R, W)

    def rstd_from_ss(ss_obj, ss_ap, tmp_ap, out_ap, inv_n):
        ACT(tmp_ap, ss_ap, AF.Sqrt, [ss_obj], [ss_obj], scale=float(inv_n), bias=float(EPS))
        RCP(out_ap, tmp_ap, [ss_obj], [ss_obj])

    def din(name, shape, dt=F32):
        return fw.dram(name, shape, dt, kind="ExternalInput")

    x_in = din("x", [S, D])
    mem_in = din("mem", [256, D])
    g_mix = din("g_mix", [nl, D])
    w_in = din("w_in", [nl, D, DIN])
    b_gates = din("b_gates", [nl, 16])
    convw_d = din("convw", [nl, 128, 4, 5])
    g_head = din("g_head", [nl, 512])
    gcols_d = din("gcols", [nl, 128, 4])
    natab = din("natab", [nl, 5, 128, 2560])
    g_mem = din("g_mem", [nl, D])
    w_mkv = din("w_mem_kv", [nl, D, 512])
    w_out = din("w_out", [nl, D, D])
    g_ffn = din("g_ffn", [nl, D])
    w_rt = din("w_router", [nl, D, NE])
    w1 = din("w1", [nl, ne_decl, D, DFF])
    w3 = din("w3", [nl, ne_decl, D, DFF])
    w2 = din("w2", [nl, ne_decl, DFF, D])
    out_d = fw.dram("out", [S, D], F32, kind="ExternalOutput")
    dk = "ExternalOutput"
    qkraw_d = fw.dram("qkraw_d", [512, S], F32, kind=dk)
    so_d = fw.dram("so_d", [S, 512], BF16, kind=dk)
    v_d = fw.dram("v_d", [S, 512], BF16, kind=dk)
    hf_d = fw.dram("hf_d", [S, D], BF16, kind=dk)
    aff_d = fw.dram("aff_d", [S, NE], F32, kind=dk)
    if debug:
        dbg_xmix = fw.dram("dbg_xmix", [S, D], F32, kind=dk)
        dbg_yTm = fw.dram("dbg_yTm", [128, 4, S], BF16, kind=dk)
        dbg_yTnc = fw.dram("dbg_yTnc", [128, 4, S], BF16, kind=dk)
        dbg_qkT = fw.dram("dbg_qkT", [128, 4, S], BF16, kind=dk)
        dbg_idx = fw.dram("dbg_idx", [128, NE * 4], I32, kind=dk)
        dbg_nqT = fw.dram("dbg_nqT", [128, 2, S], BF16, kind=dk)
        dbg_gates = fw.dram("dbg_gates", [128, NT, 16], F32, kind=dk)
    out_tiles = [Obj("out_t%d" % t, None) for t in range(NT)]

    ident_f = fw.sbuf("ident_f", [128, 128], F32)
    ident_b = fw.sbuf("ident_b", [128, 128], BF16)
    io = fw.sbuf("io", [128, 128], F32)
    triU = fw.sbuf("triU", [128, 128], F32)
    triL = fw.sbuf("triL", [128, 128], F32)
    ones_f = fw.sbuf("ones_f", [128, 128], F32)
    ones_b = fw.sbuf("ones_b", [128, 128], BF16)
    iota512 = fw.sbuf("iota512", [128, 512], F32)
    regs = {}

    def _mkreg(e):
        regs["bc"] = e.alloc_register("bcreg")
        return e.reg_mov(regs["bc"], S - 1)
    op("pool", _mkreg, (), ())
    op("pool", lambda e: e.iota(io[:], pattern=[[1, 128]], base=0, channel_multiplier=-1,
                                allow_small_or_imprecise_dtypes=True), (), [io])
    op("pool", lambda e: e.iota(iota512[:], pattern=[[1, 512]], base=0, channel_multiplier=0,
                                allow_small_or_imprecise_dtypes=True), (), [iota512])
    op("dve", lambda e: e.tensor_single_scalar(out=ident_f[:], in_=io[:], scalar=0.0, op=ALU.is_equal), [io], [ident_f])
    CP(ident_b[:], ident_f[:], [ident_f], [ident_b])
    op("dve", lambda e: e.tensor_single_scalar(out=triU[:], in_=io[:], scalar=0.0, op=ALU.is_ge), [io], [triU])
    op("dve", lambda e: e.tensor_single_scalar(out=triL[:], in_=io[:], scalar=0.0, op=ALU.is_le), [io], [triL])
    MS(ones_f[:], 1.0, [ones_f])
    MS(ones_b[:], 1.0, [ones_b])
    try:
        fw.barrier()
        _layers(nl, debug, fw, locals())
    except StopBuild:
        while len(fw.scopes) > 1:
            fw.scopes.pop().close()
        scr = fw.sbuf("scr", [1, 64], F32)
        scrb = fw.sbuf("scrb", [1, 64], BF16)
        scri = fw.sbuf("scri", [1, 64], I32)
        for nm, o in list(locals().items()):
            if isinstance(o, Obj) and o.ap is not None and hasattr(o.ap, "shape") and "dram" in str(type(o.ap.tensor if hasattr(o.ap, "tensor") else "")).lower():
                pass
        for o in _dram_objs:
            ap = o.ap
            ix = tuple([0] * (len(ap.shape) - 2)) + (slice(0, 1), slice(0, 1))
            t = {F32: scr, BF16: scrb, I32: scri}[ap.dtype]
            if o.name in _dram_inputs:
                fw.dma("sp", t[0:1, 0:1], ap[ix], [o], [t], t)
            else:
                fw.dma("sp", ap[ix], t[0:1, 0:1], [t], [o], t)
    fw.emit()
    return nc, fw


def _layers(nl, debug, fw, env):
    globals().update({k: v for k, v in env.items() if k not in ("nl", "debug", "fw", "env")})
    op = fw.op
    for l in range(nl):
        x_src = x_in if l == 0 else out_d
        with fw.scope():
            kmT = fw.sbuf("kmT", [128, 2, 256], BF16)
            vm = fw.sbuf("vm", [128, 2, 4, 65], BF16)
            gates = fw.sbuf("gates", [128, NT, 16], F32)
            aff_all = fw.sbuf("aff_all", [128, NT, NE], F32)
            cnt_all = fw.sbuf("cnt_all", [128, NT, NE], F32)
            gcols = fw.sbuf("gcols", [128, 4], F32)
            fw.dma("sp", gcols[:], gcols_d[l], [gcols_d], [gcols], gcols)

            with fw.scope():
                yT_nc = fw.sbuf("yT_nc", [128, 4, S], BF16)
                with fw.scope():
                    nqT = fw.sbuf("nqT", [128, 2, S], BF16)
                    nkT = fw.sbuf("nkT", [128, 2, S], BF16)
                    cqT = fw.sbuf("cqT", [128, 2, S], BF16)
                    nv = fw.sbuf("nv", [128, NT, 4, 65], BF16)
                    MS(nv[:, :, :, 64:65], 1.0, [nv])
                    MS(vm[:, :, :, 64:65], 1.0, [vm])

                    with fw.scope():
                        gmem_b = fw.sbuf("gmem_b", [128, D], F32)
                        wkv = fw.sbuf("wkv", [128, 8, 512], BF16)
                        mt = [fw.sbuf("mt%d" % i, [128, D], F32) for i in range(2)]
                        mjunk = fw.sbuf("mjunk", [128, D], F32)
                        mn = fw.sbuf("mn", [128, D], BF16)
                        memT = fw.sbuf("memT", [128, 8, 256], BF16)
                        mst = fw.sbuf("mst", [128, 8], F32)
                        msq = fw.sbuf("msq", [128, 256], F32)
                        mss = fw.sbuf("mss", [128, 8], F32)
                        kmn = fw.sbuf("kmn", [128, 4, 64], BF16)
                        pT0 = fw.psum("pT0", [128, 8, 128], BF16)
                        pkv = fw.psum("pkv", [128, 512], F32)
                        fw.dma("sp", gmem_b[:], g_mem[l:l + 1, :].partition_broadcast(128), [g_mem], [gmem_b], gmem_b)
                        fw.dma("pool", wkv[:], w_mkv[l].rearrange("(k p) n -> p k n", p=128), [w_mkv], [wkv], wkv)
                        for i in range(2):
                            fw.dma("sp", mt[i][:], mem_in[i * 128:(i + 1) * 128, :], [mem_in], [mt[i]], mt[i])
                            MS(mst[:, 0:1], 0.0, [mst])
                            ACT(mjunk[:], mt[i][:], AF.Square, [mt[i]], [mjunk, mst], accum_out=mst[:, 0:1])
                            rstd_from_ss(mst, mst[:, 0:1], mst[:, 1:2], mst[:, 2:3], 1.0 / D)
                            STT(mn[:], mt[i][:], mst[:, 2:3], gmem_b[:], ALU.mult, ALU.mult, [mt[i], mst, gmem_b], [mn])
                            for k in range(8):
                                TR(pT0[:, k, :], mn[:, k * 128:(k + 1) * 128], ident_b[:], [mn, ident_b], [pT0])
                            ACT(memT[:, :, i * 128:(i + 1) * 128], pT0[:], AF.Copy, [pT0], [memT])
                        for i in range(2):
                            for k in range(8):
                                MM(pkv[:], memT[:, k, i * 128:(i + 1) * 128], wkv[:, k, :], [memT, wkv], [pkv],
                                   start=(k == 0), stop=(k == 7))
                            CP(vm[:, i, :, 0:64], pkv[:, 256:512].rearrange("p (h d) -> p h d", d=64), [pkv], [vm])
                            MS(mss[:, 0:4], 0.0, [mss])
                            for hh in range(4):
                                ACT(msq[:, hh * 64:(hh + 1) * 64], pkv[:, hh * 64:(hh + 1) * 64], AF.Square, [pkv], [msq, mss],
                                    accum_out=mss[:, hh:hh + 1])
                            rstd_from_ss(mss, mss[:, 0:4], mss[:, 4:8], mss[:, 4:8], 1.0 / 64)
                            TT(kmn[:], pkv[:, 0:256].rearrange("p (h d) -> p h d", d=64),
                               mss[:, 4:8].unsqueeze(2).to_broadcast([128, 4, 64]), ALU.mult, [pkv, mss], [kmn])
                            for hp in range(2):
                                TR(pT0[:, hp, :], kmn[:, 2 * hp:2 * hp + 2, :].rearrange("p h d -> p (h d)"), ident_b[:],
                                   [kmn, ident_b], [pT0])
                            ACT(kmT[:, :, i * 128:(i + 1) * 128], pT0[:, 0:2, :], AF.Copy, [pT0, gcols], [kmT],
                                scale=gcols[:, 3:4])
                    fw.barrier()

                    with fw.scope():
                        wi = fw.sbuf("wi", [128, 8, DIN], BF16)
                        gmix_b = fw.sbuf("gmix_b", [128, D], F32)
                        bg_b = fw.sbuf("bg_b", [128, 16], F32)
                        xt = [fw.sbuf("xt%d" % i, [128, D], F32) for i in range(2)]
                        xjunk = fw.sbuf("xjunk", [128, D], F32)
                        xn = [fw.sbuf("xn%d" % i, [128, D], BF16) for i in range(2)]
                        hT = [fw.sbuf("hT%d" % i, [128, 8, 512], BF16) for i in range(2)]
                        xst = [fw.sbuf("xst%d" % i, [128, 4], F32) for i in range(2)]
                        qkst = [fw.sbuf("qkst%d" % i, [128, 512], F32) for i in range(2)]
                        sot = [fw.sbuf("sot%d" % i, [128, 512], BF16) for i in range(2)]
                        vt = [fw.sbuf("vt%d" % i, [128, 512], BF16) for i in range(2)]
                        nsq = [fw.sbuf("nsq%d" % i, [128, 512], F32) for i in range(2)]
                        nss = [fw.sbuf("nss%d" % i, [128, 24], F32) for i in range(2)]
                        nqn = [fw.sbuf("nqn%d" % i, [128, 512], BF16) for i in range(2)]
                        cqn = [fw.sbuf("cqn%d" % i, [128, 256], BF16) for i in range(2)]
                        pT1 = [fw.psum("pT1_%d" % i, [128, 8, 128], BF16) for i in range(2)]
                        pmm = [fw.psum("pmm%d" % i, [128, 512], F32) for i in range(4)]
                        pqk = [fw.psum("pqk%d" % i, [128, 512], F32) for i in range(2)]
                        fw.dma("pool", wi[:, 0:4, :], w_in[l, 0:512, :].rearrange("(k p) n -> p k n", p=128), [w_in], [wi], wi)
                        fw.dma("pool", wi[:, 4:8, :], w_in[l, 512:1024, :].rearrange("(k p) n -> p k n", p=128), [w_in], [wi], wi)
                        fw.dma("sp", gmix_b[:], g_mix[l:l + 1, :].partition_broadcast(128), [g_mix], [gmix_b], gmix_b)
                        fw.dma("sp", bg_b[:], b_gates[l:l + 1, :].partition_broadcast(128), [b_gates], [bg_b], bg_b)
                        mmi = [0]

                        def p1_prep(st):
                            hTs = hT[st % 2]
                            for tt in range(4):
                                t = st * 4 + tt
                                b = t % 2
                                fw.dma("sp", xt[b][:], x_src[t * 128:(t + 1) * 128, :], [out_tiles[t]], [xt[b]], xt[b])
                                MS(xst[b][:, 0:1], 0.0, [xst[b]])
                                ACT(xjunk[:], xt[b][:], AF.Square, [xt[b]], [xjunk, xst[b]], accum_out=xst[b][:, 0:1])
                                rstd_from_ss(xst[b], xst[b][:, 0:1], xst[b][:, 1:2], xst[b][:, 2:3], 1.0 / D)
                                STT(xn[b][:], xt[b][:], xst[b][:, 2:3], gmix_b[:], ALU.mult, ALU.mult,
                                    [xt[b], xst[b], gmix_b], [xn[b]])
                                for k in range(8):
                                    TR(pT1[b][:, k, :], xn[b][:, k * 128:(k + 1) * 128], ident_b[:], [xn[b], ident_b], [pT1[b]])
                                ACT(hTs[:, :, tt * 128:(tt + 1) * 128], pT1[b][:], AF.Copy, [pT1[b]], [hTs])
                        def p1_body(st):
                            hTs = hT[st % 2]
                            for c in range(4):
                                pq = pqk[c % 2]
                                for k in range(8):
                                    MM(pq[:], wi[:, k, c * 128:(c + 1) * 128], hTs[:, k, :], [wi, hTs], [pq],
                                       start=(k == 0), stop=(k == 7))
                                qs = qkst[c % 2]
                                CP(qs[:], pq[:], [pq], [qs])
                                fw.dma("sp", qkraw_d[c * 128:(c + 1) * 128, st * 512:(st + 1) * 512], qs[:], [qs], [qkraw_d], qs)
                            for tt in range(4):
                                t = st * 4 + tt
                                b = t % 2
                                lh = lambda k: hTs[:, k, tt * 128:(tt + 1) * 128]
                                pm = pmm[mmi[0] % 4]; mmi[0] += 1
                                for k in range(8):
                                    MM(pm[:], lh(k), wi[:, k, 512:1024], [hTs, wi], [pm], start=(k == 0), stop=(k == 7))
                                ACT(vt[b][:], pm[:], AF.Copy, [pm], [vt[b]])
                                fw.dma("sp", v_d[t * 128:(t + 1) * 128, :], vt[b][:], [vt[b]], [v_d], vt[b])
                                pm = pmm[mmi[0] % 4]; mmi[0] += 1
                                for k in range(8):
                                    MM(pm[:], lh(k), wi[:, k, 1024:1536], [hTs, wi], [pm], start=(k == 0), stop=(k == 7))
                                ACT(sot[b][:], pm[:], AF.Sigmoid, [pm], [sot[b]])
                                fw.dma("sp", so_d[t * 128:(t + 1) * 128, :], sot[b][:], [sot[b]], [so_d], sot[b])
                                pm = pmm[mmi[0] % 4]; mmi[0] += 1
                                for k in range(8):
                                    MM(pm[:], lh(k), wi[:, k, 1552:2064], [hTs, wi], [pm], start=(k == 0), stop=(k == 7))
                                MS(nss[b][:, 0:8], 0.0, [nss[b]])
                                for hh in range(8):
                                    ACT(nsq[b][:, hh * 64:(hh + 1) * 64], pm[:, hh * 64:(hh + 1) * 64], AF.Square, [pm], [nsq[b], nss[b]],
                                        accum_out=nss[b][:, hh:hh + 1])
                                rstd_from_ss(nss[b], nss[b][:, 0:8], nss[b][:, 8:16], nss[b][:, 8:16], 1.0 / 64)
                                TT(nqn[b][:].rearrange("p (h d) -> p h d", d=64), pm[:].rearrange("p (h d) -> p h d", d=64),
                                   nss[b][:, 8:16].unsqueeze(2).to_broadcast([128, 8, 64]), ALU.mult, [pm, nss[b]], [nqn[b]])
                                for j in range(4):
                                    TR(pT1[b][:, j, :], nqn[b][:, j * 128:(j + 1) * 128], ident_b[:], [nqn[b], ident_b], [pT1[b]])
                                ACT(nqT[:, :, t * 128:(t + 1) * 128], pT1[b][:, 0:2, :], AF.Copy, [pT1[b], gcols], [nqT],
                                    scale=gcols[:, 0:1])
                                ACT(nkT[:, :, t * 128:(t + 1) * 128], pT1[b][:, 2:4, :], AF.Copy, [pT1[b], gcols], [nkT],
                                    scale=gcols[:, 1:2])
                                pm = pmm[mmi[0] % 4]; mmi[0] += 1
                                for k in range(8):
                                    MM(pm[:], lh(k), wi[:, k, 2064:2576], [hTs, wi], [pm], start=(k == 0), stop=(k == 7))
                                CP(nv[:, t, :, 0:64], pm[:, 0:256].rearrange("p (h d) -> p h d", d=64), [pm], [nv])
                                MS(nss[b][:, 16:20], 0.0, [nss[b]])
                                for hh in range(4):
                                    ACT(nsq[b][:, hh * 64:(hh + 1) * 64], pm[:, 256 + hh * 64:256 + (hh + 1) * 64], AF.Square, [pm],
                                        [nsq[b], nss[b]], accum_out=nss[b][:, 16 + hh:17 + hh])
                                rstd_from_ss(nss[b], nss[b][:, 16:20], nss[b][:, 20:24], nss[b][:, 20:24], 1.0 / 64)
                                TT(cqn[b][:].rearrange("p (h d) -> p h d", d=64), pm[:, 256:512].rearrange("p (h d) -> p h d", d=64),
                                   nss[b][:, 20:24].unsqueeze(2).to_broadcast([128, 4, 64]), ALU.mult, [pm, nss[b]], [cqn[b]])
                                for j in range(2):
                                    TR(pT1[b][:, 4 + j, :], cqn[b][:, j * 128:(j + 1) * 128], ident_b[:], [cqn[b], ident_b], [pT1[b]])
                                ACT(cqT[:, :, t * 128:(t + 1) * 128], pT1[b][:, 4:6, :], AF.Copy, [pT1[b], gcols], [cqT],
                                    scale=gcols[:, 2:3])
                                pm = pmm[mmi[0] % 4]; mmi[0] += 1
                                for k in range(8):
                                    MM(pm[:, 0:16], lh(k), wi[:, k, 1536:1552], [hTs, wi], [pm], start=(k == 0), stop=(k == 7))
                                TT(gates[:, t, :], pm[:, 0:16], bg_b[:], ALU.add, [pm, bg_b], [gates])

                        p1_prep(0)
                        for st in range(8):
                            if st + 1 < 8:
                                p1_prep(st + 1)
                            p1_body(st)
                    fw.barrier()

                    if debug and l == 0:
                        fw.dma("sp", dbg_nqT.ap, nqT[:], [nqT], [dbg_nqT], nqT)
                        fw.barrier()
                    with fw.scope():
                        EB = [fw.sbuf("EB%d" % i, [128, 2560], BF16) for i in range(5)]
                        tabst = fw.sbuf("tabst", [128, 2560], F32)
                        Ena = [fw.sbuf("Ena%d" % i, [128, 640], BF16) for i in range(2)]
                        PTn = [fw.sbuf("PTn%d" % i, [128, 640], BF16) for i in range(2)]
                        Ec = [fw.sbuf("Ec%d" % i, [128, 256], BF16) for i in range(2)]
                        ycat = [fw.sbuf("ycat%d" % i, [128, 512], BF16) for i in range(2)]
                        rec = [fw.sbuf("rec%d" % i, [128, 8], F32) for i in range(2)]
                        ps_na = [fw.psum("ps_na%d" % i, [128, 1024], F32) for i in range(2)]
                        ps_c = fw.psum("ps_c", [128, 2, 256], F32)
                        po_na = fw.psum("po_na", [128, 4, 65], F32)
                        po_c = fw.psum("po_c", [128, 4, 65], F32)
                        pT3 = fw.psum("pT3", [128, 4, 128], BF16)
                        for p in range(5):
                            fw.dma("sp", tabst[:], natab[l, p], [natab], [tabst], tabst)
                            ACT(EB[p][:], tabst[:], AF.Exp, [tabst], [EB[p]])
                        ui = 0
                        for j in range(NT):
                            kb = min(max(j - 2, 0), 27)
                            pat = {0: 0, 1: 1, 30: 3, 31: 4}.get(j, 2)
                            yb = ycat[j % 2]
                            rb = rec[j % 2]
                            qs = slice(j * 128, (j + 1) * 128)
                            for h in range(4):
                                hp, lo = h // 2, (h % 2) * 64
                                pn = ps_na[ui % 2]
                                En = Ena[ui % 2]
                                Pn = PTn[ui % 2]
                                Ecb = Ec[ui % 2]
                                ui += 1
                                for dl in range(5):
                                    ks = slice((kb + dl) * 128, (kb + dl + 1) * 128)
                                    MM(pn[:, dl * 128:(dl + 1) * 128], nkT[lo:lo + 64, hp, ks], nqT[lo:lo + 64, hp, qs],
                                       [nkT, nqT], [pn])
                                ACT(En[:], pn[:, 0:640], AF.Exp, [pn], [En], scale=0.125)
                                TT(Pn[:], En[:], EB[pat][:, h * 640:(h + 1) * 640], ALU.mult, [En, EB[pat]], [Pn])
                                for dl in range(5):
                                    MM(po_na[:, h, :], Pn[:, dl * 128:(dl + 1) * 128], nv[:, kb + dl, h, :], [Pn, nv], [po_na],
                                       start=(dl == 0), stop=(dl == 4))
                                for i in range(2):
                                    MM(ps_c[:, h % 2, i * 128:(i + 1) * 128], kmT[lo:lo + 64, hp, i * 128:(i + 1) * 128],
                                       cqT[lo:lo + 64, hp, qs], [kmT, cqT], [ps_c])
                                ACT(Ecb[:], ps_c[:, h % 2, :], AF.Exp, [ps_c], [Ecb], scale=0.125)
                                for i in range(2):
                                    MM(po_c[:, h, :], Ecb[:, i * 128:(i + 1) * 128], vm[:, i, h, :], [Ecb, vm], [po_c],
                                       start=(i == 0), stop=(i == 1))
                            RCP(rb[:, 0:4], po_na[:, :, 64], [po_na], [rb])
                            TT(yb[:, 0:256].rearrange("p (h d) -> p h d", d=64), po_na[:, :, 0:64],
                               rb[:, 0:4].unsqueeze(2).to_broadcast([128, 4, 64]), ALU.mult, [po_na, rb], [yb])
                            RCP(rb[:, 4:8], po_c[:, :, 64], [po_c], [rb])
                            TT(yb[:, 256:512].rearrange("p (h d) -> p h d", d=64), po_c[:, :, 0:64],
                               rb[:, 4:8].unsqueeze(2).to_broadcast([128, 4, 64]), ALU.mult, [po_c, rb], [yb])
                            for k in range(4):
                                TR(pT3[:, k, :], yb[:, k * 128:(k + 1) * 128], ident_b[:], [yb, ident_b], [pT3])
                            ACT(yT_nc[:, :, qs], pT3[:], AF.Copy, [pT3], [yT_nc])
                    fw.barrier()

                with fw.scope():
                    yT_m = fw.sbuf("yT_m", [128, 4, S], BF16)
                    with fw.scope():
                        qkT = fw.sbuf("qkT", [128, 4, S], BF16)
                        with fw.scope():
                            convw = fw.sbuf("convw", [128, 4, 5], F32)
                            raw = [fw.sbuf("raw%d" % i, [128, 516], F32) for i in range(2)]
                            cacc = [fw.sbuf("cacc%d" % i, [128, 512], F32) for i in range(2)]
                            fw.dma("sp", convw[:], convw_d[l], [convw_d], [convw], convw)
                            ui = 0
                            for st in range(8):
                                for c in range(4):
                                    r = raw[ui % 2]
                                    a = cacc[ui % 2]
                                    ui += 1
                                    lo_t = st * 512 - 2
                                    hi_t = st * 512 + 514
                                    d0, d1 = 0, 516
                                    if st == 0:
                                        MS(r[:, 0:2], 0.0, [r])
                                        lo_t, d0 = 0, 2
                                    if st == 7:
                                        MS(r[:, 514:516], 0.0, [r])
                                        hi_t, d1 = S, 514
                                    fw.dma("sp", r[:, d0:d1], qkraw_d[c * 128:(c + 1) * 128, lo_t:hi_t], [qkraw_d], [r], r)
                                    TS(a[:], r[:, 0:512], convw[:, c, 0:1], None, ALU.mult, None, [r, convw], [a])
                                    for jj in range(1, 5):
                                        STT(a[:], r[:, jj:jj + 512], convw[:, c, jj:jj + 1], a[:], ALU.mult, ALU.add,
                                            [r, convw, a], [a])
                                    ACT(qkT[:, c, st * 512:(st + 1) * 512], a[:], AF.Silu, [a], [qkT])
                        fw.barrier()

                        if debug and l == 0:
                            fw.dma("sp", dbg_qkT.ap, qkT[:], [qkT], [dbg_qkT], qkT)
                            fw.dma("sp", dbg_gates.ap, gates[:], [gates], [dbg_gates], gates)
                            fw.barrier()
                        with fw.scope():
                            ghead_b = fw.sbuf("ghead_b", [128, 512], F32)
                            fw.dma("sp", ghead_b[:], g_head[l:l + 1, :].partition_broadcast(128), [g_head], [ghead_b], ghead_b)
                            LI = [fw.sbuf("LI%d" % d, [128, 128], F32) for d in range(2)]
                            LF = [fw.sbuf("LF%d" % d, [128, 128], F32) for d in range(2)]
                            Bc = [fw.sbuf("Bc%d" % d, [128, 128], F32) for d in range(2)]
                            Gc = [fw.sbuf("Gc%d" % d, [128, 128], F32) for d in range(2)]
                            Es = [fw.sbuf("Es%d" % d, [128, 128], F32) for d in range(2)]
                            Ws = [fw.sbuf("Ws%d" % d, [128, 128], F32) for d in range(2)]
                            EM = [fw.sbuf("EM%d" % d, [128, 128], F32) for d in range(2)]
                            EG = [fw.sbuf("EG%d" % d, [128, 128], F32) for d in range(2)]
                            gtmp = fw.sbuf("gtmp", [128, 128], F32)
                            pg = fw.psum("pg", [128, 2, 128], F32)
                            v3 = lambda o: o[:].rearrange("p (c h) -> p c h", h=4)
                            for d in range(2):
                                CP(v3(LI[d]), gates[:, :, 8 * d:8 * d + 4], [gates], [LI[d]])
                                ACT(v3(gtmp), gates[:, :, 8 * d + 4:8 * d + 8], AF.Exp, [gates], [gtmp], scale=-1.0)
                                ACT(gtmp[:], gtmp[:], AF.Ln, [gtmp], [gtmp], bias=1.0)
                                TS(LF[d][:], gtmp[:], -1.0, None, ALU.mult, None, [gtmp], [LF[d]])
                                MM(pg[:, 0, :], (triU if d == 0 else triL)[:], LF[d][:], [triU, triL, LF[d]], [pg])
                                MM(pg[:, 1, :], ones_f[:], LF[d][:], [ones_f, LF[d]], [pg])
                                CP(Bc[d][:], pg[:, 0, :], [pg], [Bc[d]])
                                CP(Gc[d][:], pg[:, 1, :], [pg], [Gc[d]])
                                TT(gtmp[:], LI[d][:], Bc[d][:], ALU.subtract, [LI[d], Bc[d]], [gtmp])
                                ACT(Es[d][:], gtmp[:], AF.Exp, [gtmp], [Es[d]])
                                TT(gtmp[:], gtmp[:], Gc[d][:], ALU.add, [gtmp, Gc[d]], [gtmp])
                                ACT(Ws[d][:], gtmp[:], AF.Exp, [gtmp], [Ws[d]])
                                ACT(EM[d][:], Bc[d][:], AF.Exp, [Bc[d]], [EM[d]], scale=-1.0, bias=LN8)
                                ACT(EG[d][:], Gc[d][:], AF.Exp, [Gc[d]], [EG[d]])
                            maskd = [triU, triL]

                            Cprev = [fw.sbuf("Cprev%d" % d, [128, NT, 129], BF16) for d in range(2)]
                            Cst = [fw.sbuf("Cst%d" % d, [128, 129], F32) for d in range(2)]
                            EGs = [fw.sbuf("EGs%d" % d, [128, NT], F32) for d in range(2)]
                            kwA = [fw.sbuf("kwA%d" % i, [128, 128], BF16) for i in range(4)]
                            kwB = [fw.sbuf("kwB%d" % i, [128, 128], BF16) for i in range(4)]
                            v1 = [fw.sbuf("v1_%d" % i, [128, 2, 129], BF16) for i in range(4)]
                            sob = [fw.sbuf("sob%d" % i, [128, 256], BF16) for i in range(2)]
                            PTm = [fw.sbuf("PTm%d" % i, [128, 2, 2, 128], BF16) for i in range(2)]
                            dn = [fw.sbuf("dn%d" % i, [128, 16], F32) for i in range(2)]
                            hs = [fw.sbuf("hs%d" % i, [128, 2, 128], F32) for i in range(2)]
                            hj = fw.sbuf("hj", [128, 128], F32)
                            ymb = [fw.sbuf("ymb%d" % i, [128, 256], BF16) for i in range(2)]
                            pTk_t = fw.psum("pTk", [128, 2, 128], BF16)
                            pTk = [Obj("pTk%d" % i, pTk_t[:, i, :]) for i in range(2)]
                            pkvm_t = fw.psum("pkvm", [128, 2, 129], F32)
                            pkvm = [Obj("pkvm%d" % i, pkvm_t[:, i, :]) for i in range(2)]
                            psm = fw.psum("psm", [128, 2, 512], F32)
                            pnd = fw.psum("pnd", [128, 2, 2, 256], F32)
                            pTy = fw.psum("pTy", [128, 2, 128], BF16)
                            for i in range(4):
                                MS(kwA[i][:, 64:128], 0.0, [kwA[i]])
                                MS(kwB[i][:, 0:64], 0.0, [kwB[i]])
                                MS(v1[i][:, :, 128:129], 1.0, [v1[i]])
                            for hp in range(2):
                                h0 = 2 * hp
                                for d in range(2):
                                    MS(Cst[d][:], 0.0, [Cst[d]])
                                    CP(EGs[d][0:64, :], v3(EG[d])[0:64, :, h0], [EG[d]], [EGs[d]])
                                    CP(EGs[d][64:128, :], v3(EG[d])[64:128, :, h0 + 1], [EG[d]], [EGs[d]])
                                ui = 0
                                for ci in range(NT):
                                    for d in range(2):
                                        c = ci if d == 0 else NT - 1 - ci
                                        cs = slice(c * 128, (c + 1) * 128)
                                        ka, kb_, vv, pk, pt = kwA[ui % 4], kwB[ui % 4], v1[ui % 4], pkvm[ui % 2], pTk[ui % 2]
                                        ui += 1
                                        ACT(Cprev[d][:, c, :], Cst[d][:], AF.Copy, [Cst[d]], [Cprev[d]])
                                        fw.dma("sp", vv[:, :, 0:128], v_d[cs, h0 * 128:(h0 + 2) * 128].rearrange("p (h d) -> p h d", d=128),
                                               [v_d], [vv], vv)
                                        TR(pt[:], qkT[:, 2 + hp, cs], ident_b[:], [qkT, ident_b], [pt])
                                        TS(ka[:, 0:64], pt[:, 0:64], Ws[d][:, c * 4 + h0:c * 4 + h0 + 1], None, ALU.mult, None,
                                           [pt, Ws[d]], [ka])
                                        TS(kb_[:, 64:128], pt[:, 64:128], Ws[d][:, c * 4 + h0 + 1:c * 4 + h0 + 2], None, ALU.mult, None,
                                           [pt, Ws[d]], [kb_])
                                        MM(pk[:], ka[:], vv[:, 0, :], [ka, vv], [pk], start=True, stop=False)
                                        MM(pk[:], kb_[:], vv[:, 1, :], [kb_, vv], [pk], start=False, stop=True)
                                        STT(Cst[d][:], Cst[d][:], EGs[d][:, c:c + 1], pk[:], ALU.mult, ALU.add,
                                            [Cst[d], EGs[d], pk], [Cst[d]])
                                for c in range(NT):
                                    cs = slice(c * 128, (c + 1) * 128)
                                    vv = v1[c % 4]
                                    sb = sob[c % 2]
                                    Pm = PTm[c % 2]
                                    dnb = dn[c % 2]
                                    hsb = hs[c % 2]
                                    yb = ymb[c % 2]
                                    fw.dma("sp", vv[:, :, 0:128], v_d[cs, h0 * 128:(h0 + 2) * 128].rearrange("p (h d) -> p h d", d=128),
                                           [v_d], [vv], vv)
                                    fw.dma("sp", sb[:], so_d[cs, h0 * 128:(h0 + 2) * 128], [so_d], [sb], sb)
                                    for i in range(2):
                                        lo = i * 64
                                        MM(psm[:, i, 0:128], qkT[lo:lo + 64, 2 + hp, cs], qkT[lo:lo + 64, hp, cs], [qkT], [psm])
                                    for d in range(2):
                                        for i in range(2):
                                            col = c * 4 + h0 + i
                                            STT(Pm[:, d, i, :], psm[:, i, 0:128], Es[d][:, col:col + 1], maskd[d][:], ALU.mult, ALU.mult,
                                                [psm, Es[d], maskd[d]], [Pm])
                                    for d in range(2):
                                        for i in range(2):
                                            lo = i * 64
                                            MM(pnd[:, d, i, 0:129], Pm[:, d, i, :], vv[:, i, :], [Pm, vv], [pnd], start=True, stop=False)
                                            MM(pnd[:, d, i, 0:129], qkT[lo:lo + 64, hp, cs], Cprev[d][lo:lo + 64, c, :],
                                               [qkT, Cprev[d]], [pnd], start=False, stop=True)
                                    for d in range(2):
                                        col = c * 4 + h0
                                        TT(dnb[:, 2 * d:2 * d + 2], pnd[:, d, :, 128], EM[d][:, col:col + 2], ALU.max,
                                           [pnd, EM[d]], [dnb])
                                        STT(dnb[:, 4 + 2 * d:6 + 2 * d], pnd[:, d, :, 128], -1.0, dnb[:, 2 * d:2 * d + 2],
                                            ALU.mult, ALU.max, [pnd, dnb], [dnb])
                                    RCP(dnb[:, 8:12], dnb[:, 4:8], [dnb], [dnb])
                                    for i in range(2):
                                        TS(hsb[:, i, :], pnd[:, 0, i, 0:128], dnb[:, 8 + i:9 + i], None, ALU.mult, None, [pnd, dnb], [hsb])
                                        STT(hsb[:, i, :], pnd[:, 1, i, 0:128], dnb[:, 10 + i:11 + i], hsb[:, i, :], ALU.mult, ALU.add,
                                            [pnd, dnb, hsb], [hsb])
                                        MS(dnb[:, 12 + i:13 + i], 0.0, [dnb])
                                        ACT(hj[:], hsb[:, i, :], AF.Square, [hsb], [hj, dnb], accum_out=dnb[:, 12 + i:13 + i])
                                    rstd_from_ss(dnb, dnb[:, 12:14], dnb[:, 14:16], dnb[:, 14:16], 1.0 / 128)
                                    for i in range(2):
                                        STT(hsb[:, i, :], hsb[:, i, :], dnb[:, 14 + i:15 + i], ghead_b[:, (h0 + i) * 128:(h0 + i + 1) * 128],
                                            ALU.mult, ALU.mult, [hsb, dnb, ghead_b], [hsb])
                                    TT(yb[:], hsb[:].rearrange("p i d -> p (i d)"), sb[:], ALU.mult, [hsb, sb], [yb])
                                    for i in range(2):
                                        TR(pTy[:, i, :], yb[:, i * 128:(i + 1) * 128], ident_b[:], [yb, ident_b], [pTy])
                                    ACT(yT_m[:, h0:h0 + 2, cs], pTy[:], AF.Copy, [pTy], [yT_m])
                        fw.barrier()

                    with fw.scope():
                        wo = fw.sbuf("wo", [128, 8, D], BF16)
                        wr = fw.sbuf("wr", [128, 8, NE], BF16)
                        gffn_b = fw.sbuf("gffn_b", [128, D], F32)
                        xt5 = [fw.sbuf("xt5_%d" % i, [128, D], F32) for i in range(2)]
                        xo = [fw.sbuf("xo%d" % i, [128, D], F32) for i in range(2)]
                        xj5 = fw.sbuf("xj5", [128, D], F32)
                        hf = [fw.sbuf("hf%d" % i, [128, D], BF16) for i in range(2)]
                        hfT = [fw.sbuf("hfT%d" % i, [128, 8, 128], BF16) for i in range(2)]
                        st5 = [fw.sbuf("st5_%d" % i, [128, 8], F32) for i in range(2)]
                        ex5 = [fw.sbuf("ex5_%d" % i, [128, NE], F32) for i in range(2)]
                        po5 = [fw.psum("po5_%d" % i, [128, D], F32) for i in range(2)]
                        pT5 = [fw.psum("pT5_%d" % i, [128, 8, 128], BF16) for i in range(2)]
                        plog = fw.psum("plog", [128, NE], F32)
                        fw.dma("pool", wo[:], w_out[l].rearrange("(k p) n -> p k n", p=128), [w_out], [wo], wo)
                        if debug and l == 0:
                            fw.dma("sp", dbg_yTm.ap, yT_m[:], [yT_m], [dbg_yTm], yT_m)
                            fw.dma("sp", dbg_yTnc.ap, yT_nc[:], [yT_nc], [dbg_yTnc], yT_nc)
                        fw.dma("pool", wr[:], w_rt[l].rearrange("(k p) n -> p k n", p=128), [w_rt], [wr], wr)
                        fw.dma("sp", gffn_b[:], g_ffn[l:l + 1, :].partition_broadcast(128), [g_ffn], [gffn_b], gffn_b)
                        def p5_mm(t):
                            b = t % 2
                            ts_ = slice(t * 128, (t + 1) * 128)
                            fw.dma("sp", xt5[b][:], x_src[ts_, :], [out_tiles[t]], [xt5[b]], xt5[b])
                            for n in range(2):
                                for k in range(8):
                                    src = yT_m if k < 4 else yT_nc
                                    MM(po5[b][:, n * 512:(n + 1) * 512], src[:, k % 4, ts_], wo[:, k, n * 512:(n + 1) * 512],
                                       [src, wo], [po5[b]], start=(k == 0), stop=(k == 7))

                        p5_mm(0)
                        for t in range(NT):
                            b = t % 2
                            ts_ = slice(t * 128, (t + 1) * 128)
                            if t + 1 < NT:
                                p5_mm(t + 1)
                            TT(xo[b][:], po5[b][:], xt5[b][:], ALU.add, [po5[b], xt5[b]], [xo[b]])
                            fw.dma("sp", out_d[ts_, :], xo[b][:], [xo[b]], [out_tiles[t]], xo[b])
                            if debug and l == 0:
                                fw.dma("sp", dbg_xmix[ts_, :], xo[b][:], [xo[b]], [dbg_xmix], xo[b])
                            MS(st5[b][:, 0:1], 0.0, [st5[b]])
                            ACT(xj5[:], xo[b][:], AF.Square, [xo[b]], [xj5, st5[b]], accum_out=st5[b][:, 0:1])
                            rstd_from_ss(st5[b], st5[b][:, 0:1], st5[b][:, 1:2], st5[b][:, 2:3], 1.0 / D)
                            STT(hf[b][:], xo[b][:], st5[b][:, 2:3], gffn_b[:], ALU.mult, ALU.mult, [xo[b], st5[b], gffn_b], [hf[b]])
                            fw.dma("sp", hf_d[ts_, :], hf[b][:], [hf[b]], [hf_d], hf[b])
                            for k in range(8):
                                TR(pT5[b][:, k, :], hf[b][:, k * 128:(k + 1) * 128], ident_b[:], [hf[b], ident_b], [pT5[b]])
                            ACT(hfT[b][:], pT5[b][:], AF.Copy, [pT5[b]], [hfT[b]])
                            for k in range(8):
                                MM(plog[:], hfT[b][:, k, :], wr[:, k, :], [hfT[b], wr], [plog], start=(k == 0), stop=(k == 7))
                            MS(st5[b][:, 5:6], 0.0, [st5[b]])
                            ACT(ex5[b][:], plog[:], AF.Exp, [plog, st5[b]], [ex5[b], st5[b]], accum_out=st5[b][:, 5:6])
                            RCP(st5[b][:, 6:7], st5[b][:, 5:6], [st5[b]], [st5[b]])
                            TS(aff_all[:, t, :], ex5[b][:], st5[b][:, 6:7], None, ALU.mult, None, [ex5[b], st5[b]], [aff_all])
                        fw.dma("sp", aff_d.ap.rearrange("(i p) e -> p i e", p=128), aff_all[:], [aff_all], [aff_d], aff_all)
                    fw.barrier()

            with fw.scope():
                affT = fw.sbuf("affT", [16, S], F32)
                rjunk = fw.sbuf("rjunk", [16, S], F32)
                rmask = fw.sbuf("rmask", [16, S], F32)
                rones = fw.sbuf("rones", [16, S], F32)
                rs = fw.sbuf("rs", [16, 8], F32)
                pTa = [fw.psum("pTa%d" % i, [16, 512], F32) for i in range(2)]
                pcn = fw.psum("pcn", [128, NT, NE], F32)
                for g4 in range(8):
                    pa = pTa[g4 % 2]
                    for i in range(4):
                        t = g4 * 4 + i
                        TR(pa[:, i * 128:(i + 1) * 128], aff_all[:, t, :], ident_f[:], [aff_all, ident_f], [pa])
                    CP(affT[:, g4 * 512:(g4 + 1) * 512], pa[:], [pa], [affT])
                MS(rones[:], 1.0, [rones])
                MS(rs[:, 0:1], 0.0, [rs])
                MS(rs[:, 1:2], 1.0, [rs])
                for it in range(28):
                    TT(rs[:, 2:3], rs[:, 0:1], rs[:, 1:2], ALU.add, [rs], [rs])
                    TS(rs[:, 2:3], rs[:, 2:3], 0.5, None, ALU.mult, None, [rs], [rs])
                    TS(rjunk[:], affT[:], rs[:, 2:3], 0.0, ALU.is_ge, ALU.add, [affT, rs], [rjunk, rs], accum_out=rs[:, 3:4])
                    TS(rs[:, 4:5], rs[:, 3:4], float(CAP), None, ALU.is_ge, None, [rs], [rs])
                    TT(rs[:, 5:6], rs[:, 2:3], rs[:, 0:1], ALU.subtract, [rs], [rs])
                    STT(rs[:, 0:1], rs[:, 5:6], rs[:, 4:5], rs[:, 0:1], ALU.mult, ALU.add, [rs], [rs])
                    TT(rs[:, 5:6], rs[:, 1:2], rs[:, 2:3], ALU.subtract, [rs], [rs])
                    STT(rs[:, 1:2], rs[:, 5:6], rs[:, 4:5], rs[:, 2:3], ALU.mult, ALU.add, [rs], [rs])
                TS(rmask[:], affT[:], rs[:, 0:1], None, ALU.is_ge, None, [affT, rs], [rmask])
                op("dve", lambda e: e.tensor_tensor_scan(out=rjunk[:], data0=rones[:], data1=rmask[:], initial=0.0,
                                                         op0=ALU.mult, op1=ALU.add), [rones, rmask], [rjunk])
                for t in range(NT):
                    TR(pcn[:, t, :], rjunk[:, t * 128:(t + 1) * 128], ident_f[0:16, 0:16], [rjunk, ident_f], [pcn])
                CP(cnt_all[:], pcn[:], [pcn], [cnt_all])
            fw.barrier()

            with fw.scope():
                NB13 = 3
                w1c = [fw.sbuf("w1c%d" % i, [128, 8, 512], BF16) for i in range(NB13)]
                w3c = [fw.sbuf("w3c%d" % i, [128, 8, 512], BF16) for i in range(NB13)]
                w2c = [fw.sbuf("w2c%d" % i, [128, 4, D], BF16) for i in range(4)]
                xes = [fw.sbuf("xes%d" % i, [128, D], BF16) for i in range(4)]
                gts = [fw.sbuf("gts%d" % i, [128, NE], F32) for i in range(4)]
                xeT = [fw.sbuf("xeT%d" % i, [128, 8, 512], BF16) for i in range(2)]
                hidT = fw.sbuf("hidT", [128, 16, 512], BF16)
                stmp = [fw.sbuf("stmp%d" % i, [128, 512], F32) for i in range(2)]
                ye = [fw.sbuf("ye%d" % i, [128, D], F32) for i in range(2)]
                idxf = fw.sbuf("idxf", [128, NE * 4], I32)
                cacc7 = [fw.sbuf("cacc7_%d" % i, [128, 512], BF16) for i in range(2)]
                pT7 = fw.psum("pT7", [128, 8, 128], BF16)
                ph = [fw.psum("ph%d" % i, [128, 512], F32) for i in range(4)]
                py = [fw.psum("py%d" % i, [128, 512], F32) for i in range(2)]
                pidx = fw.psum("pidx", [128, NE * 4], F32)
                idx_objs = [Obj("idx%d" % e, None) for e in range(NE)]
                idxg = [fw.sbuf("idxg%d" % i, [128, 1], I32) for i in range(4)]
                idxs = [fw.sbuf("idxs%d" % i, [128, 1], I32) for i in range(4)]
                w13_i = [0]

                def load_w13(e, c):
                    bi = w13_i[0] % NB13
                    w13_i[0] += 1
                    fw.dma("pool", w1c[bi][:], w1[l, e, :, c * 512:(c + 1) * 512].rearrange("(k p) f -> p k f", p=128),
                           [w1], [w1c[bi]], w1c[bi])
                    fw.dma("pool", w3c[bi][:], w3[l, e, :, c * 512:(c + 1) * 512].rearrange("(k p) f -> p k f", p=128),
                           [w3], [w3c[bi]], w3c[bi])
                    return bi

                def load_w2(e):
                    for c in range(4):
                        fw.dma("pool", w2c[c][:], w2[l, e, c * 512:(c + 1) * 512, :].rearrange("(c p) d -> p c d", p=128),
                               [w2], [w2c[c]], w2c[c])

                def build_idx_gen(e):
                    a = cacc7[e % 2]
                    MS(a[:], 0.0, [a])
                    for t in range(NT):
                        STT(a[:], iota512[:], cnt_all[:, t, e:e + 1], a[:], ALU.is_ge, ALU.add, [iota512, cnt_all, a], [a])
                        if t % 2 == 1:
                            yield
                    for g in range(4):
                        MM(pidx[:, e * 4 + g:e * 4 + g + 1], a[:, g * 128:(g + 1) * 128], ones_b[:, 0:1], [a, ones_b], [pidx])
                    CP(idxf[:, e * 4:(e + 1) * 4], pidx[:, e * 4:(e + 1) * 4], [pidx], [idx_objs[e]])

                def gather(e):
                    for g in range(4):
                        col = e * 4 + g
                        CP(idxg[g][:, 0:1], idxf[:, col:col + 1], [idx_objs[e]], [idxg[g]])
                        op("pool", lambda en, g=g: en.indirect_dma_start(
                            out=xes[g][:], out_offset=None, in_=hf_d.ap,
                            in_offset=bass.IndirectOffsetOnAxis(ap=idxg[g][:, 0:1], axis=0),
                            bounds_check=regs["bc"], oob_is_err=False), [hf_d, idxg[g]], [xes[g]], dma=True, sem_obj=xes[g])
                        op("pool", lambda en, g=g: en.indirect_dma_start(
                            out=gts[g][:], out_offset=None, in_=aff_d.ap,
                            in_offset=bass.IndirectOffsetOnAxis(ap=idxg[g][:, 0:1], axis=0),
                            bounds_check=regs["bc"], oob_is_err=False), [aff_d, idxg[g]], [gts[g]], dma=True, sem_obj=gts[g])

                def build_idx(e):
                    for _ in build_idx_gen(e):
                        pass

                build_idx(0)
                pending = {}
                for c in range(3):
                    pending[(0, c)] = load_w13(0, c)
                for g in range(4):
                    MS(xes[g][:], 0.0, [xes[g]])
                    MS(gts[g][:], 0.0, [gts[g]])
                gather(0)
                load_w2(0)
                hi_ = 0
                for e in range(NE):
                    xT = xeT[e % 2]
                    gen = build_idx_gen(e + 1) if e + 1 < NE else iter(())
                    gate_cols = []
                    for g in range(4):
                        for k in range(8):
                            TR(pT7[:, k, :], xes[g][:, k * 128:(k + 1) * 128], ident_b[:], [xes[g], ident_b], [pT7])
                        ACT(xT[:, :, g * 128:(g + 1) * 128], pT7[:], AF.Copy, [pT7], [xT])
                    for c in range(4):
                        bi = pending.pop((e, c))
                        for fcl in range(4):
                            fc = c * 4 + fcl
                            p1 = ph[hi_ % 4]; hi_ += 1
                            p3 = ph[hi_ % 4]; hi_ += 1
                            for k in range(8):
                                MM(p1[:], w1c[bi][:, k, fcl * 128:(fcl + 1) * 128], xT[:, k, :], [w1c[bi], xT], [p1],
                                   start=(k == 0), stop=(k == 7))
                            for k in range(8):
                                MM(p3[:], w3c[bi][:, k, fcl * 128:(fcl + 1) * 128], xT[:, k, :], [w3c[bi], xT], [p3],
                                   start=(k == 0), stop=(k == 7))
                            sb = stmp[fc % 2]
                            ACT(sb[:], p1[:], AF.Silu, [p1], [sb])
                            TT(hidT[:, fc, :], sb[:], p3[:], ALU.mult, [sb, p3], [hidT])
                            next(gen, None)
                        if c == 0:
                            pending[(e, 3)] = load_w13(e, 3)
                        elif e + 1 < NE:
                            pending[(e + 1, c - 1)] = load_w13(e + 1, c - 1)
                    for _ in gen:
                        pass
                    for g in range(4):
                        yb = ye[g % 2]
                        for n in range(2):
                            pyb = py[(g * 2 + n) % 2]
                            for fc in range(16):
                                MM(pyb[:], hidT[:, fc, g * 128:(g + 1) * 128], w2c[fc // 4][:, fc % 4, n * 512:(n + 1) * 512],
                                   [hidT, w2c[fc // 4]], [pyb], start=(fc == 0), stop=(fc == 15))
                            ACT(yb[:, n * 512:(n + 1) * 512], pyb[:], AF.Copy, [pyb, gts[g]], [yb], scale=gts[g][:, e:e + 1])
                        col = e * 4 + g
                        CP(idxs[g][:, 0:1], idxf[:, col:col + 1], [idx_objs[e]], [idxs[g]])
                        op("pool", lambda en, yb=yb, g=g: en.indirect_dma_start(
                            out=out_d.ap, out_offset=bass.IndirectOffsetOnAxis(ap=idxs[g][:, 0:1], axis=0),
                            in_=yb[:], in_offset=None, bounds_check=regs["bc"], oob_is_err=False, compute_op=ALU.add),
                            [yb, idxs[g]], out_tiles, dma=True, sem_obj=yb)
                    if e + 1 < NE:
                        gather(e + 1)
                        load_w2(e + 1)
                if debug and l == 0:
                    fw.dma("sp", dbg_idx.ap, idxf[:], idx_objs, [dbg_idx], idxf)
            fw.barrier()


def _natab(rpb):
    L = rpb.shape[0]
    pad = np.concatenate([rpb.reshape(L, 4, 15 * 31), np.full((L, 4, 1), -30000.0, np.float32)], axis=2)
    idx = np.zeros((5, 128, 5, 128), np.int64)
    k = np.arange(128)[:, None, None]
    dl = np.arange(5)[None, :, None]
    q = np.arange(128)[None, None, :]
    for p, j in enumerate([0, 1, 2, 30, 31]):
        kb = min(max(j - 2, 0), 27)
        key = (kb + dl) * 128 + k
        qq = j * 128 + q
        rk, ck = key // 64, key % 64
        rq, cq = qq // 64, qq % 64
        rs = np.clip(rq - 4, 0, 56)
        cs = np.clip(cq - 8, 0, 48)
        ok = (rk >= rs) & (rk < rs + 8) & (ck >= cs) & (ck < cs + 16)
        ii = (rk - rq + 7) * 31 + (ck - cq + 15)
        idx[p] = np.where(ok, ii, 465)
    tab = pad[:, :, idx]
    tab = np.ascontiguousarray(tab.transpose(0, 2, 3, 1, 4, 5)).reshape(L, 5, 128, 2560)
    return tab.astype(np.float32)


def _layer_inputs(inp, ls):
    f = lambda a: np.ascontiguousarray(a, dtype=np.float32)
    L = len(ls)
    sel = lambda k: f(np.asarray(inp[k])[ls])
    convw = sel("conv_qk").transpose(0, 2, 1).reshape(L, 4, 128, 5).transpose(0, 2, 1, 3)
    gc = np.stack([np.tile(sel(k), (1, 2)) for k in ("na_gq", "na_gk", "mem_gq", "mem_gk")], axis=2)
    return {
        "g_mix": sel("g_mix"), "w_in": sel("w_in"), "b_gates": sel("b_gates"), "convw": f(convw),
        "g_head": sel("g_mlstm_head"), "gcols": f(gc), "natab": _natab(sel("na_rpb")),
        "g_mem": sel("g_mem"), "w_mem_kv": sel("w_mem_kv"), "w_out": sel("w_out"), "g_ffn": sel("g_ffn"),
        "w_router": sel("w_router"), "w1": sel("w1"), "w3": sel("w3"), "w2": sel("w2"),
    }


_CACHE = {}
N_FUSED_LAYERS = 4


def kernel(**inputs):
    x = np.ascontiguousarray(inputs["x"], dtype=np.float32)
    mem = np.ascontiguousarray(inputs["mem"], dtype=np.float32)
    depth = np.asarray(inputs["g_mix"]).shape[0]
    nl = N_FUSED_LAYERS
    if nl not in _CACHE:
        _CACHE[nl] = build(nl)[0]
    nc = _CACHE[nl]
    cur = x
    for l0 in range(0, depth, nl):
        shared = _layer_inputs(inputs, list(range(l0, l0 + nl)))
        in_maps = []
        for c in range(8):
            m = dict(shared)
            m["x"] = cur[c]
            m["mem"] = mem[c]
            in_maps.append(m)
        res = run_bass_kernel_spmd(nc, in_maps, core_ids=list(range(8)))
        cur = np.stack([np.asarray(r["out"]) for r in res.results], axis=0).astype(np.float32)
    return cur
```

```python
import contextlib
import numpy as np
import concourse.bass as bass
import concourse.mybir as mybir
from concourse.bass_utils import run_bass_kernel_spmd

F32 = mybir.dt.float32
BF16 = mybir.dt.bfloat16
I32 = mybir.dt.int32
ALU = mybir.AluOpType
AF = mybir.ActivationFunctionType
AX = mybir.AxisListType

ENGS = ("pe", "act", "dve", "pool", "sp")

S = 4096
D = 1024
NT = 32
DIN = 2576
NE = 16
CAP = 512
DFF = 2048
EPS = 1e-6
LN8 = float(np.log(8.0))


class Obj:
    __slots__ = ("name", "ap", "last_write", "readers")

    def __init__(self, name, ap):
        self.name = name
        self.ap = ap
        self.last_write = None
        self.readers = []

    def __getitem__(self, k):
        return self.ap[k]


class Op:
    __slots__ = ("eng", "fn", "deps", "is_dma", "sem", "sobj", "value", "signal", "waits", "epoch", "release")

    def __init__(self, eng, fn, deps, is_dma):
        self.eng = eng
        self.fn = fn
        self.deps = deps
        self.is_dma = is_dma
        self.sem = None
        self.sobj = None
        self.value = None
        self.signal = False
        self.waits = []
        self.epoch = 0
        self.release = False


class StopBuild(Exception):
    pass


_dram_objs = []
_dram_inputs = set()


class FW:
    def __init__(self, nc):
        self.nc = nc
        self.stack = contextlib.ExitStack()
        self.scopes = [self.stack]
        self.ops = []
        self.eng_ops = {e: [] for e in ENGS}
        self.last_eng_op = {e: None for e in ENGS}
        self.dma_since_barrier = []
        self.uid = 0

    @contextlib.contextmanager
    def scope(self):
        st = contextlib.ExitStack()
        self.scopes.append(st)
        try:
            yield
        finally:
            self.scopes.pop()
            st.close()

    def _nm(self, name):
        self.uid += 1
        return "%s_%d" % (name, self.uid)

    def sbuf(self, name, shape, dtype):
        t = self.scopes[-1].enter_context(self.nc.sbuf_tensor(self._nm(name), list(shape), dtype))
        return Obj(name, t)

    def psum(self, name, shape, dtype=F32):
        t = self.scopes[-1].enter_context(self.nc.psum_tensor(self._nm(name), list(shape), dtype))
        return Obj(name, t)

    def dram(self, name, shape, dtype, kind="Internal"):
        t = self.nc.dram_tensor(name, list(shape), dtype, kind=kind)
        o = Obj(name, t.ap())
        if kind != "Internal":
            _dram_objs.append(o)
            if kind == "ExternalInput":
                _dram_inputs.add(name)
        return o

    def op(self, eng, fn, reads=(), writes=(), dma=False, sem_obj=None, extra_deps=()):
        deps = list(extra_deps)
        for o in reads:
            if o.last_write is not None:
                deps.append(o.last_write)
        for o in writes:
            if o.last_write is not None:
                deps.append(o.last_write)
            deps.extend(o.readers)
        op = Op(eng, fn, deps, dma)
        op.epoch = getattr(self, "epoch", 0)
        if dma:
            assert sem_obj is not None
            op.sobj = sem_obj
            self.dma_since_barrier.append(op)
        for o in writes:
            o.last_write = op
            o.readers = []
        for o in reads:
            o.readers.append(op)
        self.ops.append(op)
        self.eng_ops[eng].append(op)
        self.last_eng_op[eng] = op
        lim = getattr(self, "op_limit", 0)
        if lim and len(self.ops) == lim:
            self.op_limit = 0
            raise StopBuild()
        return op

    def dma(self, eng, out_ap, in_ap, reads, writes, sem_obj, **kw):
        return self.op(eng, lambda e: e.dma_start(out=out_ap, in_=in_ap, **kw), reads=reads, writes=writes,
                       dma=True, sem_obj=sem_obj)

    def barrier(self):
        self.nbar = getattr(self, "nbar", 0) + 1
        self._barrier()
        if self.nbar == getattr(self, "stop_at", -1):
            raise StopBuild()

    def _barrier(self):
        tails = [o for o in self.last_eng_op.values() if o is not None]
        seen = {}
        for o in self.dma_since_barrier:
            seen[id(o.sobj)] = o
        deps = tails + list(seen.values())
        self.dma_since_barrier = []
        last = None
        for e in ENGS:
            last = self.op(e, lambda en: en.nop(nofuse=True), extra_deps=deps)
        last.release = True
        self.epoch = getattr(self, "epoch", 0) + 1
        self.last_eng_op = {e: None for e in ENGS}

    def emit(self):
        nc = self.nc
        for op in self.ops:
            for d in op.deps:
                if d.eng == "pe" and op.eng == "pe" and not d.is_dma:
                    continue
                d.signal = True
        eng_sem = {}
        for e in ENGS:
            eng_sem[e] = self.stack.enter_context(nc.semaphore("sem_" + e))
        phys = []
        free = []
        cur_map = {}
        cur_epoch = 0
        eng_cnt = {e: 0 for e in ENGS}
        known = {e: {} for e in ENGS}
        for op in self.ops:
            w = {}
            for d in op.deps:
                if d.eng == "pe" and op.eng == "pe" and not d.is_dma:
                    continue
                if d.is_dma:
                    if d.epoch < cur_epoch:
                        continue
                    rec = phys[cur_map[id(d.sobj)]]
                    s, v = rec[0], rec[1]
                else:
                    s, v = eng_sem[d.eng], d.value
                key = id(s)
                if known[op.eng].get(key, 0) >= v:
                    continue
                if key not in w or w[key][1] < v:
                    w[key] = (s, v)
            for key, (s, v) in w.items():
                known[op.eng][key] = v
            op.waits = list(w.values())
            if op.is_dma:
                k = id(op.sobj)
                if k not in cur_map:
                    if free:
                        cur_map[k] = free.pop()
                    else:
                        phys.append([self.stack.enter_context(nc.semaphore("dsem_%d" % len(phys))), 0])
                        cur_map[k] = len(phys) - 1
                rec = phys[cur_map[k]]
                rec[1] += 16
                op.value = rec[1]
                op.sem = rec[0]
                op.signal = True
            elif op.signal:
                eng_cnt[op.eng] += 1
                op.value = eng_cnt[op.eng]
                op.sem = eng_sem[op.eng]
            if op.release:
                free.extend(cur_map.values())
                cur_map.clear()
                cur_epoch += 1
        final_dma = [(rec[0], rec[1]) for rec in phys]
        self.n_sems = len(phys) + len(ENGS)
        self.n_ops = len(self.ops)

        def run(eng_name, e):
            for op in self.eng_ops[eng_name]:
                for (s, v) in op.waits:
                    e.wait_ge(s, v)
                ins = op.fn(e)
                if op.signal:
                    ins.then_inc(op.sem, 16 if op.is_dma else 1)
            if eng_name == "sp":
                for (s, v) in final_dma:
                    if v > 0:
                        e.wait_ge(s, v)

        with nc.Block() as block:
            @block.tensor
            def _(e):
                run("pe", e)

            @block.scalar
            def _(e):
                run("act", e)

            @block.vector
            def _(e):
                run("dve", e)

            @block.gpsimd
            def _(e):
                run("pool", e)

            @block.sync
            def _(e):
                run("sp", e)
        self.stack.close()


def build(nl, debug=False, stop_at=-1, op_limit=0, ne_decl=NE):
    nc = bass.Bass("TRN2", target_bir_lowering=False)
    del _dram_objs[:]
    _dram_inputs.clear()
    fw = FW(nc)
    fw.stop_at = stop_at
    fw.op_limit = op_limit
    op = fw.op

    def MM(out, lhsT, rhs, R, W, start=True, stop=True):
        op("pe", lambda e: e.matmul(out, lhsT=lhsT, rhs=rhs, start=start, stop=stop), R, W)

    def TR(out, in_, ident, R, W):
        op("pe", lambda e: e.transpose(out=out, in_=in_, identity=ident), R, W)

    def ACT(out, in_, func, R, W, **kw):
        op("act", lambda e: e.activation(out=out, in_=in_, func=func, **kw), R, W)

    def TT(out, in0, in1, alu, R, W, eng="dve"):
        op(eng, lambda e: e.tensor_tensor(out=out, in0=in0, in1=in1, op=alu), R, W)

    def TS(out, in0, s1, s2, op0, op1, R, W, eng="dve", **kw):
        if op1 is None:
            op(eng, lambda e: e.tensor_scalar(out=out, in0=in0, scalar1=s1, scalar2=s2, op0=op0, **kw), R, W)
        else:
            op(eng, lambda e: e.tensor_scalar(out=out, in0=in0, scalar1=s1, scalar2=s2, op0=op0, op1=op1, **kw), R, W)

    def STT(out, in0, scalar, in1, op0, op1, R, W, eng="dve"):
        op(eng, lambda e: e.scalar_tensor_tensor(out=out, in0=in0, scalar=scalar, in1=in1, op0=op0, op1=op1), R, W)

    def CP(out, in_, R, W, eng="dve"):
        op(eng, lambda e: e.tensor_copy(out=out, in_=in_), R, W)

    def MS(ap, val, W, eng="dve"):
        op(eng, lambda e: e.memset(ap, val), (), W)

    def RED(out, in_, alu, R, W):
        op("dve", lambda e: e.tensor_reduce(out=out, in_=in_, axis=AX.X, op=alu), R, W)

    def RCP(out, in_, R, W):
        op("dve", lambda e: e.reciprocal(out=out, in_=in_), R, W)

    def rstd_from_ss(ss_obj, ss_ap, tmp_ap, out_ap, inv_n):
        ACT(tmp_ap, ss_ap, AF.Sqrt, [ss_obj], [ss_obj], scale=float(inv_n), bias=float(EPS))
        RCP(out_ap, tmp_ap, [ss_obj], [ss_obj])

    def din(name, shape, dt=F32):
        return fw.dram(name, shape, dt, kind="ExternalInput")

    x_in = din("x", [S, D])
    mem_in = din("mem", [256, D])
    g_mix = din("g_mix", [nl, D])
    w_in = din("w_in", [nl, D, DIN])
    b_gates = din("b_gates", [nl, 16])
    convw_d = din("convw", [nl, 128, 4, 5])
    g_head = din("g_head", [nl, 512])
    gcols_d = din("gcols", [nl, 128, 4])
    natab = din("natab", [nl, 5, 128, 2560])
    g_mem = din("g_mem", [nl, D])
    w_mkv = din("w_mem_kv", [nl, D, 512])
    w_out = din("w_out", [nl, D, D])
    g_ffn = din("g_ffn", [nl, D])
    w_rt = din("w_router", [nl, D, NE])
    w1 = din("w1", [nl, ne_decl, D, DFF])
    w3 = din("w3", [nl, ne_decl, D, DFF])
    w2 = din("w2", [nl, ne_decl, DFF, D])
    out_d = fw.dram("out", [S, D], F32, kind="ExternalOutput")
    dk = "ExternalOutput"
    qkraw_d = fw.dram("qkraw_d", [512, S], F32, kind=dk)
    so_d = fw.dram("so_d", [S, 512], BF16, kind=dk)
    v_d = fw.dram("v_d", [S, 512], BF16, kind=dk)
    hf_d = fw.dram("hf_d", [S, D], BF16, kind=dk)
    aff_d = fw.dram("aff_d", [S, NE], F32, kind=dk)
    if debug:
        dbg_xmix = fw.dram("dbg_xmix", [S, D], F32, kind=dk)
        dbg_yTm = fw.dram("dbg_yTm", [128, 4, S], BF16, kind=dk)
        dbg_yTnc = fw.dram("dbg_yTnc", [128, 4, S], BF16, kind=dk)
        dbg_qkT = fw.dram("dbg_qkT", [128, 4, S], BF16, kind=dk)
        dbg_idx = fw.dram("dbg_idx", [128, NE * 4], I32, kind=dk)
        dbg_nqT = fw.dram("dbg_nqT", [128, 2, S], BF16, kind=dk)
        dbg_gates = fw.dram("dbg_gates", [128, NT, 16], F32, kind=dk)
    out_tiles = [Obj("out_t%d" % t, None) for t in range(NT)]

    ident_f = fw.sbuf("ident_f", [128, 128], F32)
    ident_b = fw.sbuf("ident_b", [128, 128], BF16)
    io = fw.sbuf("io", [128, 128], F32)
    triU = fw.sbuf("triU", [128, 128], F32)
    triL = fw.sbuf("triL", [128, 128], F32)
    ones_f = fw.sbuf("ones_f", [128, 128], F32)
    ones_b = fw.sbuf("ones_b", [128, 128], BF16)
    iota512 = fw.sbuf("iota512", [128, 512], F32)
    regs = {}

    def _mkreg(e):
        regs["bc"] = e.alloc_register("bcreg")
        return e.reg_mov(regs["bc"], S - 1)
    op("pool", _mkreg, (), ())
    op("pool", lambda e: e.iota(io[:], pattern=[[1, 128]], base=0, channel_multiplier=-1,
                                allow_small_or_imprecise_dtypes=True), (), [io])
    op("pool", lambda e: e.iota(iota512[:], pattern=[[1, 512]], base=0, channel_multiplier=0,
                                allow_small_or_imprecise_dtypes=True), (), [iota512])
    op("dve", lambda e: e.tensor_single_scalar(out=ident_f[:], in_=io[:], scalar=0.0, op=ALU.is_equal), [io], [ident_f])
    CP(ident_b[:], ident_f[:], [ident_f], [ident_b])
    op("dve", lambda e: e.tensor_single_scalar(out=triU[:], in_=io[:], scalar=0.0, op=ALU.is_ge), [io], [triU])
    op("dve", lambda e: e.tensor_single_scalar(out=triL[:], in_=io[:], scalar=0.0, op=ALU.is_le), [io], [triL])
    MS(ones_f[:], 1.0, [ones_f])
    MS(ones_b[:], 1.0, [ones_b])
    try:
        fw.barrier()
        _layers(nl, debug, fw, locals())
    except StopBuild:
        while len(fw.scopes) > 1:
            fw.scopes.pop().close()
        scr = fw.sbuf("scr", [1, 64], F32)
        scrb = fw.sbuf("scrb", [1, 64], BF16)
        scri = fw.sbuf("scri", [1, 64], I32)
        for nm, o in list(locals().items()):
            if isinstance(o, Obj) and o.ap is not None and hasattr(o.ap, "shape") and "dram" in str(type(o.ap.tensor if hasattr(o.ap, "tensor") else "")).lower():
                pass
        for o in _dram_objs:
            ap = o.ap
            ix = tuple([0] * (len(ap.shape) - 2)) + (slice(0, 1), slice(0, 1))
            t = {F32: scr, BF16: scrb, I32: scri}[ap.dtype]
            if o.name in _dram_inputs:
                fw.dma("sp", t[0:1, 0:1], ap[ix], [o], [t], t)
            else:
                fw.dma("sp", ap[ix], t[0:1, 0:1], [t], [o], t)
    fw.emit()
    return nc, fw


def _layers(nl, debug, fw, env):
    globals().update({k: v for k, v in env.items() if k not in ("nl", "debug", "fw", "env")})
    op = fw.op
    for l in range(nl):
        x_src = x_in if l == 0 else out_d
        with fw.scope():
            kmT = fw.sbuf("kmT", [128, 2, 256], BF16)
            vm = fw.sbuf("vm", [128, 2, 4, 65], BF16)
            gates = fw.sbuf("gates", [128, NT, 16], F32)
            aff_all = fw.sbuf("aff_all", [128, NT, NE], F32)
            cnt_all = fw.sbuf("cnt_all", [128, NT, NE], F32)
            gcols = fw.sbuf("gcols", [128, 4], F32)
            fw.dma("sp", gcols[:], gcols_d[l], [gcols_d], [gcols], gcols)

            with fw.scope():
                yT_nc = fw.sbuf("yT_nc", [128, 4, S], BF16)
                with fw.scope():
                    nqT = fw.sbuf("nqT", [128, 2, S], BF16)
                    nkT = fw.sbuf("nkT", [128, 2, S], BF16)
                    cqT = fw.sbuf("cqT", [128, 2, S], BF16)
                    nv = fw.sbuf("nv", [128, NT, 4, 65], BF16)
                    MS(nv[:, :, :, 64:65], 1.0, [nv])
                    MS(vm[:, :, :, 64:65], 1.0, [vm])

                    with fw.scope():
                        gmem_b = fw.sbuf("gmem_b", [128, D], F32)
                        wkv = fw.sbuf("wkv", [128, 8, 512], BF16)
                        mt = [fw.sbuf("mt%d" % i, [128, D], F32) for i in range(2)]
                        mjunk = fw.sbuf("mjunk", [128, D], F32)
                        mn = fw.sbuf("mn", [128, D], BF16)
                        memT = fw.sbuf("memT", [128, 8, 256], BF16)
                        mst = fw.sbuf("mst", [128, 8], F32)
                        msq = fw.sbuf("msq", [128, 256], F32)
                        mss = fw.sbuf("mss", [128, 8], F32)
                        kmn = fw.sbuf("kmn", [128, 4, 64], BF16)
                        pT0 = fw.psum("pT0", [128, 8, 128], BF16)
                        pkv = fw.psum("pkv", [128, 512], F32)
                        fw.dma("sp", gmem_b[:], g_mem[l:l + 1, :].partition_broadcast(128), [g_mem], [gmem_b], gmem_b)
                        fw.dma("pool", wkv[:], w_mkv[l].rearrange("(k p) n -> p k n", p=128), [w_mkv], [wkv], wkv)
                        for i in range(2):
                            fw.dma("sp", mt[i][:], mem_in[i * 128:(i + 1) * 128, :], [mem_in], [mt[i]], mt[i])
                            MS(mst[:, 0:1], 0.0, [mst])
                            ACT(mjunk[:], mt[i][:], AF.Square, [mt[i]], [mjunk, mst], accum_out=mst[:, 0:1])
                            rstd_from_ss(mst, mst[:, 0:1], mst[:, 1:2], mst[:, 2:3], 1.0 / D)
                            STT(mn[:], mt[i][:], mst[:, 2:3], gmem_b[:], ALU.mult, ALU.mult, [mt[i], mst, gmem_b], [mn])
                            for k in range(8):
                                TR(pT0[:, k, :], mn[:, k * 128:(k + 1) * 128], ident_b[:], [mn, ident_b], [pT0])
                            ACT(memT[:, :, i * 128:(i + 1) * 128], pT0[:], AF.Copy, [pT0], [memT])
                        for i in range(2):
                            for k in range(8):
                                MM(pkv[:], memT[:, k, i * 128:(i + 1) * 128], wkv[:, k, :], [memT, wkv], [pkv],
                                   start=(k == 0), stop=(k == 7))
                            CP(vm[:, i, :, 0:64], pkv[:, 256:512].rearrange("p (h d) -> p h d", d=64), [pkv], [vm])
                            MS(mss[:, 0:4], 0.0, [mss])
                            for hh in range(4):
                                ACT(msq[:, hh * 64:(hh + 1) * 64], pkv[:, hh * 64:(hh + 1) * 64], AF.Square, [pkv], [msq, mss],
                                    accum_out=mss[:, hh:hh + 1])
                            rstd_from_ss(mss, mss[:, 0:4], mss[:, 4:8], mss[:, 4:8], 1.0 / 64)
                            TT(kmn[:], pkv[:, 0:256].rearrange("p (h d) -> p h d", d=64),
                               mss[:, 4:8].unsqueeze(2).to_broadcast([128, 4, 64]), ALU.mult, [pkv, mss], [kmn])
                            for hp in range(2):
                                TR(pT0[:, hp, :], kmn[:, 2 * hp:2 * hp + 2, :].rearrange("p h d -> p (h d)"), ident_b[:],
                                   [kmn, ident_b], [pT0])
                            ACT(kmT[:, :, i * 128:(i + 1) * 128], pT0[:, 0:2, :], AF.Copy, [pT0, gcols], [kmT],
                                scale=gcols[:, 3:4])
                    fw.barrier()

                    with fw.scope():
                        wi = fw.sbuf("wi", [128, 8, DIN], BF16)
                        gmix_b = fw.sbuf("gmix_b", [128, D], F32)
                        bg_b = fw.sbuf("bg_b", [128, 16], F32)
                        xt = [fw.sbuf("xt%d" % i, [128, D], F32) for i in range(2)]
                        xjunk = fw.sbuf("xjunk", [128, D], F32)
                        xn = [fw.sbuf("xn%d" % i, [128, D], BF16) for i in range(2)]
                        hT = [fw.sbuf("hT%d" % i, [128, 8, 512], BF16) for i in range(2)]
                        xst = [fw.sbuf("xst%d" % i, [128, 4], F32) for i in range(2)]
                        qkst = [fw.sbuf("qkst%d" % i, [128, 512], F32) for i in range(2)]
                        sot = [fw.sbuf("sot%d" % i, [128, 512], BF16) for i in range(2)]
                        vt = [fw.sbuf("vt%d" % i, [128, 512], BF16) for i in range(2)]
                        nsq = [fw.sbuf("nsq%d" % i, [128, 512], F32) for i in range(2)]
                        nss = [fw.sbuf("nss%d" % i, [128, 24], F32) for i in range(2)]
                        nqn = [fw.sbuf("nqn%d" % i, [128, 512], BF16) for i in range(2)]
                        cqn = [fw.sbuf("cqn%d" % i, [128, 256], BF16) for i in range(2)]
                        pT1 = [fw.psum("pT1_%d" % i, [128, 8, 128], BF16) for i in range(2)]
                        pmm = [fw.psum("pmm%d" % i, [128, 512], F32) for i in range(4)]
                        pqk = [fw.psum("pqk%d" % i, [128, 512], F32) for i in range(2)]
                        fw.dma("pool", wi[:, 0:4, :], w_in[l, 0:512, :].rearrange("(k p) n -> p k n", p=128), [w_in], [wi], wi)
                        fw.dma("pool", wi[:, 4:8, :], w_in[l, 512:1024, :].rearrange("(k p) n -> p k n", p=128), [w_in], [wi], wi)
                        fw.dma("sp", gmix_b[:], g_mix[l:l + 1, :].partition_broadcast(128), [g_mix], [gmix_b], gmix_b)
                        fw.dma("sp", bg_b[:], b_gates[l:l + 1, :].partition_broadcast(128), [b_gates], [bg_b], bg_b)
                        mmi = [0]

                        def p1_prep(st):
                            hTs = hT[st % 2]
                            for tt in range(4):
                                t = st * 4 + tt
                                b = t % 2
                                fw.dma("sp", xt[b][:], x_src[t * 128:(t + 1) * 128, :], [out_tiles[t]], [xt[b]], xt[b])
                                MS(xst[b][:, 0:1], 0.0, [xst[b]])
                                ACT(xjunk[:], xt[b][:], AF.Square, [xt[b]], [xjunk, xst[b]], accum_out=xst[b][:, 0:1])
                                rstd_from_ss(xst[b], xst[b][:, 0:1], xst[b][:, 1:2], xst[b][:, 2:3], 1.0 / D)
                                STT(xn[b][:], xt[b][:], xst[b][:, 2:3], gmix_b[:], ALU.mult, ALU.mult,
                                    [xt[b], xst[b], gmix_b], [xn[b]])
                                for k in range(8):
                                    TR(pT1[b][:, k, :], xn[b][:, k * 128:(k + 1) * 128], ident_b[:], [xn[b], ident_b], [pT1[b]])
                                ACT(hTs[:, :, tt * 128:(tt + 1) * 128], pT1[b][:], AF.Copy, [pT1[b]], [hTs])
                        def p1_body(st):
                            hTs = hT[st % 2]
                            for c in range(4):
                                pq = pqk[c % 2]
                                for k in range(8):
                                    MM(pq[:], wi[:, k, c * 128:(c + 1) * 128], hTs[:, k, :], [wi, hTs], [pq],
                                       start=(k == 0), stop=(k == 7))
                                qs = qkst[c % 2]
                                CP(qs[:], pq[:], [pq], [qs])
                                fw.dma("sp", qkraw_d[c * 128:(c + 1) * 128, st * 512:(st + 1) * 512], qs[:], [qs], [qkraw_d], qs)
                            for tt in range(4):
                                t = st * 4 + tt
                                b = t % 2
                                lh = lambda k: hTs[:, k, tt * 128:(tt + 1) * 128]
                                pm = pmm[mmi[0] % 4]; mmi[0] += 1
                                for k in range(8):
                                    MM(pm[:], lh(k), wi[:, k, 512:1024], [hTs, wi], [pm], start=(k == 0), stop=(k == 7))
                                ACT(vt[b][:], pm[:], AF.Copy, [pm], [vt[b]])
                                fw.dma("sp", v_d[t * 128:(t + 1) * 128, :], vt[b][:], [vt[b]], [v_d], vt[b])
                                pm = pmm[mmi[0] % 4]; mmi[0] += 1
                                for k in range(8):
                                    MM(pm[:], lh(k), wi[:, k, 1024:1536], [hTs, wi], [pm], start=(k == 0), stop=(k == 7))
                                ACT(sot[b][:], pm[:], AF.Sigmoid, [pm], [sot[b]])
                                fw.dma("sp", so_d[t * 128:(t + 1) * 128, :], sot[b][:], [sot[b]], [so_d], sot[b])
                                pm = pmm[mmi[0] % 4]; mmi[0] += 1
                                for k in range(8):
                                    MM(pm[:], lh(k), wi[:, k, 1552:2064], [hTs, wi], [pm], start=(k == 0), stop=(k == 7))
                                MS(nss[b][:, 0:8], 0.0, [nss[b]])
                                for hh in range(8):
                                    ACT(nsq[b][:, hh * 64:(hh + 1) * 64], pm[:, hh * 64:(hh + 1) * 64], AF.Square, [pm], [nsq[b], nss[b]],
                                        accum_out=nss[b][:, hh:hh + 1])
                                rstd_from_ss(nss[b], nss[b][:, 0:8], nss[b][:, 8:16], nss[b][:, 8:16], 1.0 / 64)
                                TT(nqn[b][:].rearrange("p (h d) -> p h d", d=64), pm[:].rearrange("p (h d) -> p h d", d=64),
                                   nss[b][:, 8:16].unsqueeze(2).to_broadcast([128, 8, 64]), ALU.mult, [pm, nss[b]], [nqn[b]])
                                for j in range(4):
                                    TR(pT1[b][:, j, :], nqn[b][:, j * 128:(j + 1) * 128], ident_b[:], [nqn[b], ident_b], [pT1[b]])
                                ACT(nqT[:, :, t * 128:(t + 1) * 128], pT1[b][:, 0:2, :], AF.Copy, [pT1[b], gcols], [nqT],
                                    scale=gcols[:, 0:1])
                                ACT(nkT[:, :, t * 128:(t + 1) * 128], pT1[b][:, 2:4, :], AF.Copy, [pT1[b], gcols], [nkT],
                                    scale=gcols[:, 1:2])
                                pm = pmm[mmi[0] % 4]; mmi[0] += 1
                                for k in range(8):
                                    MM(pm[:], lh(k), wi[:, k, 2064:2576], [hTs, wi], [pm], start=(k == 0), stop=(k == 7))
                                CP(nv[:, t, :, 0:64], pm[:, 0:256].rearrange("p (h d) -> p h d", d=64), [pm], [nv])
                                MS(nss[b][:, 16:20], 0.0, [nss[b]])
                                for hh in range(4):
                                    ACT(nsq[b][:, hh * 64:(hh + 1) * 64], pm[:, 256 + hh * 64:256 + (hh + 1) * 64], AF.Square, [pm],
                                        [nsq[b], nss[b]], accum_out=nss[b][:, 16 + hh:17 + hh])
                                rstd_from_ss(nss[b], nss[b][:, 16:20], nss[b][:, 20:24], nss[b][:, 20:24], 1.0 / 64)
                                TT(cqn[b][:].rearrange("p (h d) -> p h d", d=64), pm[:, 256:512].rearrange("p (h d) -> p h d", d=64),
                                   nss[b][:, 20:24].unsqueeze(2).to_broadcast([128, 4, 64]), ALU.mult, [pm, nss[b]], [cqn[b]])
                                for j in range(2):
                                    TR(pT1[b][:, 4 + j, :], cqn[b][:, j * 128:(j + 1) * 128], ident_b[:], [cqn[b], ident_b], [pT1[b]])
                                ACT(cqT[:, :, t * 128:(t + 1) * 128], pT1[b][:, 4:6, :], AF.Copy, [pT1[b], gcols], [cqT],
                                    scale=gcols[:, 2:3])
                                pm = pmm[mmi[0] % 4]; mmi[0] += 1
                                for k in range(8):
                                    MM(pm[:, 0:16], lh(k), wi[:, k, 1536:1552], [hTs, wi], [pm], start=(k == 0), stop=(k == 7))
                                TT(gates[:, t, :], pm[:, 0:16], bg_b[:], ALU.add, [pm, bg_b], [gates])

                        p1_prep(0)
                        for st in range(8):
                            if st + 1 < 8:
                                p1_prep(st + 1)
                            p1_body(st)
                    fw.barrier()

                    if debug and l == 0:
                        fw.dma("sp", dbg_nqT.ap, nqT[:], [nqT], [dbg_nqT], nqT)
                        fw.barrier()
                    with fw.scope():
                        EB = [fw.sbuf("EB%d" % i, [128, 2560], BF16) for i in range(5)]
                        tabst = fw.sbuf("tabst", [128, 2560], F32)
                        Ena = [fw.sbuf("Ena%d" % i, [128, 640], BF16) for i in range(2)]
                        PTn = [fw.sbuf("PTn%d" % i, [128, 640], BF16) for i in range(2)]
                        Ec = [fw.sbuf("Ec%d" % i, [128, 256], BF16) for i in range(2)]
                        ycat = [fw.sbuf("ycat%d" % i, [128, 512], BF16) for i in range(2)]
                        rec = [fw.sbuf("rec%d" % i, [128, 8], F32) for i in range(2)]
                        ps_na = [fw.psum("ps_na%d" % i, [128, 1024], F32) for i in range(2)]
                        ps_c = fw.psum("ps_c", [128, 2, 256], F32)
                        po_na = fw.psum("po_na", [128, 4, 65], F32)
                        po_c = fw.psum("po_c", [128, 4, 65], F32)
                        pT3 = fw.psum("pT3", [128, 4, 128], BF16)
                        for p in range(5):
                            fw.dma("sp", tabst[:], natab[l, p], [natab], [tabst], tabst)
                            ACT(EB[p][:], tabst[:], AF.Exp, [tabst], [EB[p]])
                        ui = 0
                        for j in range(NT):
                            kb = min(max(j - 2, 0), 27)
                            pat = {0: 0, 1: 1, 30: 3, 31: 4}.get(j, 2)
                            yb = ycat[j % 2]
                            rb = rec[j % 2]
                            qs = slice(j * 128, (j + 1) * 128)
                            for h in range(4):
                                hp, lo = h // 2, (h % 2) * 64
                                pn = ps_na[ui % 2]
                                En = Ena[ui % 2]
                                Pn = PTn[ui % 2]
                                Ecb = Ec[ui % 2]
                                ui += 1
                                for dl in range(5):
                                    ks = slice((kb + dl) * 128, (kb + dl + 1) * 128)
                                    MM(pn[:, dl * 128:(dl + 1) * 128], nkT[lo:lo + 64, hp, ks], nqT[lo:lo + 64, hp, qs],
                                       [nkT, nqT], [pn])
                                ACT(En[:], pn[:, 0:640], AF.Exp, [pn], [En], scale=0.125)
                                TT(Pn[:], En[:], EB[pat][:, h * 640:(h + 1) * 640], ALU.mult, [En, EB[pat]], [Pn])
                                for dl in range(5):
                                    MM(po_na[:, h, :], Pn[:, dl * 128:(dl + 1) * 128], nv[:, kb + dl, h, :], [Pn, nv], [po_na],
                                       start=(dl == 0), stop=(dl == 4))
                                for i in range(2):
                                    MM(ps_c[:, h % 2, i * 128:(i + 1) * 128], kmT[lo:lo + 64, hp, i * 128:(i + 1) * 128],
                                       cqT[lo:lo + 64, hp, qs], [kmT, cqT], [ps_c])
                                ACT(Ecb[:], ps_c[:, h % 2, :], AF.Exp, [ps_c], [Ecb], scale=0.125)
                                for i in range(2):
                                    MM(po_c[:, h, :], Ecb[:, i * 128:(i + 1) * 128], vm[:, i, h, :], [Ecb, vm], [po_c],
                                       start=(i == 0), stop=(i == 1))
                            RCP(rb[:, 0:4], po_na[:, :, 64], [po_na], [rb])
                            TT(yb[:, 0:256].rearrange("p (h d) -> p h d", d=64), po_na[:, :, 0:64],
                               rb[:, 0:4].unsqueeze(2).to_broadcast([128, 4, 64]), ALU.mult, [po_na, rb], [yb])
                            RCP(rb[:, 4:8], po_c[:, :, 64], [po_c], [rb])
                            TT(yb[:, 256:512].rearrange("p (h d) -> p h d", d=64), po_c[:, :, 0:64],
                               rb[:, 4:8].unsqueeze(2).to_broadcast([128, 4, 64]), ALU.mult, [po_c, rb], [yb])
                            for k in range(4):
                                TR(pT3[:, k, :], yb[:, k * 128:(k + 1) * 128], ident_b[:], [yb, ident_b], [pT3])
                            ACT(yT_nc[:, :, qs], pT3[:], AF.Copy, [pT3], [yT_nc])
                    fw.barrier()

                with fw.scope():
                    yT_m = fw.sbuf("yT_m", [128, 4, S], BF16)
                    with fw.scope():
                        qkT = fw.sbuf("qkT", [128, 4, S], BF16)
                        with fw.scope():
                            convw = fw.sbuf("convw", [128, 4, 5], F32)
                            raw = [fw.sbuf("raw%d" % i, [128, 516], F32) for i in range(2)]
                            cacc = [fw.sbuf("cacc%d" % i, [128, 512], F32) for i in range(2)]
                            fw.dma("sp", convw[:], convw_d[l], [convw_d], [convw], convw)
                            ui = 0
                            for st in range(8):
                                for c in range(4):
                                    r = raw[ui % 2]
                                    a = cacc[ui % 2]
                                    ui += 1
                                    lo_t = st * 512 - 2
                                    hi_t = st * 512 + 514
                                    d0, d1 = 0, 516
                                    if st == 0:
                                        MS(r[:, 0:2], 0.0, [r])
                                        lo_t, d0 = 0, 2
                                    if st == 7:
                                        MS(r[:, 514:516], 0.0, [r])
                                        hi_t, d1 = S, 514
                                    fw.dma("sp", r[:, d0:d1], qkraw_d[c * 128:(c + 1) * 128, lo_t:hi_t], [qkraw_d], [r], r)
                                    TS(a[:], r[:, 0:512], convw[:, c, 0:1], None, ALU.mult, None, [r, convw], [a])
                                    for jj in range(1, 5):
                                        STT(a[:], r[:, jj:jj + 512], convw[:, c, jj:jj + 1], a[:], ALU.mult, ALU.add,
                                            [r, convw, a], [a])
                                    ACT(qkT[:, c, st * 512:(st + 1) * 512], a[:], AF.Silu, [a], [qkT])
                        fw.barrier()

                        if debug and l == 0:
                            fw.dma("sp", dbg_qkT.ap, qkT[:], [qkT], [dbg_qkT], qkT)
                            fw.dma("sp", dbg_gates.ap, gates[:], [gates], [dbg_gates], gates)
                            fw.barrier()
                        with fw.scope():
                            ghead_b = fw.sbuf("ghead_b", [128, 512], F32)
                            fw.dma("sp", ghead_b[:], g_head[l:l + 1, :].partition_broadcast(128), [g_head], [ghead_b], ghead_b)
                            LI = [fw.sbuf("LI%d" % d, [128, 128], F32) for d in range(2)]
                            LF = [fw.sbuf("LF%d" % d, [128, 128], F32) for d in range(2)]
                            Bc = [fw.sbuf("Bc%d" % d, [128, 128], F32) for d in range(2)]
                            Gc = [fw.sbuf("Gc%d" % d, [128, 128], F32) for d in range(2)]
                            Es = [fw.sbuf("Es%d" % d, [128, 128], F32) for d in range(2)]
                            Ws = [fw.sbuf("Ws%d" % d, [128, 128], F32) for d in range(2)]
                            EM = [fw.sbuf("EM%d" % d, [128, 128], F32) for d in range(2)]
                            EG = [fw.sbuf("EG%d" % d, [128, 128], F32) for d in range(2)]
                            gtmp = fw.sbuf("gtmp", [128, 128], F32)
                            pg = fw.psum("pg", [128, 2, 128], F32)
                            v3 = lambda o: o[:].rearrange("p (c h) -> p c h", h=4)
                            for d in range(2):
                                CP(v3(LI[d]), gates[:, :, 8 * d:8 * d + 4], [gates], [LI[d]])
                                ACT(v3(gtmp), gates[:, :, 8 * d + 4:8 * d + 8], AF.Exp, [gates], [gtmp], scale=-1.0)
                                ACT(gtmp[:], gtmp[:], AF.Ln, [gtmp], [gtmp], bias=1.0)
                                TS(LF[d][:], gtmp[:], -1.0, None, ALU.mult, None, [gtmp], [LF[d]])
                                MM(pg[:, 0, :], (triU if d == 0 else triL)[:], LF[d][:], [triU, triL, LF[d]], [pg])
                                MM(pg[:, 1, :], ones_f[:], LF[d][:], [ones_f, LF[d]], [pg])
                                CP(Bc[d][:], pg[:, 0, :], [pg], [Bc[d]])
                                CP(Gc[d][:], pg[:, 1, :], [pg], [Gc[d]])
                                TT(gtmp[:], LI[d][:], Bc[d][:], ALU.subtract, [LI[d], Bc[d]], [gtmp])
                                ACT(Es[d][:], gtmp[:], AF.Exp, [gtmp], [Es[d]])
                                TT(gtmp[:], gtmp[:], Gc[d][:], ALU.add, [gtmp, Gc[d]], [gtmp])
                                ACT(Ws[d][:], gtmp[:], AF.Exp, [gtmp], [Ws[d]])
                                ACT(EM[d][:], Bc[d][:], AF.Exp, [Bc[d]], [EM[d]], scale=-1.0, bias=LN8)
                                ACT(EG[d][:], Gc[d][:], AF.Exp, [Gc[d]], [EG[d]])
                            maskd = [triU, triL]

                            Cprev = [fw.sbuf("Cprev%d" % d, [128, NT, 129], BF16) for d in range(2)]
                            Cst = [fw.sbuf("Cst%d" % d, [128, 129], F32) for d in range(2)]
                            EGs = [fw.sbuf("EGs%d" % d, [128, NT], F32) for d in range(2)]
                            kwA = [fw.sbuf("kwA%d" % i, [128, 128], BF16) for i in range(4)]
                            kwB = [fw.sbuf("kwB%d" % i, [128, 128], BF16) for i in range(4)]
                            v1 = [fw.sbuf("v1_%d" % i, [128, 2, 129], BF16) for i in range(4)]
                            sob = [fw.sbuf("sob%d" % i, [128, 256], BF16) for i in range(2)]
                            PTm = [fw.sbuf("PTm%d" % i, [128, 2, 2, 128], BF16) for i in range(2)]
                            dn = [fw.sbuf("dn%d" % i, [128, 16], F32) for i in range(2)]
                            hs = [fw.sbuf("hs%d" % i, [128, 2, 128], F32) for i in range(2)]
                            hj = fw.sbuf("hj", [128, 128], F32)
                            ymb = [fw.sbuf("ymb%d" % i, [128, 256], BF16) for i in range(2)]
                            pTk_t = fw.psum("pTk", [128, 2, 128], BF16)
                            pTk = [Obj("pTk%d" % i, pTk_t[:, i, :]) for i in range(2)]
                            pkvm_t = fw.psum("pkvm", [128, 2, 129], F32)
                            pkvm = [Obj("pkvm%d" % i, pkvm_t[:, i, :]) for i in range(2)]
                            psm = fw.psum("psm", [128, 2, 512], F32)
                            pnd = fw.psum("pnd", [128, 2, 2, 256], F32)
                            pTy = fw.psum("pTy", [128, 2, 128], BF16)
                            for i in range(4):
                                MS(kwA[i][:, 64:128], 0.0, [kwA[i]])
                                MS(kwB[i][:, 0:64], 0.0, [kwB[i]])
                                MS(v1[i][:, :, 128:129], 1.0, [v1[i]])
                            for hp in range(2):
                                h0 = 2 * hp
                                for d in range(2):
                                    MS(Cst[d][:], 0.0, [Cst[d]])
                                    CP(EGs[d][0:64, :], v3(EG[d])[0:64, :, h0], [EG[d]], [EGs[d]])
                                    CP(EGs[d][64:128, :], v3(EG[d])[64:128, :, h0 + 1], [EG[d]], [EGs[d]])
                                ui = 0
                                for ci in range(NT):
                                    for d in range(2):
                                        c = ci if d == 0 else NT - 1 - ci
                                        cs = slice(c * 128, (c + 1) * 128)
                                        ka, kb_, vv, pk, pt = kwA[ui % 4], kwB[ui % 4], v1[ui % 4], pkvm[ui % 2], pTk[ui % 2]
                                        ui += 1
                                        ACT(Cprev[d][:, c, :], Cst[d][:], AF.Copy, [Cst[d]], [Cprev[d]])
                                        fw.dma("sp", vv[:, :, 0:128], v_d[cs, h0 * 128:(h0 + 2) * 128].rearrange("p (h d) -> p h d", d=128),
                                               [v_d], [vv], vv)
                                        TR(pt[:], qkT[:, 2 + hp, cs], ident_b[:], [qkT, ident_b], [pt])
                                        TS(ka[:, 0:64], pt[:, 0:64], Ws[d][:, c * 4 + h0:c * 4 + h0 + 1], None, ALU.mult, None,
                                           [pt, Ws[d]], [ka])
                                        TS(kb_[:, 64:128], pt[:, 64:128], Ws[d][:, c * 4 + h0 + 1:c * 4 + h0 + 2], None, ALU.mult, None,
                                           [pt, Ws[d]], [kb_])
                                        MM(pk[:], ka[:], vv[:, 0, :], [ka, vv], [pk], start=True, stop=False)
                                        MM(pk[:], kb_[:], vv[:, 1, :], [kb_, vv], [pk], start=False, stop=True)
                                        STT(Cst[d][:], Cst[d][:], EGs[d][:, c:c + 1], pk[:], ALU.mult, ALU.add,
                                            [Cst[d], EGs[d], pk], [Cst[d]])
                                for c in range(NT):
                                    cs = slice(c * 128, (c + 1) * 128)
                                    vv = v1[c % 4]
                                    sb = sob[c % 2]
                                    Pm = PTm[c % 2]
                                    dnb = dn[c % 2]
                                    hsb = hs[c % 2]
                                    yb = ymb[c % 2]
                                    fw.dma("sp", vv[:, :, 0:128], v_d[cs, h0 * 128:(h0 + 2) * 128].rearrange("p (h d) -> p h d", d=128),
                                           [v_d], [vv], vv)
                                    fw.dma("sp", sb[:], so_d[cs, h0 * 128:(h0 + 2) * 128], [so_d], [sb], sb)
                                    for i in range(2):
                                        lo = i * 64
                                        MM(psm[:, i, 0:128], qkT[lo:lo + 64, 2 + hp, cs], qkT[lo:lo + 64, hp, cs], [qkT], [psm])
                                    for d in range(2):
                                        for i in range(2):
                                            col = c * 4 + h0 + i
                                            STT(Pm[:, d, i, :], psm[:, i, 0:128], Es[d][:, col:col + 1], maskd[d][:], ALU.mult, ALU.mult,
                                                [psm, Es[d], maskd[d]], [Pm])
                                    for d in range(2):
                                        for i in range(2):
                                            lo = i * 64
                                            MM(pnd[:, d, i, 0:129], Pm[:, d, i, :], vv[:, i, :], [Pm, vv], [pnd], start=True, stop=False)
                                            MM(pnd[:, d, i, 0:129], qkT[lo:lo + 64, hp, cs], Cprev[d][lo:lo + 64, c, :],
                                               [qkT, Cprev[d]], [pnd], start=False, stop=True)
                                    for d in range(2):
                                        col = c * 4 + h0
                                        TT(dnb[:, 2 * d:2 * d + 2], pnd[:, d, :, 128], EM[d][:, col:col + 2], ALU.max,
                                           [pnd, EM[d]], [dnb])
                                        STT(dnb[:, 4 + 2 * d:6 + 2 * d], pnd[:, d, :, 128], -1.0, dnb[:, 2 * d:2 * d + 2],
                                            ALU.mult, ALU.max, [pnd, dnb], [dnb])
                                    RCP(dnb[:, 8:12], dnb[:, 4:8], [dnb], [dnb])
                                    for i in range(2):
                                        TS(hsb[:, i, :], pnd[:, 0, i, 0:128], dnb[:, 8 + i:9 + i], None, ALU.mult, None, [pnd, dnb], [hsb])
                                        STT(hsb[:, i, :], pnd[:, 1, i, 0:128], dnb[:, 10 + i:11 + i], hsb[:, i, :], ALU.mult, ALU.add,
                                            [pnd, dnb, hsb], [hsb])
                                        MS(dnb[:, 12 + i:13 + i], 0.0, [dnb])
                                        ACT(hj[:], hsb[:, i, :], AF.Square, [hsb], [hj, dnb], accum_out=dnb[:, 12 + i:13 + i])
                                    rstd_from_ss(dnb, dnb[:, 12:14], dnb[:, 14:16], dnb[:, 14:16], 1.0 / 128)
                                    for i in range(2):
                                        STT(hsb[:, i, :], hsb[:, i, :], dnb[:, 14 + i:15 + i], ghead_b[:, (h0 + i) * 128:(h0 + i + 1) * 128],
                                            ALU.mult, ALU.mult, [hsb, dnb, ghead_b], [hsb])
                                    TT(yb[:], hsb[:].rearrange("p i d -> p (i d)"), sb[:], ALU.mult, [hsb, sb], [yb])
                                    for i in range(2):
                                        TR(pTy[:, i, :], yb[:, i * 128:(i + 1) * 128], ident_b[:], [yb, ident_b], [pTy])
                                    ACT(yT_m[:, h0:h0 + 2, cs], pTy[:], AF.Copy, [pTy], [yT_m])
                        fw.barrier()

                    with fw.scope():
                        wo = fw.sbuf("wo", [128, 8, D], BF16)
                        wr = fw.sbuf("wr", [128, 8, NE], BF16)
                        gffn_b = fw.sbuf("gffn_b", [128, D], F32)
                        xt5 = [fw.sbuf("xt5_%d" % i, [128, D], F32) for i in range(2)]
                        xo = [fw.sbuf("xo%d" % i, [128, D], F32) for i in range(2)]
                        xj5 = fw.sbuf("xj5", [128, D], F32)
                        hf = [fw.sbuf("hf%d" % i, [128, D], BF16) for i in range(2)]
                        hfT = [fw.sbuf("hfT%d" % i, [128, 8, 128], BF16) for i in range(2)]
                        st5 = [fw.sbuf("st5_%d" % i, [128, 8], F32) for i in range(2)]
                        ex5 = [fw.sbuf("ex5_%d" % i, [128, NE], F32) for i in range(2)]
                        po5 = [fw.psum("po5_%d" % i, [128, D], F32) for i in range(2)]
                        pT5 = [fw.psum("pT5_%d" % i, [128, 8, 128], BF16) for i in range(2)]
                        plog = fw.psum("plog", [128, NE], F32)
                        fw.dma("pool", wo[:], w_out[l].rearrange("(k p) n -> p k n", p=128), [w_out], [wo], wo)
                        if debug and l == 0:
                            fw.dma("sp", dbg_yTm.ap, yT_m[:], [yT_m], [dbg_yTm], yT_m)
                            fw.dma("sp", dbg_yTnc.ap, yT_nc[:], [yT_nc], [dbg_yTnc], yT_nc)
                        fw.dma("pool", wr[:], w_rt[l].rearrange("(k p) n -> p k n", p=128), [w_rt], [wr], wr)
                        fw.dma("sp", gffn_b[:], g_ffn[l:l + 1, :].partition_broadcast(128), [g_ffn], [gffn_b], gffn_b)
                        def p5_mm(t):
                            b = t % 2
                            ts_ = slice(t * 128, (t + 1) * 128)
                            fw.dma("sp", xt5[b][:], x_src[ts_, :], [out_tiles[t]], [xt5[b]], xt5[b])
                            for n in range(2):
                                for k in range(8):
                                    src = yT_m if k < 4 else yT_nc
                                    MM(po5[b][:, n * 512:(n + 1) * 512], src[:, k % 4, ts_], wo[:, k, n * 512:(n + 1) * 512],
                                       [src, wo], [po5[b]], start=(k == 0), stop=(k == 7))

                        p5_mm(0)
                        for t in range(NT):
                            b = t % 2
                            ts_ = slice(t * 128, (t + 1) * 128)
                            if t + 1 < NT:
                                p5_mm(t + 1)
                            TT(xo[b][:], po5[b][:], xt5[b][:], ALU.add, [po5[b], xt5[b]], [xo[b]])
                            fw.dma("sp", out_d[ts_, :], xo[b][:], [xo[b]], [out_tiles[t]], xo[b])
                            if debug and l == 0:
                                fw.dma("sp", dbg_xmix[ts_, :], xo[b][:], [xo[b]], [dbg_xmix], xo[b])
                            MS(st5[b][:, 0:1], 0.0, [st5[b]])
                            ACT(xj5[:], xo[b][:], AF.Square, [xo[b]], [xj5, st5[b]], accum_out=st5[b][:, 0:1])
                            rstd_from_ss(st5[b], st5[b][:, 0:1], st5[b][:, 1:2], st5[b][:, 2:3], 1.0 / D)
                            STT(hf[b][:], xo[b][:], st5[b][:, 2:3], gffn_b[:], ALU.mult, ALU.mult, [xo[b], st5[b], gffn_b], [hf[b]])
                            fw.dma("sp", hf_d[ts_, :], hf[b][:], [hf[b]], [hf_d], hf[b])
                            for k in range(8):
                                TR(pT5[b][:, k, :], hf[b][:, k * 128:(k + 1) * 128], ident_b[:], [hf[b], ident_b], [pT5[b]])
                            ACT(hfT[b][:], pT5[b][:], AF.Copy, [pT5[b]], [hfT[b]])
                            for k in range(8):
                                MM(plog[:], hfT[b][:, k, :], wr[:, k, :], [hfT[b], wr], [plog], start=(k == 0), stop=(k == 7))
                            MS(st5[b][:, 5:6], 0.0, [st5[b]])
                            ACT(ex5[b][:], plog[:], AF.Exp, [plog, st5[b]], [ex5[b], st5[b]], accum_out=st5[b][:, 5:6])
                            RCP(st5[b][:, 6:7], st5[b][:, 5:6], [st5[b]], [st5[b]])
                            TS(aff_all[:, t, :], ex5[b][:], st5[b][:, 6:7], None, ALU.mult, None, [ex5[b], st5[b]], [aff_all])
                        fw.dma("sp", aff_d.ap.rearrange("(i p) e -> p i e", p=128), aff_all[:], [aff_all], [aff_d], aff_all)
                    fw.barrier()

            with fw.scope():
                affT = fw.sbuf("affT", [16, S], F32)
                rjunk = fw.sbuf("rjunk", [16, S], F32)
                rmask = fw.sbuf("rmask", [16, S], F32)
                rones = fw.sbuf("rones", [16, S], F32)
                rs = fw.sbuf("rs", [16, 8], F32)
                pTa = [fw.psum("pTa%d" % i, [16, 512], F32) for i in range(2)]
                pcn = fw.psum("pcn", [128, NT, NE], F32)
                for g4 in range(8):
                    pa = pTa[g4 % 2]
                    for i in range(4):
                        t = g4 * 4 + i
                        TR(pa[:, i * 128:(i + 1) * 128], aff_all[:, t, :], ident_f[:], [aff_all, ident_f], [pa])
                    CP(affT[:, g4 * 512:(g4 + 1) * 512], pa[:], [pa], [affT])
                MS(rones[:], 1.0, [rones])
                MS(rs[:, 0:1], 0.0, [rs])
                MS(rs[:, 1:2], 1.0, [rs])
                for it in range(28):
                    TT(rs[:, 2:3], rs[:, 0:1], rs[:, 1:2], ALU.add, [rs], [rs])
                    TS(rs[:, 2:3], rs[:, 2:3], 0.5, None, ALU.mult, None, [rs], [rs])
                    TS(rjunk[:], affT[:], rs[:, 2:3], 0.0, ALU.is_ge, ALU.add, [affT, rs], [rjunk, rs], accum_out=rs[:, 3:4])
                    TS(rs[:, 4:5], rs[:, 3:4], float(CAP), None, ALU.is_ge, None, [rs], [rs])
                    TT(rs[:, 5:6], rs[:, 2:3], rs[:, 0:1], ALU.subtract, [rs], [rs])
                    STT(rs[:, 0:1], rs[:, 5:6], rs[:, 4:5], rs[:, 0:1], ALU.mult, ALU.add, [rs], [rs])
                    TT(rs[:, 5:6], rs[:, 1:2], rs[:, 2:3], ALU.subtract, [rs], [rs])
                    STT(rs[:, 1:2], rs[:, 5:6], rs[:, 4:5], rs[:, 2:3], ALU.mult, ALU.add, [rs], [rs])
                TS(rmask[:], affT[:], rs[:, 0:1], None, ALU.is_ge, None, [affT, rs], [rmask])
                op("dve", lambda e: e.tensor_tensor_scan(out=rjunk[:], data0=rones[:], data1=rmask[:], initial=0.0,
                                                         op0=ALU.mult, op1=ALU.add), [rones, rmask], [rjunk])
                for t in range(NT):
                    TR(pcn[:, t, :], rjunk[:, t * 128:(t + 1) * 128], ident_f[0:16, 0:16], [rjunk, ident_f], [pcn])
                CP(cnt_all[:], pcn[:], [pcn], [cnt_all])
            fw.barrier()

            with fw.scope():
                NB13 = 3
                w1c = [fw.sbuf("w1c%d" % i, [128, 8, 512], BF16) for i in range(NB13)]
                w3c = [fw.sbuf("w3c%d" % i, [128, 8, 512], BF16) for i in range(NB13)]
                w2c = [fw.sbuf("w2c%d" % i, [128, 4, D], BF16) for i in range(4)]
                xes = [fw.sbuf("xes%d" % i, [128, D], BF16) for i in range(4)]
                gts = [fw.sbuf("gts%d" % i, [128, NE], F32) for i in range(8)]
                xeT = [fw.sbuf("xeT%d" % i, [128, 8, 512], BF16) for i in range(2)]
                hidT = fw.sbuf("hidT", [128, 16, 512], BF16)
                stmp = [fw.sbuf("stmp%d" % i, [128, 512], F32) for i in range(2)]
                ye = [fw.sbuf("ye%d" % i, [128, D], F32) for i in range(2)]
                idxf = fw.sbuf("idxf", [128, NE * 4], I32)
                cacc7 = [fw.sbuf("cacc7_%d" % i, [128, 512], BF16) for i in range(2)]
                pT7 = fw.psum("pT7", [128, 8, 128], BF16)
                ph = [fw.psum("ph%d" % i, [128, 512], F32) for i in range(4)]
                py = [fw.psum("py%d" % i, [128, 512], F32) for i in range(2)]
                pidx = fw.psum("pidx", [128, NE * 4], F32)
                idx_objs = [Obj("idx%d" % e, None) for e in range(NE)]
                idxg = [fw.sbuf("idxg%d" % i, [128, 1], I32) for i in range(4)]
                idxs = [fw.sbuf("idxs%d" % i, [128, 1], I32) for i in range(4)]
                w13_i = [0]

                def load_w13(e, c):
                    bi = w13_i[0] % NB13
                    w13_i[0] += 1
                    fw.dma("pool", w1c[bi][:], w1[l, e, :, c * 512:(c + 1) * 512].rearrange("(k p) f -> p k f", p=128),
                           [w1], [w1c[bi]], w1c[bi])
                    fw.dma("pool", w3c[bi][:], w3[l, e, :, c * 512:(c + 1) * 512].rearrange("(k p) f -> p k f", p=128),
                           [w3], [w3c[bi]], w3c[bi])
                    return bi

                def load_w2(e):
                    for c in range(4):
                        fw.dma("pool", w2c[c][:], w2[l, e, c * 512:(c + 1) * 512, :].rearrange("(c p) d -> p c d", p=128),
                               [w2], [w2c[c]], w2c[c])

                def build_idx_gen(e):
                    a = cacc7[e % 2]
                    MS(a[:], 0.0, [a])
                    for t in range(NT):
                        STT(a[:], iota512[:], cnt_all[:, t, e:e + 1], a[:], ALU.is_ge, ALU.add, [iota512, cnt_all, a], [a])
                        if t % 2 == 1:
                            yield
                    for g in range(4):
                        MM(pidx[:, e * 4 + g:e * 4 + g + 1], a[:, g * 128:(g + 1) * 128], ones_b[:, 0:1], [a, ones_b], [pidx])
                    CP(idxf[:, e * 4:(e + 1) * 4], pidx[:, e * 4:(e + 1) * 4], [pidx], [idx_objs[e]])

                def gather(e):
                    for g in range(4):
                        col = e * 4 + g
                        CP(idxg[g][:, 0:1], idxf[:, col:col + 1], [idx_objs[e]], [idxg[g]])
                        op("pool", lambda en, g=g: en.indirect_dma_start(
                            out=xes[g][:], out_offset=None, in_=hf_d.ap,
                            in_offset=bass.IndirectOffsetOnAxis(ap=idxg[g][:, 0:1], axis=0),
                            bounds_check=regs["bc"], oob_is_err=False), [hf_d, idxg[g]], [xes[g]], dma=True, sem_obj=xes[g])
                        gt = gts[(e % 2) * 4 + g]
                        op("pool", lambda en, g=g, gt=gt: en.indirect_dma_start(
                            out=gt[:], out_offset=None, in_=aff_d.ap,
                            in_offset=bass.IndirectOffsetOnAxis(ap=idxg[g][:, 0:1], axis=0),
                            bounds_check=regs["bc"], oob_is_err=False), [aff_d, idxg[g]], [gt], dma=True, sem_obj=gt)

                def build_idx(e):
                    for _ in build_idx_gen(e):
                        pass

                build_idx(0)
                pending = {}
                for c in range(3):
                    pending[(0, c)] = load_w13(0, c)
                for g in range(4):
                    MS(xes[g][:], 0.0, [xes[g]])
                    MS(gts[g][:], 0.0, [gts[g]])
                    MS(gts[4 + g][:], 0.0, [gts[4 + g]])
                gather(0)
                load_w2(0)
                hi_ = 0
                for e in range(NE):
                    xT = xeT[e % 2]
                    gen = build_idx_gen(e + 1) if e + 1 < NE else iter(())
                    gate_cols = []
                    for g in range(4):
                        for k in range(8):
                            TR(pT7[:, k, :], xes[g][:, k * 128:(k + 1) * 128], ident_b[:], [xes[g], ident_b], [pT7])
                        ACT(xT[:, :, g * 128:(g + 1) * 128], pT7[:], AF.Copy, [pT7], [xT])
                    for c in range(4):
                        bi = pending.pop((e, c))
                        for fcl in range(4):
                            fc = c * 4 + fcl
                            p1 = ph[hi_ % 4]; hi_ += 1
                            p3 = ph[hi_ % 4]; hi_ += 1
                            for k in range(8):
                                MM(p1[:], w1c[bi][:, k, fcl * 128:(fcl + 1) * 128], xT[:, k, :], [w1c[bi], xT], [p1],
                                   start=(k == 0), stop=(k == 7))
                            for k in range(8):
                                MM(p3[:], w3c[bi][:, k, fcl * 128:(fcl + 1) * 128], xT[:, k, :], [w3c[bi], xT], [p3],
                                   start=(k == 0), stop=(k == 7))
                            sb = stmp[fc % 2]
                            ACT(sb[:], p1[:], AF.Silu, [p1], [sb])
                            TT(hidT[:, fc, :], sb[:], p3[:], ALU.mult, [sb, p3], [hidT])
                            next(gen, None)
                        if c == 0:
                            pending[(e, 3)] = load_w13(e, 3)
                        elif e + 1 < NE:
                            pending[(e + 1, c - 1)] = load_w13(e + 1, c - 1)
                    for _ in gen:
                        pass
                    if e + 1 < NE:
                        gather(e + 1)
                    for g in range(4):
                        yb = ye[g % 2]
                        for n in range(2):
                            pyb = py[(g * 2 + n) % 2]
                            for fc in range(16):
                                MM(pyb[:], hidT[:, fc, g * 128:(g + 1) * 128], w2c[fc // 4][:, fc % 4, n * 512:(n + 1) * 512],
                                   [hidT, w2c[fc // 4]], [pyb], start=(fc == 0), stop=(fc == 15))
                            gt = gts[(e % 2) * 4 + g]
                            ACT(yb[:, n * 512:(n + 1) * 512], pyb[:], AF.Copy, [pyb, gt], [yb], scale=gt[:, e:e + 1])
                        col = e * 4 + g
                        CP(idxs[g][:, 0:1], idxf[:, col:col + 1], [idx_objs[e]], [idxs[g]])
                        op("pool", lambda en, yb=yb, g=g: en.indirect_dma_start(
                            out=out_d.ap, out_offset=bass.IndirectOffsetOnAxis(ap=idxs[g][:, 0:1], axis=0),
                            in_=yb[:], in_offset=None, bounds_check=regs["bc"], oob_is_err=False, compute_op=ALU.add),
                            [yb, idxs[g]], out_tiles, dma=True, sem_obj=yb)
                    if e + 1 < NE:
                        load_w2(e + 1)
                if debug and l == 0:
                    fw.dma("sp", dbg_idx.ap, idxf[:], idx_objs, [dbg_idx], idxf)
            fw.barrier()


def _natab(rpb):
    L = rpb.shape[0]
    pad = np.concatenate([rpb.reshape(L, 4, 15 * 31), np.full((L, 4, 1), -30000.0, np.float32)], axis=2)
    idx = np.zeros((5, 128, 5, 128), np.int64)
    k = np.arange(128)[:, None, None]
    dl = np.arange(5)[None, :, None]
    q = np.arange(128)[None, None, :]
    for p, j in enumerate([0, 1, 2, 30, 31]):
        kb = min(max(j - 2, 0), 27)
        key = (kb + dl) * 128 + k
        qq = j * 128 + q
        rk, ck = key // 64, key % 64
        rq, cq = qq // 64, qq % 64
        rs = np.clip(rq - 4, 0, 56)
        cs = np.clip(cq - 8, 0, 48)
        ok = (rk >= rs) & (rk < rs + 8) & (ck >= cs) & (ck < cs + 16)
        ii = (rk - rq + 7) * 31 + (ck - cq + 15)
        idx[p] = np.where(ok, ii, 465)
    tab = pad[:, :, idx]
    tab = np.ascontiguousarray(tab.transpose(0, 2, 3, 1, 4, 5)).reshape(L, 5, 128, 2560)
    return tab.astype(np.float32)


def _layer_inputs(inp, ls):
    f = lambda a: np.ascontiguousarray(a, dtype=np.float32)
    L = len(ls)
    sel = lambda k: f(np.asarray(inp[k])[ls])
    convw = sel("conv_qk").transpose(0, 2, 1).reshape(L, 4, 128, 5).transpose(0, 2, 1, 3)
    gc = np.stack([np.tile(sel(k), (1, 2)) for k in ("na_gq", "na_gk", "mem_gq", "mem_gk")], axis=2)
    return {
        "g_mix": sel("g_mix"), "w_in": sel("w_in"), "b_gates": sel("b_gates"), "convw": f(convw),
        "g_head": sel("g_mlstm_head"), "gcols": f(gc), "natab": _natab(sel("na_rpb")),
        "g_mem": sel("g_mem"), "w_mem_kv": sel("w_mem_kv"), "w_out": sel("w_out"), "g_ffn": sel("g_ffn"),
        "w_router": sel("w_router"), "w1": sel("w1"), "w3": sel("w3"), "w2": sel("w2"),
    }


_CACHE = {}
N_FUSED_LAYERS = 4


def kernel(**inputs):
    x = np.ascontiguousarray(inputs["x"], dtype=np.float32)
    mem = np.ascontiguousarray(inputs["mem"], dtype=np.float32)
    depth = np.asarray(inputs["g_mix"]).shape[0]
    nl = N_FUSED_LAYERS
    if nl not in _CACHE:
        _CACHE[nl] = build(nl)[0]
    nc = _CACHE[nl]
    cur = x
    for l0 in range(0, depth, nl):
        shared = _layer_inputs(inputs, list(range(l0, l0 + nl)))
        in_maps = []
        for c in range(8):
            m = dict(shared)
            m["x"] = cur[c]
            m["mem"] = mem[c]
            in_maps.append(m)
        res = run_bass_kernel_spmd(nc, in_maps, core_ids=list(range(8)))
        cur = np.stack([np.asarray(r["out"]) for r in res.results], axis=0).astype(np.float32)
    return cur
```
